# Optimizing a Trainium2 kernel written in Bass

```python
import jax, jax.numpy as jnp
from jax import lax
import numpy as np

D_MODEL = 1024
BATCH = 4
SEQ = 4096
DEPTH = 1

PLE_DIM = 256
RW_HEADS = 8
RW_HEAD = 64
RW_DIM = RW_HEADS * RW_HEAD
RW_DECAY_LORA = 64
RW_A_LORA = 64
RW_GATE_LORA = 128
RW_GN_EPS = 64e-5
HG_HEADS = 4
HG_EXPAND = 128
HG_HEAD_V = 128
HG_KDIM = HG_HEADS * HG_EXPAND
HG_VDIM = HG_HEADS * HG_HEAD_V
HG_CHUNK = 64
N_BRANCH = 2
C_R = 0
C_K = C_R + RW_DIM
C_V = C_K + RW_DIM
C_WD = C_V + RW_DIM
C_AD = C_WD + RW_DECAY_LORA
C_GD = C_AD + RW_A_LORA
C_RW_END = C_GD + RW_GATE_LORA
C_Q = C_RW_END
C_F = C_Q + HG_KDIM
C_I = C_F + HG_KDIM
C_OG = C_I + HG_VDIM
C_GATE = C_OG + HG_VDIM
N_IN = C_GATE + N_BRANCH * D_MODEL
RW_COLS = C_RW_END
N_GROUPS = 4
EXPERTS_PER_GROUP = 8
N_EXPERTS = N_GROUPS * EXPERTS_PER_GROUP
TOP_K = 2
D_EXPERT = 512
MOE_BLOCK = 128
LN_EPS = 1e-5
RMS_EPS = 1e-6
DEEPNORM_ALPHA = (2 * DEPTH) ** 0.25
DEEPNORM_BETA = (8 * DEPTH) ** -0.25

kernel_name = 'hybrid_rwkv7_hgrn2_hmoe_deepnorm'


def layer_norm(x, g, b):
    xf = x.astype(jnp.float32)
    mu = jnp.mean(xf, -1, keepdims=True)
    var = jnp.mean(jnp.square(xf - mu), -1, keepdims=True)
    return ((xf - mu) * lax.rsqrt(var + LN_EPS) * g + b).astype(x.dtype)


def token_shift(u):
    return jnp.pad(u, ((0, 0), (1, 0), (0, 0)))[:, :-1]


def rwkv7_scan(r, w, k, v, a_vec, b_vec):
    B, T, H, N = r.shape

    def step(S, inp):
        r_t, w_t, k_t, v_t, a_t, b_t = inp
        sa = jnp.einsum('bhij,bhj->bhi', S, a_t)
        S = S * w_t[:, :, None, :] + sa[..., None] * b_t[:, :, None, :] + v_t[..., None] * k_t[:, :, None, :]
        return S, jnp.einsum('bhij,bhj->bhi', S, r_t)

    seqs = tuple(jnp.moveaxis(t, 1, 0) for t in (r, w, k, v, a_vec, b_vec))
    _, y = lax.scan(step, jnp.zeros((B, H, N, N), jnp.float32), seqs)
    return jnp.moveaxis(y, 0, 1)


def rwkv7_branch(u, mu, w0, w_up, a0, a_up, g_up, k_k, k_a, r_k, gn_w, gn_b):
    B, T, _ = u.shape
    dt = u.dtype
    u = u + (token_shift(u) - u) * mu
    r = u[..., C_R:C_K]
    k = u[..., C_K:C_V]
    v = u[..., C_V:C_WD]
    xw = u[..., C_WD:C_AD]
    xa = u[..., C_AD:C_GD]
    xg = u[..., C_GD:C_RW_END]
    w_log = -jax.nn.softplus(-(w0 + jnp.tanh(xw) @ w_up)) - 0.5
    decay = jnp.exp(-jnp.exp(w_log.astype(jnp.float32)))
    a = jax.nn.sigmoid(a0 + xa @ a_up)
    g = jax.nn.sigmoid(xg) @ g_up
    hs = lambda t: t.astype(jnp.float32).reshape(B, T, RW_HEADS, RW_HEAD)
    kk = hs(k * k_k)
    kk = kk / jnp.maximum(jnp.sqrt(jnp.sum(kk * kk, -1, keepdims=True)), 1e-12)
    k = k * (1.0 + (a - 1.0) * k_a)
    rh, kh, vh, ah = hs(r), hs(k), hs(v), hs(a)
    y = rwkv7_scan(rh, decay.reshape(B, T, RW_HEADS, RW_HEAD), kh, vh, -kk, kk * ah)
    m = jnp.mean(y, -1, keepdims=True)
    var = jnp.mean(jnp.square(y - m), -1, keepdims=True)
    y = ((y - m) * lax.rsqrt(var + RW_GN_EPS)).reshape(B, T, RW_DIM) * gn_w + gn_b
    bonus = jnp.sum(rh * kh * r_k, -1, keepdims=True) * vh
    y = (y + bonus.reshape(B, T, RW_DIM)) * g
    return y.astype(dt)


def hgrn2_branch(u, lb, norm_w):
    B, T, _ = u.shape
    dt = u.dtype
    z = u[..., HG_KDIM:2 * HG_KDIM].astype(jnp.float32)
    f = lb + (1.0 - lb) * jax.nn.sigmoid(z)
    k = (1.0 - lb) * jax.nn.sigmoid(-z)
    logf = jnp.log(f)
    q = jax.nn.silu(u[..., :HG_KDIM].astype(jnp.float32))
    vin = u[..., 2 * HG_KDIM:2 * HG_KDIM + HG_VDIM].astype(jnp.float32)
    og = u[..., 2 * HG_KDIM + HG_VDIM:].astype(jnp.float32)
    nc = T // HG_CHUNK

    def chunks(t, d):
        return t.reshape(B, nc, HG_CHUNK, HG_HEADS, d).transpose(1, 0, 3, 2, 4)

    qc, kc = chunks(q, HG_EXPAND), chunks(k, HG_EXPAND)
    bc = jnp.cumsum(chunks(logf, HG_EXPAND), axis=3)
    vc = chunks(vin, HG_HEAD_V)
    mask = jnp.tril(jnp.ones((HG_CHUNK, HG_CHUNK), bool))[:, :, None]

    def step(S, inp):
        q_c, k_c, b_c, v_c = inp
        diff = b_c[:, :, :, None, :] - b_c[:, :, None, :, :]
        dec = jnp.exp(jnp.where(mask, diff, -jnp.inf))
        att = jnp.einsum('bhte,bhse,bhtse->bhts', q_c, k_c, dec)
        o = att @ v_c + jnp.einsum('bhte,bhev->bhtv', q_c * jnp.exp(b_c), S)
        b_last = b_c[:, :, -1:, :]
        S = jnp.exp(b_last[:, :, 0, :])[..., None] * S + jnp.einsum('bhse,bhsv->bhev', k_c * jnp.exp(b_last - b_c), v_c)
        return S, o

    S0 = jnp.zeros((B, HG_HEADS, HG_EXPAND, HG_HEAD_V), jnp.float32)
    _, o = lax.scan(step, S0, (qc, kc, bc, vc))
    o = o.transpose(1, 0, 3, 2, 4).reshape(B, T, HG_HEADS, HG_HEAD_V)
    o = o * lax.rsqrt(jnp.mean(o * o, -1, keepdims=True) + RMS_EPS)
    o = o.reshape(B, T, HG_VDIM) * norm_w * jax.nn.sigmoid(og)
    return o.astype(dt)


def hier_route(xf, wg, bg, we, be):
    pg = jax.nn.softmax((xf @ wg).astype(jnp.float32) + bg, axis=-1)
    pg_sel, gidx = lax.top_k(pg, 1)
    le = ((xf @ we).astype(jnp.float32) + be).reshape(-1, N_GROUPS, EXPERTS_PER_GROUP)
    le_sel = jnp.take_along_axis(le, gidx[:, :, None], axis=1)[:, 0]
    pe = jax.nn.softmax(le_sel, axis=-1)
    pv, eidx = lax.top_k(pe, TOP_K)
    pv = pv / jnp.sum(pv, -1, keepdims=True)
    return gidx * EXPERTS_PER_GROUP + eidx, pg_sel * pv


def moe_experts(xf, eid, ew, w1, w3, w2):
    N, D = xf.shape
    NA = N * TOP_K
    eid_f = eid.reshape(-1)
    ew_f = ew.reshape(-1)
    tok = jnp.repeat(jnp.arange(N, dtype=jnp.int32), TOP_K)
    order = jnp.argsort(eid_f)
    se = eid_f[order]
    counts = jnp.zeros((N_EXPERTS,), jnp.int32).at[eid_f].add(1)
    start = jnp.cumsum(counts) - counts
    padded = (counts + MOE_BLOCK - 1) // MOE_BLOCK * MOE_BLOCK
    pad_end = jnp.cumsum(padded)
    pad_start = pad_end - padded
    dest = pad_start[se] + (jnp.arange(NA, dtype=jnp.int32) - start[se])
    P = (NA + MOE_BLOCK - 1) // MOE_BLOCK * MOE_BLOCK + N_EXPERTS * MOE_BLOCK
    nblk = P // MOE_BLOCK
    tokbuf = jnp.full((P,), N, jnp.int32).at[dest].set(tok[order])
    wbuf = jnp.zeros((P,), jnp.float32).at[dest].set(ew_f[order])
    blk_e = jnp.minimum(jnp.searchsorted(pad_end, jnp.arange(nblk, dtype=jnp.int32) * MOE_BLOCK, side='right'), N_EXPERTS - 1)
    xpad = jnp.concatenate([xf, jnp.zeros((1, D), xf.dtype)], axis=0)
    xbuf = xpad[tokbuf].reshape(nblk, MOE_BLOCK, D)

    def block(args):
        xb, e = args
        h = jax.nn.silu(xb @ w1[e]) * (xb @ w3[e])
        return h @ w2[e]

    ybuf = lax.map(block, (xbuf, blk_e)).reshape(P, D) * wbuf[:, None].astype(xf.dtype)
    return jnp.zeros((N + 1, D), xf.dtype).at[tokbuf].add(ybuf)[:N]


def setup_inputs(seed: int = 0) -> dict:
    key = jax.random.key(seed)
    ks = jax.random.split(key, 40)
    f32 = jnp.float32
    nrm = lambda k, shape, s: jax.random.normal(k, shape, f32) * s
    D = D_MODEL
    return {
        'x': nrm(ks[0], (BATCH, SEQ, D), 1.0),
        'p': nrm(ks[1], (DEPTH, BATCH, SEQ, PLE_DIM), 1.0),
        'w_in': nrm(ks[2], (DEPTH, D, N_IN), D ** -0.5),
        'rw_mu': jax.random.uniform(ks[3], (DEPTH, RW_COLS), f32, 0.1, 0.9),
        'rw_w0': jax.random.uniform(ks[4], (DEPTH, RW_DIM), f32, -6.5, -1.5),
        'rw_w_up': nrm(ks[5], (DEPTH, RW_DECAY_LORA, RW_DIM), 0.1),
        'rw_a0': nrm(ks[6], (DEPTH, RW_DIM), 0.1),
        'rw_a_up': nrm(ks[7], (DEPTH, RW_A_LORA, RW_DIM), RW_A_LORA ** -0.5),
        'rw_g_up': nrm(ks[8], (DEPTH, RW_GATE_LORA, RW_DIM), RW_GATE_LORA ** -0.5),
        'rw_k_k': 0.85 + nrm(ks[9], (DEPTH, RW_DIM), 0.02),
        'rw_k_a': 1.0 + nrm(ks[10], (DEPTH, RW_DIM), 0.02),
        'rw_r_k': nrm(ks[11], (DEPTH, RW_HEADS, RW_HEAD), 0.1),
        'rw_gn_w': 1.0 + nrm(ks[12], (DEPTH, RW_DIM), 0.02),
        'rw_gn_b': nrm(ks[13], (DEPTH, RW_DIM), 0.02),
        'w_a_out': nrm(ks[14], (DEPTH, RW_DIM, D), RW_DIM ** -0.5),
        'hg_lb_logits': nrm(ks[15], (DEPTH + 1, HG_KDIM), 0.1),
        'hg_norm_w': 1.0 + nrm(ks[16], (DEPTH, HG_VDIM), 0.02),
        'w_b_out': nrm(ks[17], (DEPTH, HG_VDIM, D), HG_VDIM ** -0.5),
        'w_o': nrm(ks[18], (DEPTH, D, D), D ** -0.5 * DEEPNORM_BETA),
        'ln1_g': 1.0 + nrm(ks[19], (DEPTH, D), 0.02),
        'ln1_b': nrm(ks[20], (DEPTH, D), 0.02),
        'router_g_w': nrm(ks[21], (DEPTH, D, N_GROUPS), D ** -0.5),
        'router_g_b': nrm(ks[22], (DEPTH, N_GROUPS), 0.01),
        'router_e_w': nrm(ks[23], (DEPTH, D, N_EXPERTS), D ** -0.5),
        'router_e_b': nrm(ks[24], (DEPTH, N_EXPERTS), 0.01),
        'w1': nrm(ks[25], (DEPTH, N_EXPERTS, D, D_EXPERT), D ** -0.5),
        'w3': nrm(ks[26], (DEPTH, N_EXPERTS, D, D_EXPERT), D ** -0.5),
        'w2': nrm(ks[27], (DEPTH, N_EXPERTS, D_EXPERT, D), D_EXPERT ** -0.5 * DEEPNORM_BETA),
        'ln2_g': 1.0 + nrm(ks[28], (DEPTH, D), 0.02),
        'ln2_b': nrm(ks[29], (DEPTH, D), 0.02),
        'w_pe': nrm(ks[30], (DEPTH, PLE_DIM, D), PLE_DIM ** -0.5),
        'w_pg': nrm(ks[31], (DEPTH, D, D), D ** -0.5),
    }


def reference(x, p, w_in, rw_mu, rw_w0, rw_w_up, rw_a0, rw_a_up, rw_g_up, rw_k_k, rw_k_a, rw_r_k,
              rw_gn_w, rw_gn_b, w_a_out, hg_lb_logits, hg_norm_w, w_b_out, w_o, ln1_g, ln1_b,
              router_g_w, router_g_b, router_e_w, router_e_b, w1, w3, w2, ln2_g, ln2_b, w_pe, w_pg):
    B, T, D = x.shape
    lb_all = jnp.cumsum(jax.nn.softmax(hg_lb_logits.astype(jnp.float32), axis=0), axis=0)
    for i in range(DEPTH):
        proj = x @ w_in[i]
        y_a = rwkv7_branch(proj[..., :C_RW_END], rw_mu[i], rw_w0[i], rw_w_up[i], rw_a0[i], rw_a_up[i],
                           rw_g_up[i], rw_k_k[i], rw_k_a[i], rw_r_k[i], rw_gn_w[i], rw_gn_b[i])
        y_b = hgrn2_branch(proj[..., C_Q:C_GATE], lb_all[i].astype(x.dtype) if False else lb_all[i], hg_norm_w[i])
        gates = jax.nn.sigmoid(proj[..., C_GATE:])
        merged = gates[..., :D] * (y_a @ w_a_out[i]) + gates[..., D:] * (y_b @ w_b_out[i])
        x = layer_norm(DEEPNORM_ALPHA * x + merged @ w_o[i], ln1_g[i], ln1_b[i])
        xf = x.reshape(B * T, D)
        eid, ew = hier_route(xf, router_g_w[i], router_g_b[i], router_e_w[i], router_e_b[i])
        ffn = moe_experts(xf, eid, ew, w1[i], w3[i], w2[i]).reshape(B, T, D)
        x = layer_norm(DEEPNORM_ALPHA * x + ffn, ln2_g[i], ln2_b[i])
        x = x + jax.nn.sigmoid(x @ w_pg[i]) * (p[i] @ w_pe[i])
    return x
```

```python
import os
import numpy as np
from contextlib import ExitStack
import concourse.bass as bass
import concourse.mybir as mybir
from concourse.bass_utils import run_bass_kernel_spmd

F32 = mybir.dt.float32
BF16 = mybir.dt.bfloat16
ALU = mybir.AluOpType
AF = mybir.ActivationFunctionType
AX = mybir.AxisListType

NDSEM = 8
T = 4096
TOWN = 2048
D = 1024
NB = 256
NCH = NB // 64
NBLK = T // NB
OWNB = (T - TOWN) // NB
CDEC = 0.6065306597126334
ALPHA = 2.0 ** 0.25
N_IN = 5888
C_Q = 1792
C_GATE = 3840


class Prog:
    def __init__(self, nc, es):
        self.nc = nc
        self.es = es
        self.engs = ['pe', 'act', 'dve', 'pool', 'sp']
        self.streams = {e: [] for e in self.engs}
        self.prods = ['pe', 'act', 'dve', 'pool']
        self.count = {p: 0 for p in self.prods}
        self.sems = {p: es.enter_context(nc.semaphore('s_' + p)) for p in self.prods}
        self.waited = {}
        self.last_write = {}
        self.readers = {}
        self.ndma = 0
        self.uid = 0
        self.floor = {}

    def barrier(self):
        self.floor = dict(self.count)

    def sb(self, es, name, shape, dt=F32):
        self.uid += 1
        return es.enter_context(self.nc.sbuf_tensor('%s_%d' % (name, self.uid), list(shape), dt))

    def _deps(self, eng, prod, reads, writes):
        deps = {}
        writes = list(writes) + [k for k in reads if k.startswith('bank')]
        reads = [k for k in reads if not k.startswith('bank')]

        def add(p, n):
            if n > deps.get(p, 0):
                deps[p] = n
        for k in reads:
            if k in self.last_write:
                add(*self.last_write[k])
        for k in writes:
            if k in self.last_write:
                add(*self.last_write[k])
            for p, n in self.readers.get(k, {}).items():
                add(p, n)
        waits = []
        for p, n in self.floor.items():
            if n > deps.get(p, 0) and self.waited.get((eng, p), 0) < n:
                deps[p] = n
        for p, n in deps.items():
            if p == 'pe' and eng == 'pe' and self.floor.get('pe', 0) < n:
                continue
            if self.waited.get((eng, p), 0) >= n:
                continue
            self.waited[(eng, p)] = n
            waits.append((p, n))
        self.count[prod] += 1
        idx = self.count[prod]
        for k in writes:
            self.last_write[k] = (prod, idx)
            self.readers[k] = {}
        for k in reads:
            rd = self.readers.setdefault(k, {})
            if rd.get(prod, 0) < idx:
                rd[prod] = idx
        return waits

    def op(self, eng, fn, reads=(), writes=()):
        self.nops = getattr(self, 'nops', 0) + 1
        if self.nops > int(os.environ.get('KMAX', '100000000')):
            return
        waits = self._deps(eng, eng, reads, writes)
        self.streams[eng].append((waits, fn, eng))

    def dma(self, out, in_, reads=(), writes=(), eng='sp'):
        self.nops = getattr(self, 'nops', 0) + 1
        if self.nops > int(os.environ.get('KMAX', '100000000')):
            return
        skey = [k for k in list(writes) + list(reads) if not k.startswith('dram_')][0]
        prod = 'd_' + skey
        if prod not in self.sems:
            self.sems[prod] = self.es.enter_context(self.nc.semaphore('s_' + prod))
            self.count[prod] = 0
            self.prods.append(prod)
        self.ndma += 1
        waits = self._deps(eng, prod, reads, writes)
        self.streams[eng].append((waits, lambda e: e.dma_start(out=out, in_=in_), prod))

    def emit(self):
        self.flush(final=True)

    def flush(self, final=False):
        nc = self.nc
        if not final and not any(self.streams.values()):
            return
        streams = self.streams
        self.streams = {e: [] for e in self.engs}

        def run(ename, e):
            for waits, fn, prod in streams[ename]:
                for p, n in waits:
                    e.wait_ge(self.sems[p], n * 16 if p[0] == 'd' else n)
                ins = fn(e)
                ins.then_inc(self.sems[prod], 16 if prod[0] == 'd' else 1)
            if ename == 'sp' and final:
                for p in self.prods:
                    if self.count[p] > 0:
                        e.wait_ge(self.sems[p], self.count[p] * (16 if p[0] == 'd' else 1))
        with nc.Block() as block:
            @block.tensor
            def _(e):
                run('pe', e)

            @block.scalar
            def _(e):
                run('act', e)

            @block.vector
            def _(e):
                run('dve', e)

            @block.gpsimd
            def _(e):
                run('pool', e)

            @block.sync
            def _(e):
                run('sp', e)


class PsumAlloc:
    def __init__(self, P, es):
        self.banks = [es.enter_context(P.nc.psum_tensor('psb%d' % i, [128, 512], F32)) for i in range(8)]
        self.used = [0] * 8
        self.n = 0

    def alloc(self, cols, bank):
        b = bank
        assert self.used[b] + cols <= 512, (bank, cols, self.used[b])
        o = self.used[b]
        self.used[b] += cols
        return self.banks[b][:, o:o + cols], 'bank%d' % b

    def reset(self):
        self.used = [0] * 8


def colvec(v, n):
    return np.ascontiguousarray(np.asarray(v, np.float32).reshape(n, 128).T)


def host_consts():
    c = {}
    c['ident'] = np.eye(128, dtype=np.float32)
    su = np.triu(np.ones((64, 64), np.float32), 1)
    sl = np.tril(np.ones((64, 64), np.float32), -1)
    iu = np.triu(np.ones((64, 64), np.float32), 0)
    m = np.concatenate([su, sl, su, iu, iu], 1)
    c['mask320'] = np.concatenate([m, m], 0)
    c['iu2'] = np.concatenate([iu, iu], 0)
    i64 = np.eye(64, dtype=np.float32)
    ii = np.concatenate([i64, i64], 1)
    c['ii'] = np.concatenate([ii, ii], 0)
    ob = np.zeros((128, 128), np.float32)
    ob[:64, :64] = 1
    ob[64:, 64:] = 1
    c['onesblk'] = ob
    sm = np.ones((128, NB), np.float32)
    sm[:, ::64] = 0
    c['scanmask'] = sm
    sel = np.zeros((32, 32 * 128), np.float32)
    for e in range(32):
        sel[e, e * 128:(e + 1) * 128] = 1
    c['sel'] = sel
    return c


PV = {}
_o = 0
for _name, _n in [('w0', 4), ('a0', 4), ('k_k', 4), ('k_a', 4), ('r_k', 4), ('lb0', 4), ('lb1', 4)]:
    PV[_name] = _o
    _o += _n
NPV = _o
ROWS = {}
_o = 0
for _name, _n in [('mu', 1792), ('gn_w', 512), ('gn_b', 512), ('hg_nw', 512), ('ln1_g', 1024), ('ln1_b', 1024),
                  ('ln2_g', 1024), ('ln2_b', 1024), ('rb', 36)]:
    ROWS[_name] = (_o, _n)
    _o += _n
NROWS = _o


def build(stage='full', nblk=NBLK, ownb=OWNB):
    nc = bass.Bass("TRN2", target_bir_lowering=False)

    def din(name, shape, dt=F32):
        return nc.dram_tensor(name, list(shape), dt, kind="ExternalInput").ap()

    def dout(name, shape, dt=F32):
        return nc.dram_tensor(name, list(shape), dt, kind="ExternalOutput").ap()

    xT = din('xT', [D, T])
    w_in = din('w_in', [D, N_IN])
    pv_d = din('pv', [128, NPV])
    rows_d = din('rows', [1, NROWS])
    ident_d = din('ident', [128, 128])
    mask320_d = din('mask320', [128, 320])
    iu2_d = din('iu2', [128, 64])
    ii_d = din('ii', [128, 128])
    onesblk_d = din('onesblk', [128, 128])
    scanmask_d = din('scanmask', [128, NB])
    w_up_d = din('rw_w_up', [64, 512])
    a_up_d = din('rw_a_up', [64, 512])
    g_up_d = din('rw_g_up', [128, 512])
    gnw_d = din('gnw_t', [4, 128, 64])
    gnb_d = din('gnb_t', [4, 128, 64])

    if stage == 'full':
        xo_d = din('xo', [TOWN, D])
        pT_d = din('pT', [256, TOWN])
        w_a_out_d = din('w_a_out', [512, D])
        w_b_out_d = din('w_b_out', [512, D])
        w_o_d = din('w_o', [D, D])
        rw_d = din('rw', [D, 36])
        w1_d = din('w1', [32, D, 512])
        w3_d = din('w3', [32, D, 512])
        w2_d = din('w2', [32, 512, D])
        w_pe_d = din('w_pe', [256, D])
        w_pg_d = din('w_pg', [D, D])
    yaT_d = nc.dram_tensor('yaT_s', [512, TOWN], BF16, kind="Internal").ap()
    ybT_d = nc.dram_tensor('ybT_s', [512, TOWN], BF16, kind="Internal").ap()
    if stage == 'rwkv':
        ya_dbg = dout('ya_dbg', [TOWN // 64, 4, 128, 64])
    elif stage == 'hgrn':
        yb_dbg = dout('yb_dbg', [TOWN, 512])
    elif stage == 'fullA':
        yaT_o = dout('yaT_o', [512, TOWN], BF16)
        ybT_o = dout('ybT_o', [512, TOWN], BF16)
    else:
        out_d = dout('out', [TOWN, D])

    with ExitStack() as es0:
        P = Prog(nc, es0)
        PSA = PsumAlloc(P, es0)
        ident = P.sb(es0, 'ident', [128, 128])
        pvt = P.sb(es0, 'pv', [128, NPV])
        P.dma(ident[:], ident_d, writes=['ident'])
        P.dma(pvt[:], pv_d, writes=['pv'])

        def pcol(name, i):
            o = PV[name] + i
            return pvt[:, o:o + 1]

        with ExitStack() as es:
            PSA.reset()
            mask320 = P.sb(es, 'mask320', [128, 320])
            iit = P.sb(es, 'ii', [128, 128])
            onesblk = P.sb(es, 'onesblk', [128, 128])
            scanmask = P.sb(es, 'scanmask', [128, NB])
            P.dma(mask320[:], mask320_d, writes=['mask320'])
            P.dma(iit[:], ii_d, writes=['ii'])
            P.dma(onesblk[:], onesblk_d, writes=['onesblk'])
            P.dma(scanmask[:], scanmask_d, writes=['scanmask'])
            wA = P.sb(es, 'wA', [128, 8, 1792], BF16)
            wB = P.sb(es, 'wB', [128, 8, 1792], BF16)
            LW = P.sb(es, 'LW', [128, 512], BF16)
            LW2 = P.sb(es, 'LW2', [128, 512], BF16)
            GU = P.sb(es, 'GU', [128, 512], BF16)
            gnw = P.sb(es, 'gnw', [128, 4, 64])
            gnb = P.sb(es, 'gnb', [128, 4, 64])
            for hp in range(4):
                P.dma(gnw[:, hp, :], gnw_d[hp], writes=['gnw'])
                P.dma(gnb[:, hp, :], gnb_d[hp], writes=['gnb'])
            if os.environ.get('KSTOP') == '1':
                P.emit()
                return nc
            with ExitStack() as es1:
                MU = P.sb(es1, 'MU', [128, 1792])
                OMM = P.sb(es1, 'OMM', [128, 1792])
                o, n = ROWS['mu']
                P.dma(MU[:], rows_d[:, o:o + n].partition_broadcast(128), writes=['MU'])
                P.op('dve', lambda e: e.tensor_scalar(out=OMM[:], in0=MU[:], scalar1=-1.0, scalar2=1.0,
                                                      op0=ALU.mult, op1=ALU.add), reads=['MU'], writes=['OMM'])
                stg = [P.sb(es1, 'wstg', [128, 1792]) for _ in range(2)]
                for dc in range(8):
                    s = stg[dc % 2]
                    k = 'wstg%d' % (dc % 2)
                    P.dma(s[:], w_in[dc * 128:(dc + 1) * 128, 0:1792], writes=[k])
                    P.op('dve', lambda e, s=s, dc=dc: e.tensor_tensor(out=wA[:, dc, :], in0=s[:], in1=OMM[:], op=ALU.mult),
                         reads=[k, 'OMM'], writes=['wA'])
                    P.op('pool', lambda e, s=s, dc=dc: e.tensor_tensor(out=wB[:, dc, :], in0=s[:], in1=MU[:], op=ALU.mult),
                         reads=[k, 'MU'], writes=['wB'])
                if os.environ.get('KSTOP') == '2':
                    P.emit()
                    return nc
                ls = P.sb(es1, 'lstg', [128, 512])
                P.dma(ls[0:64, :], w_up_d, writes=['lstg'])
                P.dma(ls[64:128, :], a_up_d, writes=['lstg'])
                P.op('pool', lambda e: e.memset(LW[:], 0.0), writes=['LW'])
                P.op('pool', lambda e: e.memset(LW2[:], 0.0), writes=['LW'])
                P.op('dve', lambda e: e.tensor_copy(out=LW[0:64, :], in_=ls[0:64, :]), reads=['lstg'], writes=['LW'])
                P.op('dve', lambda e: e.tensor_copy(out=LW2[64:128, :], in_=ls[64:128, :]), reads=['lstg'], writes=['LW'])
                gs = P.sb(es1, 'gstg', [128, 512])
                P.dma(gs[:], g_up_d, writes=['gstg'])
                P.op('dve', lambda e: e.tensor_copy(out=GU[:], in_=gs[:]), reads=['gstg'], writes=['GU'])
                P.flush()
            if os.environ.get('KSTOP') == '3':
                P.emit()
                return nc
            if os.environ.get('KNOBAR') != '1':
                P.barrier()

            xstg = [P.sb(es, 'xstg', [128, NB + 1]) for _ in range(2)]
            xb = [P.sb(es, 'xb', [128, 8, NB], BF16) for _ in range(2)]
            xbs_ = [P.sb(es, 'xbs', [128, 8, NB], BF16) for _ in range(2)]
            FM = {}
            for nm in ['Rr', 'Kr', 'Vt', 'At', 'Bt', 'Kt', 'Rt', 'rk', 'eG']:
                FM[nm] = [P.sb(es, nm, [128, NB]) for _ in range(4)]
            tmp = {nm: P.sb(es, nm, [128, NB]) for nm in ['sg', 'a', 'Gs', 'Gp', 'eGn', 'eGp', 'kk', 'sq', 'rn', 'kkn', 't1', 'kp']}
            LX = P.sb(es, 'LX', [128, NB], BF16)
            SGX = P.sb(es, 'SGX', [128, NB], BF16)
            HH = [[P.sb(es, 'H', [128, 64]) for _ in range(2)] for _ in range(4)]
            for hp in range(4):
                P.op('pool', lambda e, hp=hp: e.memset(HH[hp][0][:], 0.0), writes=['H%d_0' % hp])
            NPAR = 2
            TM = [P.sb(es, 'TM', [128, 4, 64]) for _ in range(NPAR)]
            MM = [P.sb(es, 'MM', [128, 320]) for _ in range(NPAR)]
            TT = [[P.sb(es, 'TT', [128, 128]) for _ in range(2)] for _ in range(NPAR)]
            PP = [[P.sb(es, 'PP', [128, 128]) for _ in range(2)] for _ in range(NPAR)]
            X2s = [P.sb(es, 'X2s', [128, 64]) for _ in range(NPAR)]
            WW = [P.sb(es, 'WW', [128, 192]) for _ in range(NPAR)]
            Dg = [P.sb(es, 'Dg', [128, 64]) for _ in range(NPAR)]
            HDg = [P.sb(es, 'HDg', [128, 64]) for _ in range(NPAR)]
            Us = [P.sb(es, 'Us', [128, 64]) for _ in range(NPAR)]
            Yb = [P.sb(es, 'Yb', [128, NCH, 64]) for _ in range(2)]
            Yc = [P.sb(es, 'Yc', [128, NCH, 64]) for _ in range(2)]
            st = [P.sb(es, 'st', [128, 4 * NCH]) for _ in range(2)]
            cfs = [P.sb(es, 'cfs', [128, NCH]) for _ in range(2)]
            yaTs = [P.sb(es, 'yaTs', [128, NB], BF16) for _ in range(2)]
            pj = [PSA.alloc(NB, 0), PSA.alloc(NB, 1)]
            pl = PSA.alloc(2 * NB, 2)
            pss = PSA.alloc(NB, 3)
            psY = [PSA.alloc(NB, 3)] * 2
            psT = [PSA.alloc(256, 4)] * 2
            psW = psT
            psD = [PSA.alloc(128, 4)] * 2
            psX = [PSA.alloc(64, 4)] * 2
            psU = [PSA.alloc(64, 4)] * 2
            psM = [PSA.alloc(320, 5)] * 2
            psE = [PSA.alloc(128, 5)] * 2
            psH = [PSA.alloc(64, 5)] * 2
            pgate = [PSA.alloc(NB, 6)] * 2
            pyt = [PSA.alloc(NB, 6)] * 2
            pcf = [PSA.alloc(NCH, 7)] * 2

            evq = [0]

            def evac_copy(out, in_, reads, writes):
                evq[0] += 1
                if evq[0] % 2:
                    P.op('act', lambda e: e.copy(out=out, in_=in_), reads=reads, writes=writes)
                else:
                    P.op('dve', lambda e: e.tensor_copy(out=out, in_=in_), reads=reads, writes=writes)

            for tb in range(nblk if stage != 'hgrn' else 0):
                own = tb >= ownb
                t0 = tb * NB
                xbt = xb[tb % 2]
                xbst = xbs_[tb % 2]
                xk = 'xb%d' % (tb % 2)
                for dc in range(8):
                    s = xstg[dc % 2]
                    k = 'xstg%d' % (dc % 2)
                    if tb == 0:
                        P.op('pool', lambda e, s=s: e.memset(s[:, 0:1], 0.0), writes=[k])
                        P.dma(s[:, 1:NB + 1], xT[dc * 128:(dc + 1) * 128, 0:NB], writes=[k])
                    else:
                        P.dma(s[:], xT[dc * 128:(dc + 1) * 128, t0 - 1:t0 + NB], writes=[k])
                    P.op('pool', lambda e, s=s, dc=dc, xbt=xbt: e.tensor_copy(out=xbt[:, dc, :], in_=s[:, 1:NB + 1]),
                         reads=[k], writes=[xk])
                    P.op('dve', lambda e, s=s, dc=dc, xbst=xbst: e.tensor_copy(out=xbst[:, dc, :], in_=s[:, 0:NB]),
                         reads=[k], writes=[xk])
                for pc in range(14):
                    pap, pk = pj[pc % 2]
                    for dc in range(8):
                        P.op('pe', lambda e, pap=pap, dc=dc, pc=pc, xbt=xbt: e.matmul(
                            pap, lhsT=wA[:, dc, pc * 128:(pc + 1) * 128], rhs=xbt[:, dc, :], start=(dc == 0), stop=False),
                            reads=['wA', xk], writes=[pk])
                    for dc in range(8):
                        P.op('pe', lambda e, pap=pap, dc=dc, pc=pc, xbst=xbst: e.matmul(
                            pap, lhsT=wB[:, dc, pc * 128:(pc + 1) * 128], rhs=xbst[:, dc, :], start=False, stop=(dc == 7)),
                            reads=['wB', xk], writes=[pk])
                    if pc < 12:
                        nm = ['Rr', 'Kr', 'Vt'][pc // 4]
                        hp = pc % 4
                        if nm == 'Rr' and not own:
                            pass
                        else:
                            evac_copy(FM[nm][hp][:], pap, [pk], ['%s%d' % (nm, hp)])
                    elif pc == 12:
                        P.op('act', lambda e, pap=pap: e.activation(out=LX[0:64, :], in_=pap[0:64, :], func=AF.Tanh),
                             reads=[pk], writes=['LX'])
                        P.op('dve', lambda e, pap=pap: e.tensor_copy(out=LX[64:128, :], in_=pap[64:128, :]),
                             reads=[pk], writes=['LX'])
                    else:
                        if own:
                            P.op('act', lambda e, pap=pap: e.activation(out=SGX[:], in_=pap, func=AF.Sigmoid),
                                 reads=[pk], writes=['SGX'])
                for hp in range(4):
                    Rr, Kr, Vt = FM['Rr'][hp], FM['Kr'][hp], FM['Vt'][hp]
                    At, Bt, Kt, Rt, rk, eG = (FM[n_][hp] for n_ in ['At', 'Bt', 'Kt', 'Rt', 'rk', 'eG'])
                    kn = lambda n_: '%s%d' % (n_, hp)
                    plap, plk = pl
                    P.op('pe', lambda e, hp=hp: e.matmul(plap[:, 0:NB], lhsT=LW[:, hp * 128:(hp + 1) * 128], rhs=LX[:],
                                                         start=True, stop=True), reads=['LW', 'LX'], writes=[plk])
                    P.op('pe', lambda e, hp=hp: e.matmul(plap[:, NB:2 * NB], lhsT=LW2[:, hp * 128:(hp + 1) * 128], rhs=LX[:],
                                                         start=True, stop=True), reads=['LW', 'LX'], writes=[plk])
                    sg, a, Gs, Gp, eGn, eGp, kk, sq, rn, kkn, t1, kp = (tmp[n_] for n_ in
                                                                         ['sg', 'a', 'Gs', 'Gp', 'eGn', 'eGp', 'kk', 'sq', 'rn', 'kkn', 't1', 'kp'])
                    P.op('act', lambda e, hp=hp: e.activation(out=sg[:], in_=plap[:, 0:NB], func=AF.Sigmoid, bias=pcol('w0', hp)),
                         reads=[plk, 'pv'], writes=['sg'])
                    P.op('act', lambda e, hp=hp: e.activation(out=a[:], in_=plap[:, NB:2 * NB], func=AF.Sigmoid, bias=pcol('a0', hp)),
                         reads=[plk, 'pv'], writes=['a'])
                    P.op('dve', lambda e: e.tensor_tensor_scan(out=Gs[:], data0=scanmask[:], data1=sg[:], initial=0.0,
                                                               op0=ALU.mult, op1=ALU.add), reads=['scanmask', 'sg'], writes=['Gs'])
                    P.op('pool', lambda e: e.tensor_tensor(out=Gp[:], in0=Gs[:], in1=sg[:], op=ALU.subtract),
                         reads=['Gs', 'sg'], writes=['Gp'])
                    P.op('act', lambda e, eG=eG: e.activation(out=eG[:], in_=Gs[:], func=AF.Exp, scale=-CDEC),
                         reads=['Gs'], writes=[kn('eG')])
                    P.op('act', lambda e: e.activation(out=eGn[:], in_=Gs[:], func=AF.Exp, scale=CDEC),
                         reads=['Gs'], writes=['eGn'])
                    P.op('act', lambda e: e.activation(out=eGp[:], in_=Gp[:], func=AF.Exp, scale=-CDEC),
                         reads=['Gp'], writes=['eGp'])
                    P.op('dve', lambda e, Kr=Kr, hp=hp: e.tensor_scalar(out=kk[:], in0=Kr[:], scalar1=pcol('k_k', hp), scalar2=None,
                                                                        op0=ALU.mult), reads=[kn('Kr'), 'pv'], writes=['kk'])
                    P.op('pool', lambda e: e.tensor_tensor(out=sq[:], in0=kk[:], in1=kk[:], op=ALU.mult), reads=['kk'], writes=['sq'])
                    psap, psk = pss
                    P.op('pe', lambda e: e.matmul(psap, lhsT=onesblk[:], rhs=sq[:], start=True, stop=True),
                         reads=['onesblk', 'sq'], writes=[psk])
                    P.op('act', lambda e: e.activation(out=rn[:], in_=psap, func=AF.Sqrt), reads=[psk], writes=['rn'])
                    P.op('dve', lambda e: e.tensor_scalar(out=rn[:], in0=rn[:], scalar1=1e-12, scalar2=None, op0=ALU.max),
                         reads=['rn'], writes=['rn'])
                    P.op('dve', lambda e: e.reciprocal(out=rn[:], in_=rn[:]), reads=['rn'], writes=['rn'])
                    P.op('dve', lambda e: e.tensor_tensor(out=kkn[:], in0=kk[:], in1=rn[:], op=ALU.mult),
                         reads=['kk', 'rn'], writes=['kkn'])
                    P.op('dve', lambda e, hp=hp: e.tensor_scalar(out=t1[:], in0=a[:], scalar1=-1.0, scalar2=pcol('k_a', hp),
                                                                 op0=ALU.add, op1=ALU.mult), reads=['a', 'pv'], writes=['t1'])
                    P.op('pool', lambda e: e.tensor_scalar(out=t1[:], in0=t1[:], scalar1=1.0, scalar2=None, op0=ALU.add),
                         reads=['t1'], writes=['t1'])
                    P.op('dve', lambda e, Kr=Kr: e.tensor_tensor(out=kp[:], in0=Kr[:], in1=t1[:], op=ALU.mult),
                         reads=[kn('Kr'), 't1'], writes=['kp'])
                    P.op('dve', lambda e, At=At: e.scalar_tensor_tensor(out=At[:], in0=kkn[:], scalar=-1.0, in1=eGp[:],
                                                                         op0=ALU.mult, op1=ALU.mult),
                         reads=['kkn', 'eGp'], writes=[kn('At')])
                    P.op('pool', lambda e: e.tensor_tensor(out=t1[:], in0=kkn[:], in1=a[:], op=ALU.mult),
                         reads=['kkn', 'a'], writes=['t1'])
                    P.op('pool', lambda e, Bt=Bt: e.tensor_tensor(out=Bt[:], in0=t1[:], in1=eGn[:], op=ALU.mult),
                         reads=['t1', 'eGn'], writes=[kn('Bt')])
                    P.op('dve', lambda e, Kt=Kt: e.tensor_tensor(out=Kt[:], in0=kp[:], in1=eGn[:], op=ALU.mult),
                         reads=['kp', 'eGn'], writes=[kn('Kt')])
                    if own:
                        P.op('pool', lambda e, Rt=Rt, Rr=Rr, eG=eG: e.tensor_tensor(out=Rt[:], in0=Rr[:], in1=eG[:], op=ALU.mult),
                             reads=[kn('Rr'), kn('eG')], writes=[kn('Rt')])
                        P.op('dve', lambda e, rk=rk, Rr=Rr, hp=hp: e.scalar_tensor_tensor(out=rk[:], in0=Rr[:], scalar=pcol('r_k', hp),
                                                                                           in1=kp[:], op0=ALU.mult, op1=ALU.mult),
                             reads=[kn('Rr'), 'kp', 'pv'], writes=[kn('rk')])
                    par = hp % NPAR
                    yb_i = hp % 2
                    for c in range(NCH):
                        gc = tb * NCH + c
                        cs = slice(c * 64, (c + 1) * 64)
                        Hc, Hn = HH[hp][gc % 2], HH[hp][(gc + 1) % 2]
                        hck, hnk = 'H%d_%d' % (hp, gc % 2), 'H%d_%d' % (hp, (gc + 1) % 2)
                        tmk, mmk = 'TM%d' % par, 'MM%d' % par
                        ptap, ptk = psT[par]
                        for i_, (X, xn) in enumerate([(Vt, 'Vt'), (At, 'At'), (Bt, 'Bt'), (Kt, 'Kt')]):
                            for h2 in range(2):
                                hs = slice(64 * h2, 64 * h2 + 64)
                                P.op('pe', lambda e, X=X, hs=hs, i_=i_, cs=cs, ptap=ptap: e.matmul(
                                    ptap[hs, i_ * 64:(i_ + 1) * 64], lhsT=X[hs, cs], rhs=ident[hs, hs], start=True, stop=True),
                                    reads=[kn(xn), 'ident'], writes=[ptk])
                        evac_copy(TM[par][:].rearrange("p a b -> p (a b)"), ptap, [ptk], [tmk])
                        pmap, pmk = psM[par]
                        prs = [(Bt, At, 'Bt', 'At'), (At, Bt, 'At', 'Bt'), (Kt, At, 'Kt', 'At')]
                        if own:
                            prs += [(Bt, Rt, 'Bt', 'Rt'), (Kt, Rt, 'Kt', 'Rt')]
                        for i_, (L_, R_, ln, rn_) in enumerate(prs):
                            for h2 in range(2):
                                hs = slice(64 * h2, 64 * h2 + 64)
                                P.op('pe', lambda e, L_=L_, R_=R_, hs=hs, i_=i_, cs=cs, pmap=pmap: e.matmul(
                                    pmap[hs, i_ * 64:(i_ + 1) * 64], lhsT=L_[hs, cs], rhs=R_[hs, cs], start=True, stop=True),
                                    reads=[kn(ln), kn(rn_)], writes=[pmk])
                        ncol = 64 * len(prs)
                        P.op('dve', lambda e, par=par, pmap=pmap, ncol=ncol: e.tensor_tensor(
                            out=MM[par][:, 0:ncol], in0=pmap[:, 0:ncol], in1=mask320[:, 0:ncol], op=ALU.mult),
                            reads=[pmk, 'mask320'], writes=[mmk])
                        ttk = ['TT%d_%d' % (par, i_) for i_ in range(2)]
                        ppk = ['PP%d_%d' % (par, i_) for i_ in range(2)]
                        P.op('pool', lambda e, par=par: e.tensor_tensor(out=TT[par][0][:], in0=MM[par][:, 0:128], in1=iit[:], op=ALU.add),
                             reads=[mmk, 'ii'], writes=[ttk[0]])
                        Pcur, Pk = MM[par][:, 0:128], mmk
                        tcur = 0
                        for lvl in range(5):
                            pdap, pdk = psD[par]
                            peap, pek = psE[par]
                            for h2 in range(2):
                                hs = slice(64 * h2, 64 * h2 + 64)
                                P.op('pe', lambda e, Pcur=Pcur, hs=hs, pdap=pdap: e.matmul(
                                    pdap[hs, 0:64], lhsT=Pcur[hs, 64:128], rhs=Pcur[hs, 0:64], start=True, stop=True),
                                    reads=[Pk], writes=[pdk])
                                P.op('pe', lambda e, Pcur=Pcur, hs=hs, pdap=pdap: e.matmul(
                                    pdap[hs, 64:128], lhsT=Pcur[hs, 0:64], rhs=Pcur[hs, 64:128], start=True, stop=True),
                                    reads=[Pk], writes=[pdk])
                            pn = PP[par][lvl % 2]
                            pnk = ppk[lvl % 2]
                            evac_copy(pn[:], pdap, [pdk], [pnk])
                            Pcur, Pk = pn[:], pnk
                            Tc = TT[par][tcur]
                            Tn = TT[par][1 - tcur]
                            for h2 in range(2):
                                hs = slice(64 * h2, 64 * h2 + 64)
                                P.op('pe', lambda e, Tc=Tc, Pcur=Pcur, hs=hs, peap=peap: e.matmul(
                                    peap[hs, 0:64], lhsT=Tc[hs, 64:128], rhs=Pcur[hs, 0:64], start=True, stop=True),
                                    reads=[ttk[tcur], Pk], writes=[pek])
                                P.op('pe', lambda e, Tc=Tc, Pcur=Pcur, hs=hs, peap=peap: e.matmul(
                                    peap[hs, 64:128], lhsT=Pcur[hs, 0:64], rhs=Tc[hs, 64:128], start=True, stop=True),
                                    reads=[ttk[tcur], Pk], writes=[pek])
                            P.op('dve', lambda e, Tc=Tc, Tn=Tn, peap=peap: e.tensor_tensor(out=Tn[:], in0=peap, in1=Tc[:], op=ALU.add),
                                 reads=[pek, ttk[tcur]], writes=[ttk[1 - tcur]])
                            tcur = 1 - tcur
                        Tt = TT[par][tcur]
                        tk = ttk[tcur]
                        pxap, pxk = psX[par]
                        for h2 in range(2):
                            hs = slice(64 * h2, 64 * h2 + 64)
                            P.op('pe', lambda e, hs=hs, par=par, pxap=pxap: e.matmul(
                                pxap[hs, :], lhsT=MM[par][hs, 128:192], rhs=TM[par][hs, 0, :], start=True, stop=True),
                                reads=[mmk, tmk], writes=[pxk])
                        evac_copy(X2s[par][:], pxap, [pxk], ['X2s%d' % par])
                        pwap, pwk = psW[par]
                        for h2 in range(2):
                            hs = slice(64 * h2, 64 * h2 + 64)
                            P.op('pe', lambda e, hs=hs, par=par, Tt=Tt, pwap=pwap: e.matmul(
                                pwap[hs, 0:64], lhsT=Tt[hs, 0:64], rhs=X2s[par][hs, :], start=True, stop=True),
                                reads=[tk, 'X2s%d' % par], writes=[pwk])
                            P.op('pe', lambda e, hs=hs, par=par, Tt=Tt, pwap=pwap: e.matmul(
                                pwap[hs, 64:128], lhsT=TM[par][hs, 1, :], rhs=Tt[hs, 0:64], start=True, stop=True),
                                reads=[tk, tmk], writes=[pwk])
                            P.op('pe', lambda e, hs=hs, par=par, pwap=pwap: e.matmul(
                                pwap[hs, 128:192], lhsT=TM[par][hs, 3, :], rhs=TM[par][hs, 0, :], start=True, stop=True),
                                reads=[tmk], writes=[pwk])
                        wwk = 'WW%d' % par
                        evac_copy(WW[par][:], pwap[:, 0:192], [pwk], [wwk])
                        gam = eG[:, c * 64 + 63:c * 64 + 64]
                        P.op('pool', lambda e, par=par, gam=gam: e.tensor_scalar(out=Dg[par][:], in0=WW[par][:, 128:192], scalar1=gam,
                                                                                 scalar2=None, op0=ALU.mult),
                             reads=[wwk, kn('eG')], writes=['Dg%d' % par])
                        P.op('dve', lambda e, par=par, gam=gam, Hc=Hc: e.scalar_tensor_tensor(
                            out=HDg[par][:], in0=Hc[:], scalar=gam, in1=Dg[par][:], op0=ALU.mult, op1=ALU.add),
                            reads=[hck, kn('eG'), 'Dg%d' % par], writes=['HDg%d' % par])
                        puap, puk = psU[par]
                        for h2 in range(2):
                            hs = slice(64 * h2, 64 * h2 + 64)
                            P.op('pe', lambda e, hs=hs, par=par, Hc=Hc, puap=puap: e.matmul(
                                puap[hs, :], lhsT=WW[par][hs, 64:128], rhs=Hc[hs, :], start=True, stop=True),
                                reads=[wwk, hck], writes=[puk])
                        P.op('dve', lambda e, par=par, puap=puap: e.tensor_tensor(out=Us[par][:], in0=puap, in1=WW[par][:, 0:64], op=ALU.add),
                             reads=[puk, wwk], writes=['Us%d' % par])
                        if own:
                            pyap, pyk = psY[yb_i]
                            for h2 in range(2):
                                hs = slice(64 * h2, 64 * h2 + 64)
                                P.op('pe', lambda e, hs=hs, Rt=Rt, cs=cs, Hc=Hc, pyap=pyap: e.matmul(
                                    pyap[hs, cs], lhsT=Rt[hs, cs], rhs=Hc[hs, :], start=True, stop=False),
                                    reads=[kn('Rt'), hck], writes=[pyk])
                                P.op('pe', lambda e, hs=hs, par=par, cs=cs, pyap=pyap: e.matmul(
                                    pyap[hs, cs], lhsT=MM[par][hs, 192:256], rhs=Us[par][hs, :], start=False, stop=False),
                                    reads=[mmk, 'Us%d' % par], writes=[pyk])
                                P.op('pe', lambda e, hs=hs, par=par, cs=cs, pyap=pyap: e.matmul(
                                    pyap[hs, cs], lhsT=MM[par][hs, 256:320], rhs=TM[par][hs, 0, :], start=False, stop=True),
                                    reads=[mmk, tmk], writes=[pyk])
                            pcap, pck = pcf[yb_i]
                            pgap, pgk = pgate[yb_i]
                            for h2 in range(2):
                                hs = slice(64 * h2, 64 * h2 + 64)
                                P.op('pe', lambda e, hs=hs, rk=rk, cs=cs, c=c, pcap=pcap: e.matmul(
                                    pcap[hs, c:c + 1], lhsT=rk[hs, cs], rhs=onesblk[hs, 64 * (hs.start // 64):64 * (hs.start // 64) + 1],
                                    start=True, stop=True), reads=[kn('rk'), 'onesblk'], writes=[pck])
                                h = 2 * hp + h2
                                P.op('pe', lambda e, hs=hs, cs=cs, h=h, pgap=pgap: e.matmul(
                                    pgap[hs, cs], lhsT=SGX[:, cs], rhs=GU[:, h * 64:(h + 1) * 64], start=True, stop=True),
                                    reads=['SGX', 'GU'], writes=[pgk])
                            P.op('pool', lambda e, par=par, c=c, yb_i=yb_i: e.tensor_copy(out=Yc[yb_i][:, c, :], in_=TM[par][:, 0, :]),
                                 reads=[tmk], writes=['Yc%d' % yb_i])
                        phap, phk = psH[par]
                        for h2 in range(2):
                            hs = slice(64 * h2, 64 * h2 + 64)
                            P.op('pe', lambda e, hs=hs, par=par, phap=phap: e.matmul(
                                phap[hs, :], lhsT=TM[par][hs, 2, :], rhs=Us[par][hs, :], start=True, stop=True),
                                reads=[tmk, 'Us%d' % par], writes=[phk])
                        P.op('dve', lambda e, par=par, gam=gam, Hn=Hn, phap=phap: e.scalar_tensor_tensor(
                            out=Hn[:], in0=phap, scalar=gam, in1=HDg[par][:], op0=ALU.mult, op1=ALU.add),
                            reads=[phk, kn('eG'), 'HDg%d' % par], writes=[hnk])
                    if own:
                        pyap, pyk = psY[yb_i]
                        pcap, pck = pcf[yb_i]
                        pgap, pgk = pgate[yb_i]
                        Y = Yb[yb_i]
                        yk = 'Yb%d' % yb_i
                        V3 = Yc[yb_i]
                        vk = 'Yc%d' % yb_i
                        S = st[yb_i]
                        sk = 'st%d' % yb_i
                        y3 = lambda ap: ap.rearrange("p (c v) -> p c v", v=64)
                        bc = lambda ap: ap.unsqueeze(2).to_broadcast([128, NCH, 64])
                        evac_copy(Y[:].rearrange("p c v -> p (c v)"), pyap, [pyk], [yk])
                        P.op('dve', lambda e, Y=Y, S=S: e.tensor_reduce(out=S[:, 0:NCH], in_=Y[:], axis=AX.X, op=ALU.add),
                             reads=[yk], writes=[sk])
                        P.op('dve', lambda e, S=S: e.tensor_scalar(out=S[:, 0:NCH], in0=S[:, 0:NCH], scalar1=1.0 / 64, scalar2=None, op0=ALU.mult),
                             reads=[sk], writes=[sk])
                        P.op('dve', lambda e, Y=Y, S=S: e.tensor_tensor(out=Y[:], in0=Y[:], in1=bc(S[:, 0:NCH]), op=ALU.subtract),
                             reads=[yk, sk], writes=[yk])
                        sqb = tmp['sq']
                        P.op('pool', lambda e, Y=Y: e.tensor_tensor(out=y3(sqb[:]), in0=Y[:], in1=Y[:], op=ALU.mult),
                             reads=[yk], writes=['sq'])
                        P.op('dve', lambda e, S=S: e.tensor_reduce(out=S[:, NCH:2 * NCH], in_=y3(sqb[:]), axis=AX.X, op=ALU.add),
                             reads=['sq'], writes=[sk])
                        P.op('dve', lambda e, S=S: e.tensor_scalar(out=S[:, NCH:2 * NCH], in0=S[:, NCH:2 * NCH], scalar1=1.0 / 64, scalar2=64e-5,
                                                                   op0=ALU.mult, op1=ALU.add), reads=[sk], writes=[sk])
                        P.op('act', lambda e, S=S: e.activation(out=S[:, NCH:2 * NCH], in_=S[:, NCH:2 * NCH], func=AF.Sqrt),
                             reads=[sk], writes=[sk])
                        P.op('dve', lambda e, S=S: e.reciprocal(out=S[:, NCH:2 * NCH], in_=S[:, NCH:2 * NCH]), reads=[sk], writes=[sk])
                        P.op('dve', lambda e, Y=Y, S=S: e.tensor_tensor(out=Y[:], in0=Y[:], in1=bc(S[:, NCH:2 * NCH]), op=ALU.mult),
                             reads=[yk, sk], writes=[yk])
                        gw = gnw[:, hp, :].unsqueeze(1).to_broadcast([128, NCH, 64])
                        gb = gnb[:, hp, :].unsqueeze(1).to_broadcast([128, NCH, 64])
                        P.op('pool', lambda e, Y=Y, gw=gw: e.tensor_tensor(out=Y[:], in0=Y[:], in1=gw, op=ALU.mult),
                             reads=[yk, 'gnw'], writes=[yk])
                        P.op('pool', lambda e, Y=Y, gb=gb: e.tensor_tensor(out=Y[:], in0=Y[:], in1=gb, op=ALU.add),
                             reads=[yk, 'gnb'], writes=[yk])
                        cf = cfs[yb_i]
                        ck = 'cfs%d' % yb_i
                        evac_copy(cf[:], pcap, [pck], [ck])
                        P.op('dve', lambda e, V3=V3, cf=cf: e.tensor_tensor(out=V3[:], in0=V3[:], in1=bc(cf[:]), op=ALU.mult),
                             reads=[vk, ck], writes=[vk])
                        P.op('pool', lambda e, Y=Y, V3=V3: e.tensor_tensor(out=Y[:], in0=Y[:], in1=V3[:], op=ALU.add),
                             reads=[yk, vk], writes=[yk])
                        P.op('dve', lambda e, Y=Y, pgap=pgap: e.tensor_tensor(out=Y[:].rearrange("p c v -> p (c v)"),
                                                                              in0=Y[:].rearrange("p c v -> p (c v)"), in1=pgap, op=ALU.mult),
                             reads=[yk, pgk], writes=[yk])
                        ob = tb - ownb
                        if stage == 'rwkv':
                            for c in range(NCH):
                                P.dma(ya_dbg[ob * NCH + c, hp], Y[:, c, :], reads=[yk])
                        ptap2, ptk2 = pyt[yb_i]
                        for c in range(NCH):
                            for h2 in range(2):
                                hs = slice(64 * h2, 64 * h2 + 64)
                                P.op('pe', lambda e, Y=Y, hs=hs, c=c, ptap2=ptap2: e.matmul(
                                    ptap2[hs, c * 64:(c + 1) * 64], lhsT=Y[hs, c, :], rhs=ident[hs, hs], start=True, stop=True),
                                    reads=[yk, 'ident'], writes=[ptk2])
                        yT = yaTs[yb_i]
                        ytk = 'yaTs%d' % yb_i
                        evac_copy(yT[:], ptap2, [ptk2], [ytk])
                        P.dma(yaT_d[hp * 128:(hp + 1) * 128, ob * NB:(ob + 1) * NB], yT[:], reads=[ytk], writes=['dram_yaT_%d_%d' % (hp, ob)])
            P.flush()
        if stage in ('hgrn', 'full', 'fullA'):
          P.barrier()
          with ExitStack() as es:
            PSA.reset()
            scanmask_h = P.sb(es, 'scanmask_h', [128, NB])
            iu2 = P.sb(es, 'iu2', [128, 64])
            P.dma(scanmask_h[:], scanmask_d, writes=['scanmask_h'])
            P.dma(iu2[:], iu2_d, writes=['iu2'])
            wH = P.sb(es, 'wH', [128, 8, 2048], BF16)
            NWB = P.sb(es, 'NWB', [128, 512])
            o_, n_ = ROWS['hg_nw']
            P.dma(NWB[:], rows_d[:, o_:o_ + n_].partition_broadcast(128), writes=['NWB'])
            lbt = P.sb(es, 'lbt', [128, 12])
            P.op('dve', lambda e: e.tensor_tensor(out=lbt[:, 0:4], in0=pvt[:, PV['lb0']:PV['lb0'] + 4],
                                                  in1=pvt[:, PV['lb1']:PV['lb1'] + 4], op=ALU.subtract),
                 reads=['pv'], writes=['lbt'])
            P.op('act', lambda e: e.activation(out=lbt[:, 0:4], in_=lbt[:, 0:4], func=AF.Sigmoid), reads=['lbt'], writes=['lbt'])
            P.op('dve', lambda e: e.tensor_scalar(out=lbt[:, 4:8], in0=lbt[:, 0:4], scalar1=-1.0, scalar2=1.0, op0=ALU.mult, op1=ALU.add),
                 reads=['lbt'], writes=['lbt'])
            P.op('dve', lambda e: e.tensor_scalar(out=lbt[:, 8:12], in0=lbt[:, 4:8], scalar1=-1.0, scalar2=None, op0=ALU.mult),
                 reads=['lbt'], writes=['lbt'])
            with ExitStack() as es1:
                stg = [P.sb(es1, 'whstg', [128, 2048]) for _ in range(2)]
                for dc in range(8):
                    s = stg[dc % 2]
                    k = 'whstg%d' % (dc % 2)
                    P.dma(s[:], w_in[dc * 128:(dc + 1) * 128, C_Q:C_GATE], writes=[k])
                    P.op('dve' if dc % 2 else 'pool', lambda e, s=s, dc=dc: e.tensor_copy(out=wH[:, dc, :], in_=s[:]),
                         reads=[k], writes=['wH'])
                P.flush()
            P.barrier()
            xstg = [P.sb(es, 'hxstg', [128, NB]) for _ in range(2)]
            xb = [P.sb(es, 'hxb', [128, 8, NB], BF16) for _ in range(2)]
            NSET = 2
            fm = [{nm: P.sb(es, 'h' + nm, [128, NB]) for nm in ['q', 'sf', 'lf', 'k', 'b', 'eb', 'enb', 'qt', 'kt']} for _ in range(NSET)]
            Vtok = [[P.sb(es, 'Vtok', [128, 512]) for _ in range(NB // 128)] for _ in range(2)]
            SGo = [P.sb(es, 'SGo', [128, 512]) for _ in range(NB // 128)]
            KTs = [P.sb(es, 'KTs', [128, 128]) for _ in range(2)]
            ATs = [P.sb(es, 'ATs', [128, 64]) for _ in range(2)]
            Dgs = [[P.sb(es, 'hDg', [128, 128]) for _ in range(2)] for _ in range(2)]
            SS = [[P.sb(es, 'hS', [128, 128]) for _ in range(2)] for _ in range(4)]
            for h in range(4):
                P.op('pool', lambda e, h=h: e.memset(SS[h][0][:], 0.0), writes=['hS%d_0' % h])
            Ot = P.sb(es, 'Ot', [128, 4, 128])
            Osq = P.sb(es, 'Osq', [128, 4, 128])
            ost = P.sb(es, 'ost', [128, 8])
            ybTs = P.sb(es, 'ybTs', [128, 4, 128], BF16)
            pj = [PSA.alloc(NB, 0), PSA.alloc(NB, 1)]
            pv_ = PSA.alloc(512, 2)
            pog = PSA.alloc(512, 3)
            pK = PSA.alloc(128, 4)
            pA = PSA.alloc(64, 4)
            pD = PSA.alloc(256, 5)
            pO = [PSA.alloc(512, 6), PSA.alloc(512, 7)]
            pT = pv_
            evq2 = [0]

            def evac2(out, in_, reads, writes):
                evq2[0] += 1
                if evq2[0] % 2:
                    P.op('act', lambda e: e.copy(out=out, in_=in_), reads=reads, writes=writes)
                else:
                    P.op('dve', lambda e: e.tensor_copy(out=out, in_=in_), reads=reads, writes=writes)

            for tb in range(nblk):
                own = tb >= ownb
                ob = tb - ownb
                t0 = tb * NB
                xbt = xb[tb % 2]
                xk = 'hxb%d' % (tb % 2)
                for dc in range(8):
                    s = xstg[dc % 2]
                    k = 'hxstg%d' % (dc % 2)
                    P.dma(s[:], xT[dc * 128:(dc + 1) * 128, t0:t0 + NB], writes=[k])
                    P.op('pool' if dc % 2 else 'dve', lambda e, s=s, dc=dc, xbt=xbt: e.tensor_copy(out=xbt[:, dc, :], in_=s[:]),
                         reads=[k], writes=[xk])
                vt = Vtok[tb % 2]
                for tt in range(NB // 128):
                    vk = 'Vtok%d_%d' % (tb % 2, tt)
                    pap, pk = pv_
                    for dc in range(8):
                        P.op('pe', lambda e, pap=pap, dc=dc, tt=tt, xbt=xbt: e.matmul(
                            pap, lhsT=xbt[:, dc, tt * 128:(tt + 1) * 128], rhs=wH[:, dc, 1024:1536], start=(dc == 0), stop=(dc == 7)),
                            reads=['wH', xk], writes=[pk])
                    evac2(vt[tt][:], pap, [pk], [vk])
                    if own:
                        pap, pk = pog
                        for dc in range(8):
                            P.op('pe', lambda e, pap=pap, dc=dc, tt=tt, xbt=xbt: e.matmul(
                                pap, lhsT=xbt[:, dc, tt * 128:(tt + 1) * 128], rhs=wH[:, dc, 1536:2048], start=(dc == 0), stop=(dc == 7)),
                                reads=['wH', xk], writes=[pk])
                        P.op('act', lambda e, pap=pap, tt=tt: e.activation(out=SGo[tt][:], in_=pap, func=AF.Sigmoid),
                             reads=[pk], writes=['SGo%d' % tt])
                for h in range(4):
                    F = fm[h % NSET]
                    fk = lambda n_: 'h%s%d' % (n_, h % NSET)
                    pap, pk = pj[0]
                    if own:
                        for dc in range(8):
                            P.op('pe', lambda e, pap=pap, dc=dc, h=h, xbt=xbt: e.matmul(
                                pap, lhsT=wH[:, dc, h * 128:(h + 1) * 128], rhs=xbt[:, dc, :], start=(dc == 0), stop=(dc == 7)),
                                reads=['wH', xk], writes=[pk])
                        P.op('act', lambda e, pap=pap, F=F: e.activation(out=F['q'][:], in_=pap, func=AF.Silu), reads=[pk], writes=[fk('q')])
                    pap, pk = pj[1]
                    for dc in range(8):
                        P.op('pe', lambda e, pap=pap, dc=dc, h=h, xbt=xbt: e.matmul(
                            pap, lhsT=wH[:, dc, 512 + h * 128:512 + (h + 1) * 128], rhs=xbt[:, dc, :], start=(dc == 0), stop=(dc == 7)),
                            reads=['wH', xk], writes=[pk])
                    P.op('act', lambda e, pap=pap, F=F: e.activation(out=F['sf'][:], in_=pap, func=AF.Sigmoid), reads=[pk], writes=[fk('sf')])
                    P.op('act', lambda e, F=F, h=h: e.activation(out=F['lf'][:], in_=F['sf'][:], func=AF.Ln, bias=lbt[:, h:h + 1],
                                                                 scale=lbt[:, 4 + h:5 + h]), reads=[fk('sf'), 'lbt'], writes=[fk('lf')])
                    P.op('dve', lambda e, F=F, h=h: e.tensor_scalar(out=F['k'][:], in0=F['sf'][:], scalar1=lbt[:, 8 + h:9 + h],
                                                                    scalar2=lbt[:, 4 + h:5 + h], op0=ALU.mult, op1=ALU.add),
                         reads=[fk('sf'), 'lbt'], writes=[fk('k')])
                    P.op('dve', lambda e, F=F: e.tensor_tensor_scan(out=F['b'][:], data0=scanmask_h[:], data1=F['lf'][:], initial=0.0,
                                                                    op0=ALU.mult, op1=ALU.add), reads=['scanmask_h', fk('lf')], writes=[fk('b')])
                    P.op('act', lambda e, F=F: e.activation(out=F['eb'][:], in_=F['b'][:], func=AF.Exp), reads=[fk('b')], writes=[fk('eb')])
                    P.op('act', lambda e, F=F: e.activation(out=F['enb'][:], in_=F['b'][:], func=AF.Exp, scale=-1.0),
                         reads=[fk('b')], writes=[fk('enb')])
                    P.op('pool', lambda e, F=F: e.tensor_tensor(out=F['kt'][:], in0=F['k'][:], in1=F['enb'][:], op=ALU.mult),
                         reads=[fk('k'), fk('enb')], writes=[fk('kt')])
                    if own:
                        P.op('pool', lambda e, F=F: e.tensor_tensor(out=F['qt'][:], in0=F['q'][:], in1=F['eb'][:], op=ALU.mult),
                             reads=[fk('q'), fk('eb')], writes=[fk('qt')])
                    for cp in range(NB // 128):
                        vk = 'Vtok%d_%d' % (tb % 2, cp)
                        for c2 in range(2):
                            c = 2 * cp + c2
                            gc = tb * NCH + c
                            cs = slice(c * 64, (c + 1) * 64)
                            ps_ = slice(64 * c2, 64 * c2 + 64)
                            Sc, Sn = SS[h][gc % 2], SS[h][(gc + 1) % 2]
                            sck, snk = 'hS%d_%d' % (h, gc % 2), 'hS%d_%d' % (h, (gc + 1) % 2)
                            kts, ktk = KTs[c2], 'KTs%d' % c2
                            ats, atk = ATs[c2], 'ATs%d' % c2
                            dgs, dgk = Dgs[h % 2][c2], 'hDg%d_%d' % (h % 2, c2)
                            pkap, pkk = pK
                            P.op('pe', lambda e, F=F, cs=cs, ps_=ps_, pkap=pkap: e.matmul(
                                pkap[ps_, :], lhsT=F['kt'][:, cs], rhs=ident[:], start=True, stop=True),
                                reads=[fk('kt'), 'ident'], writes=[pkk])
                            evac2(kts[ps_, :], pkap[ps_, :], [pkk], [ktk])
                            if own:
                                paap, pak = pA
                                P.op('pe', lambda e, F=F, cs=cs, ps_=ps_, paap=paap: e.matmul(
                                    paap[ps_, :], lhsT=F['kt'][:, cs], rhs=F['qt'][:, cs], start=True, stop=True),
                                    reads=[fk('kt'), fk('qt')], writes=[pak])
                                P.op('dve', lambda e, ats=ats, ps_=ps_, paap=paap: e.tensor_tensor(
                                    out=ats[ps_, :], in0=paap[ps_, :], in1=iu2[ps_, :], op=ALU.mult),
                                    reads=[pak, 'iu2'], writes=[atk])
                            pdap, pdk = pD
                            P.op('pe', lambda e, kts=kts, ps_=ps_, cp=cp, h=h, c2=c2, pdap=pdap, vt=vt: e.matmul(
                                pdap[:, c2 * 128:(c2 + 1) * 128], lhsT=kts[ps_, :], rhs=vt[cp][ps_, h * 128:(h + 1) * 128],
                                start=True, stop=True), reads=[ktk, vk], writes=[pdk])
                            gl = F['eb'][:, c * 64 + 63:c * 64 + 64]
                            P.op('dve', lambda e, dgs=dgs, pdap=pdap, c2=c2, gl=gl: e.tensor_scalar(
                                out=dgs[:], in0=pdap[:, c2 * 128:(c2 + 1) * 128], scalar1=gl, scalar2=None, op0=ALU.mult),
                                reads=[pdk, fk('eb')], writes=[dgk])
                            if own:
                                poap, pok = pO[cp]
                                P.op('pe', lambda e, ats=ats, ps_=ps_, cp=cp, h=h, poap=poap, vt=vt: e.matmul(
                                    poap[ps_, h * 128:(h + 1) * 128], lhsT=ats[ps_, :], rhs=vt[cp][ps_, h * 128:(h + 1) * 128],
                                    start=True, stop=False), reads=[atk, vk], writes=[pok])
                                P.op('pe', lambda e, F=F, cs=cs, ps_=ps_, h=h, Sc=Sc, poap=poap: e.matmul(
                                    poap[ps_, h * 128:(h + 1) * 128], lhsT=F['qt'][:, cs], rhs=Sc[:], start=False, stop=True),
                                    reads=[fk('qt'), sck], writes=[pok])
                            P.op('dve', lambda e, Sc=Sc, Sn=Sn, gl=gl, dgs=dgs: e.scalar_tensor_tensor(
                                out=Sn[:], in0=Sc[:], scalar=gl, in1=dgs[:], op0=ALU.mult, op1=ALU.add),
                                reads=[sck, fk('eb'), dgk], writes=[snk])
                if own:
                    for cp in range(NB // 128):
                        poap, pok = pO[cp]
                        evac2(Ot[:].rearrange("p h v -> p (h v)"), poap, [pok], ['Ot'])
                        P.op('pool', lambda e: e.tensor_tensor(out=Osq[:], in0=Ot[:], in1=Ot[:], op=ALU.mult), reads=['Ot'], writes=['Osq'])
                        P.op('dve', lambda e: e.tensor_reduce(out=ost[:, 0:4], in_=Osq[:], axis=AX.X, op=ALU.add), reads=['Osq'], writes=['ost'])
                        P.op('dve', lambda e: e.tensor_scalar(out=ost[:, 0:4], in0=ost[:, 0:4], scalar1=1.0 / 128, scalar2=1e-6,
                                                              op0=ALU.mult, op1=ALU.add), reads=['ost'], writes=['ost'])
                        P.op('act', lambda e: e.activation(out=ost[:, 0:4], in_=ost[:, 0:4], func=AF.Sqrt), reads=['ost'], writes=['ost'])
                        P.op('dve', lambda e: e.reciprocal(out=ost[:, 0:4], in_=ost[:, 0:4]), reads=['ost'], writes=['ost'])
                        P.op('dve', lambda e: e.tensor_tensor(out=Ot[:], in0=Ot[:], in1=ost[:, 0:4].unsqueeze(2).to_broadcast([128, 4, 128]),
                                                              op=ALU.mult), reads=['Ot', 'ost'], writes=['Ot'])
                        P.op('pool', lambda e: e.tensor_tensor(out=Ot[:].rearrange("p h v -> p (h v)"), in0=Ot[:].rearrange("p h v -> p (h v)"),
                                                               in1=NWB[:], op=ALU.mult), reads=['Ot', 'NWB'], writes=['Ot'])
                        P.op('dve', lambda e, cp=cp: e.tensor_tensor(out=Ot[:].rearrange("p h v -> p (h v)"), in0=Ot[:].rearrange("p h v -> p (h v)"),
                                                                     in1=SGo[cp][:], op=ALU.mult), reads=['Ot', 'SGo%d' % cp], writes=['Ot'])
                        tok0 = ob * NB + cp * 128
                        if stage == 'hgrn':
                            P.dma(yb_dbg[tok0:tok0 + 128, :], Ot[:].rearrange("p h v -> p (h v)"), reads=['Ot'])
                        ptap, ptk = pT
                        for h in range(4):
                            P.op('pe', lambda e, h=h, ptap=ptap: e.matmul(ptap[:, h * 128:(h + 1) * 128], lhsT=Ot[:, h, :], rhs=ident[:],
                                                                         start=True, stop=True), reads=['Ot', 'ident'], writes=[ptk])
                        evac2(ybTs[:].rearrange("p h t -> p (h t)"), ptap, [ptk], ['ybTs'])
                        P.dma(ybT_d.rearrange("(h p) t -> p h t", p=128)[:, :, tok0:tok0 + 128], ybTs[:], reads=['ybTs'],
                              writes=['dram_ybT_%d' % (tok0 // 128)])
            P.flush()
        if stage == 'fullA':
            with ExitStack() as es:
                P.barrier()
                bt = P.sb(es, 'bt', [128, 4, TOWN], BF16)
                for src, dst, kp_ in [(yaT_d, yaT_o, 'a'), (ybT_d, ybT_o, 'b')]:
                    rk_ = (['dram_yaT_%d_%d' % (hp_, o__) for hp_ in range(4) for o__ in range(nblk - ownb)] if kp_ == 'a'
                           else ['dram_ybT_%d' % j_ for j_ in range(TOWN // 128)])
                    P.dma(bt[:], src.rearrange("(c p) t -> p c t", p=128), reads=rk_, writes=['bt'])
                    P.dma(dst.rearrange("(c p) t -> p c t", p=128), bt[:], reads=['bt'], writes=['dram_o' + kp_])
                P.flush()
        if stage == 'full':
          NT_H = 1024
          own_off = T - TOWN
          with ExitStack() as esB:
            P.barrier()
            PSA.reset()
            ACC = P.sb(esB, 'ACC', [128, NT_H // 128, D])
            x1T = P.sb(esB, 'x1T', [128, 8, NT_H], BF16)
            WtAll = P.sb(esB, 'WtAll', [128, NT_H // 128, 32])
            LNB = P.sb(esB, 'LNB', [128, 2, D])

            def load_ln(names):
                for i_, nm in enumerate(names):
                    o_, n_ = ROWS[nm]
                    P.dma(LNB[:, i_, :], rows_d[:, o_:o_ + n_].partition_broadcast(128), writes=['LNB'])
            RBB = P.sb(esB, 'RBB', [128, 36])
            o_, n_ = ROWS['rb']
            P.dma(RBB[:], rows_d[:, o_:o_ + n_].partition_broadcast(128), writes=['RBB'])
            evq3 = [0]

            def evac3(out, in_, reads, writes):
                evq3[0] += 1
                if evq3[0] % 2:
                    P.op('act', lambda e: e.copy(out=out, in_=in_), reads=reads, writes=writes)
                else:
                    P.op('dve', lambda e: e.tensor_copy(out=out, in_=in_), reads=reads, writes=writes)

            def load_cast(es_, dst, dkey, src_rows, ncols, nchunk, col0=0, cast_engs=('dve', 'pool')):
                stg_ = [P.sb(es_, 'lcs', [128, 1024]) for _ in range(2)]
                n_ = 0
                for c in range(nchunk):
                    for c1 in range(0, ncols, 1024):
                        s_ = stg_[n_ % 2]
                        k_ = 'lcs%d' % (n_ % 2)
                        P.dma(s_[:], src_rows(c)[:, col0 + c1:col0 + c1 + 1024], writes=[k_])
                        P.op(cast_engs[n_ % len(cast_engs)], lambda e, s_=s_, c=c, c1=c1: e.tensor_copy(out=dst[:, c, c1:c1 + 1024], in_=s_[:]),
                             reads=[k_], writes=[dkey])
                        n_ += 1

            def layer_norm_tile(Zt, zk, gi, stt, sq_t):
                P.op('dve', lambda e: e.tensor_reduce(out=stt[:, 0:1], in_=Zt[:], axis=AX.X, op=ALU.add), reads=[zk], writes=['stt'])
                P.op('dve', lambda e: e.tensor_scalar(out=stt[:, 0:1], in0=stt[:, 0:1], scalar1=1.0 / D, scalar2=None, op0=ALU.mult),
                     reads=['stt'], writes=['stt'])
                P.op('dve', lambda e: e.tensor_scalar(out=Zt[:], in0=Zt[:], scalar1=stt[:, 0:1], scalar2=None, op0=ALU.subtract),
                     reads=[zk, 'stt'], writes=[zk])
                P.op('pool', lambda e: e.tensor_tensor(out=sq_t[:], in0=Zt[:], in1=Zt[:], op=ALU.mult), reads=[zk], writes=['sq_t'])
                P.op('dve', lambda e: e.tensor_reduce(out=stt[:, 1:2], in_=sq_t[:], axis=AX.X, op=ALU.add), reads=['sq_t'], writes=['stt'])
                P.op('dve', lambda e: e.tensor_scalar(out=stt[:, 1:2], in0=stt[:, 1:2], scalar1=1.0 / D, scalar2=1e-5, op0=ALU.mult, op1=ALU.add),
                     reads=['stt'], writes=['stt'])
                P.op('act', lambda e: e.activation(out=stt[:, 1:2], in_=stt[:, 1:2], func=AF.Sqrt), reads=['stt'], writes=['stt'])
                P.op('dve', lambda e: e.reciprocal(out=stt[:, 1:2], in_=stt[:, 1:2]), reads=['stt'], writes=['stt'])
                P.op('dve', lambda e: e.tensor_scalar(out=Zt[:], in0=Zt[:], scalar1=stt[:, 1:2], scalar2=None, op0=ALU.mult),
                     reads=[zk, 'stt'], writes=[zk])
                P.op('pool', lambda e: e.tensor_tensor(out=Zt[:], in0=Zt[:], in1=LNB[:, 0, :], op=ALU.mult), reads=[zk, 'LNB'], writes=[zk])
                P.op('dve', lambda e: e.tensor_tensor(out=Zt[:], in0=Zt[:], in1=LNB[:, 1, :], op=ALU.add), reads=[zk, 'LNB'], writes=[zk])

            def do_half(hf):
                tk0 = hf * NT_H
                with ExitStack() as es:
                    P.barrier()
                    PSA.reset()
                    WA = P.sb(es, 'WA', [128, 4, D], BF16)
                    WB = P.sb(es, 'WB', [128, 4, D], BF16)
                    WG = P.sb(es, 'WG', [128, 8, 2048], BF16)
                    WO = P.sb(es, 'WO', [128, 8, D], BF16)
                    RW = P.sb(es, 'RW', [128, 8, 36])
                    load_ln(['ln1_g', 'ln1_b'])
                    with ExitStack() as es1:
                        load_cast(es1, WA, 'WA', lambda c: w_a_out_d[c * 128:(c + 1) * 128, :], D, 4)
                        load_cast(es1, WB, 'WB', lambda c: w_b_out_d[c * 128:(c + 1) * 128, :], D, 4)
                        load_cast(es1, WO, 'WO', lambda c: w_o_d[c * 128:(c + 1) * 128, :], D, 8)
                        load_cast(es1, WG, 'WG', lambda c: w_in[c * 128:(c + 1) * 128, :], 2048, 8, col0=C_GATE)
                        for c in range(8):
                            P.dma(RW[:, c, :], rw_d[c * 128:(c + 1) * 128, :], writes=['RW'])
                        P.flush()
                    P.barrier()
                    yaTb = P.sb(es, 'yaTb', [128, 4, 512], BF16)
                    ybTb = P.sb(es, 'ybTb', [128, 4, 512], BF16)
                    xgs = [P.sb(es, 'xgs', [128, 512]) for _ in range(2)]
                    xgb = P.sb(es, 'xgb', [128, 8, 512], BF16)
                    sga = P.sb(es, 'sga', [128, 512])
                    sgb = P.sb(es, 'sgb', [128, 512])
                    m1 = P.sb(es, 'm1', [128, 512])
                    m2 = P.sb(es, 'm2', [128, 512])
                    mT = P.sb(es, 'mT', [128, 8, 512], BF16)
                    xo_t = P.sb(es, 'xo_t', [128, D])
                    Zt = P.sb(es, 'Zt', [128, D])
                    sq_t = P.sb(es, 'sq_t', [128, D])
                    stt = P.sb(es, 'stt', [128, 2])
                    x1Tf = P.sb(es, 'x1Tf', [128, 8, 128])
                    Lg = P.sb(es, 'Lg', [128, 36])
                    rt = P.sb(es, 'rt', [128, 64])
                    psA = PSA.alloc(512, 0)
                    psB = PSA.alloc(512, 1)
                    psGa = PSA.alloc(512, 2)
                    psGb = PSA.alloc(512, 3)
                    psO = [PSA.alloc(512, 4), PSA.alloc(512, 5)]
                    psTr = [PSA.alloc(512, 6), PSA.alloc(512, 7)]
                    psR = psA
                    for tbk in range(NT_H // 512):
                        tb0 = tk0 + tbk * 512
                        P.dma(yaTb[:], yaT_d.rearrange("(c p) t -> p c t", p=128)[:, :, tb0:tb0 + 512],
                              reads=['dram_yaT_%d_%d' % (hp_, tb0 // NB + j_) for hp_ in range(4) for j_ in range(512 // NB)], writes=['yaTb'])
                        P.dma(ybTb[:], ybT_d.rearrange("(c p) t -> p c t", p=128)[:, :, tb0:tb0 + 512],
                              reads=['dram_ybT_%d' % (tb0 // 128 + j_) for j_ in range(4)], writes=['ybTb'])
                        for dc in range(8):
                            s_ = xgs[dc % 2]
                            k_ = 'xgs%d' % (dc % 2)
                            P.dma(s_[:], xT[dc * 128:(dc + 1) * 128, own_off + tb0:own_off + tb0 + 512], writes=[k_])
                            P.op('pool' if dc % 2 else 'dve', lambda e, s_=s_, dc=dc: e.tensor_copy(out=xgb[:, dc, :], in_=s_[:]),
                                 reads=[k_], writes=['xgb'])
                        for dco in range(8):
                            cso = slice(dco * 128, (dco + 1) * 128)
                            for fc in range(4):
                                P.op('pe', lambda e, fc=fc, cso=cso: e.matmul(psA[0], lhsT=WA[:, fc, cso], rhs=yaTb[:, fc, :],
                                                                             start=(fc == 0), stop=(fc == 3)), reads=['WA', 'yaTb'], writes=[psA[1]])
                            for fc in range(4):
                                P.op('pe', lambda e, fc=fc, cso=cso: e.matmul(psB[0], lhsT=WB[:, fc, cso], rhs=ybTb[:, fc, :],
                                                                             start=(fc == 0), stop=(fc == 3)), reads=['WB', 'ybTb'], writes=[psB[1]])
                            for dc in range(8):
                                P.op('pe', lambda e, dc=dc, dco=dco: e.matmul(psGa[0], lhsT=WG[:, dc, dco * 128:(dco + 1) * 128], rhs=xgb[:, dc, :],
                                                                             start=(dc == 0), stop=(dc == 7)), reads=['WG', 'xgb'], writes=[psGa[1]])
                            for dc in range(8):
                                P.op('pe', lambda e, dc=dc, dco=dco: e.matmul(psGb[0], lhsT=WG[:, dc, 1024 + dco * 128:1024 + (dco + 1) * 128],
                                                                             rhs=xgb[:, dc, :], start=(dc == 0), stop=(dc == 7)),
                                     reads=['WG', 'xgb'], writes=[psGb[1]])
                            P.op('act', lambda e: e.activation(out=sga[:], in_=psGa[0], func=AF.Sigmoid), reads=[psGa[1]], writes=['sga'])
                            P.op('act', lambda e: e.activation(out=sgb[:], in_=psGb[0], func=AF.Sigmoid), reads=[psGb[1]], writes=['sgb'])
                            P.op('dve', lambda e: e.tensor_tensor(out=m1[:], in0=psA[0], in1=sga[:], op=ALU.mult), reads=[psA[1], 'sga'], writes=['m1'])
                            P.op('dve', lambda e: e.tensor_tensor(out=m2[:], in0=psB[0], in1=sgb[:], op=ALU.mult), reads=[psB[1], 'sgb'], writes=['m2'])
                            P.op('pool', lambda e, dco=dco: e.tensor_tensor(out=mT[:, dco, :], in0=m1[:], in1=m2[:], op=ALU.add),
                                 reads=['m1', 'm2'], writes=['mT'])
                        for tt in range(4):
                            tile_i = tbk * 4 + tt
                            trow = tb0 + tt * 128
                            P.dma(xo_t[:], xo_d[trow:trow + 128, :], writes=['xo_t'])
                            for hh in range(2):
                                for dc in range(8):
                                    P.op('pe', lambda e, dc=dc, tt=tt, hh=hh: e.matmul(
                                        psO[hh][0], lhsT=mT[:, dc, tt * 128:(tt + 1) * 128], rhs=WO[:, dc, hh * 512:(hh + 1) * 512],
                                        start=(dc == 0), stop=(dc == 7)), reads=['mT', 'WO'], writes=[psO[hh][1]])
                                P.op('dve', lambda e, hh=hh: e.scalar_tensor_tensor(
                                    out=Zt[:, hh * 512:(hh + 1) * 512], in0=xo_t[:, hh * 512:(hh + 1) * 512], scalar=ALPHA, in1=psO[hh][0],
                                    op0=ALU.mult, op1=ALU.add), reads=['xo_t', psO[hh][1]], writes=['Zt'])
                            layer_norm_tile(Zt, 'Zt', 0, stt, sq_t)
                            P.op('act', lambda e, tile_i=tile_i: e.mul(out=ACC[:, tile_i, :], in_=Zt[:], mul=ALPHA), reads=['Zt'], writes=['ACC%d' % tile_i])
                            for dc in range(8):
                                pt = psTr[dc // 4]
                                P.op('pe', lambda e, dc=dc, pt=pt: e.matmul(pt[0][:, (dc % 4) * 128:(dc % 4 + 1) * 128],
                                                                            lhsT=Zt[:, dc * 128:(dc + 1) * 128], rhs=ident[:], start=True, stop=True),
                                     reads=['Zt', 'ident'], writes=[pt[1]])
                            for q in range(2):
                                pt = psTr[q]
                                P.op('act', lambda e, q=q, pt=pt: e.copy(out=x1Tf[:, q * 4:(q + 1) * 4, :].rearrange("p a t -> p (a t)"), in_=pt[0]),
                                     reads=[pt[1]], writes=['x1Tf'])
                                P.op('dve', lambda e, q=q, pt=pt, tile_i=tile_i: e.tensor_copy(
                                    out=x1T[:, q * 4:(q + 1) * 4, tile_i * 128:(tile_i + 1) * 128],
                                    in_=pt[0].rearrange("p (a t) -> p a t", t=128)), reads=[pt[1]], writes=['x1T'])
                            for dc in range(8):
                                P.op('pe', lambda e, dc=dc: e.matmul(psR[0][:, 0:36], lhsT=x1Tf[:, dc, :], rhs=RW[:, dc, :],
                                                                     start=(dc == 0), stop=(dc == 7)), reads=['x1Tf', 'RW'], writes=[psR[1]])
                            P.op('dve', lambda e: e.tensor_tensor(out=Lg[:], in0=psR[0][:, 0:36], in1=RBB[:], op=ALU.add),
                                 reads=[psR[1], 'RBB'], writes=['Lg'])
                            RT = lambda a, b: rt[:, a:b]
                            dv = lambda fn, rd=('Lg', 'rt'): P.op('dve', fn, reads=list(rd), writes=['rt'])
                            dv(lambda e: e.tensor_reduce(out=RT(0, 1), in_=Lg[:, 0:4], axis=AX.X, op=ALU.max))
                            dv(lambda e: e.tensor_scalar(out=RT(1, 2), in0=RT(0, 1), scalar1=-1.0, scalar2=None, op0=ALU.mult))
                            P.op('act', lambda e: e.activation(out=RT(16, 20), in_=Lg[:, 0:4], func=AF.Exp, bias=RT(1, 2)), reads=['Lg', 'rt'], writes=['rt'])
                            dv(lambda e: e.tensor_reduce(out=RT(2, 3), in_=RT(16, 20), axis=AX.X, op=ALU.add))
                            dv(lambda e: e.reciprocal(out=RT(3, 4), in_=RT(2, 3)))
                            dv(lambda e: e.tensor_scalar(out=RT(16, 20), in0=Lg[:, 0:4], scalar1=RT(0, 1), scalar2=None, op0=ALU.is_equal))
                            P.op('dve', lambda e: e.tensor_tensor(out=sq_t[:, 0:32].rearrange("p (g j) -> p g j", j=8),
                                                                  in0=Lg[:, 4:36].rearrange("p (g j) -> p g j", j=8),
                                                                  in1=RT(16, 20).unsqueeze(2).to_broadcast([128, 4, 8]), op=ALU.mult),
                                 reads=['Lg', 'rt'], writes=['sq_t'])
                            P.op('dve', lambda e: e.tensor_reduce(out=RT(24, 32), in_=sq_t[:, 0:32].rearrange("p (g j) -> p j g", j=8), axis=AX.X, op=ALU.add),
                                 reads=['sq_t', 'rt'], writes=['rt', 'sq_t'])
                            dv(lambda e: e.tensor_reduce(out=RT(4, 5), in_=RT(24, 32), axis=AX.X, op=ALU.max))
                            dv(lambda e: e.tensor_scalar(out=RT(32, 40), in0=RT(24, 32), scalar1=RT(4, 5), scalar2=None, op0=ALU.is_equal))
                            dv(lambda e: e.scalar_tensor_tensor(out=RT(40, 48), in0=RT(32, 40), scalar=-1e30, in1=RT(24, 32), op0=ALU.mult, op1=ALU.add))
                            dv(lambda e: e.tensor_reduce(out=RT(5, 6), in_=RT(40, 48), axis=AX.X, op=ALU.max))
                            dv(lambda e: e.tensor_scalar(out=RT(40, 48), in0=RT(40, 48), scalar1=RT(5, 6), scalar2=None, op0=ALU.is_equal))
                            dv(lambda e: e.tensor_tensor(out=RT(6, 7), in0=RT(4, 5), in1=RT(5, 6), op=ALU.subtract))
                            P.op('act', lambda e: e.activation(out=RT(7, 8), in_=RT(6, 7), func=AF.Sigmoid), reads=['rt'], writes=['rt'])
                            dv(lambda e: e.tensor_tensor(out=RT(8, 9), in0=RT(7, 8), in1=RT(3, 4), op=ALU.mult))
                            dv(lambda e: e.tensor_tensor(out=RT(9, 10), in0=RT(3, 4), in1=RT(8, 9), op=ALU.subtract))
                            dv(lambda e: e.tensor_scalar(out=RT(48, 56), in0=RT(32, 40), scalar1=RT(8, 9), scalar2=None, op0=ALU.mult))
                            dv(lambda e: e.scalar_tensor_tensor(out=RT(48, 56), in0=RT(40, 48), scalar=RT(9, 10), in1=RT(48, 56), op0=ALU.mult, op1=ALU.add))
                            P.op('dve', lambda e, tile_i=tile_i: e.tensor_tensor(out=WtAll[:, tile_i, :].rearrange("p (g j) -> p g j", j=8),
                                                                  in0=RT(16, 20).unsqueeze(2).to_broadcast([128, 4, 8]),
                                                                  in1=RT(48, 56).unsqueeze(1).to_broadcast([128, 4, 8]), op=ALU.mult),
                                 reads=['rt'], writes=['WtAll'])
                    P.flush()
                with ExitStack() as es:
                    P.barrier()
                    PSA.reset()
                    W1b = [P.sb(es, 'W1b', [128, 8, 512], BF16) for _ in range(2)]
                    W3b = [P.sb(es, 'W3b', [128, 8, 512], BF16) for _ in range(2)]
                    W2b = [P.sb(es, 'W2b', [128, 4, D], BF16) for _ in range(2)]
                    wst = [P.sb(es, 'wst', [128, 2048]) for _ in range(3)]
                    sil = [P.sb(es, 'sil', [128, 512]) for _ in range(2)]
                    hwT = [P.sb(es, 'hwT', [128, 4, 512], BF16) for _ in range(2)]
                    psH1 = [PSA.alloc(512, 1), PSA.alloc(512, 2)]
                    psH3 = [PSA.alloc(512, 3), PSA.alloc(512, 4)]
                    psF = [PSA.alloc(512, 5), PSA.alloc(512, 6)]
                    wq = [0]
                    ceng = ['pool', 'act']

                    def wload(dst, dkey, src3, nck):
                        for c in range(nck):
                            i_ = wq[0] % 3
                            wq[0] += 1
                            s_ = wst[i_]
                            k_ = 'wst%d' % i_
                            P.dma(s_[:].rearrange("p (a f) -> p a f", a=src3(c).shape[1]), src3(c), writes=[k_])
                            en = ceng[wq[0] % 2]
                            if en == 'act':
                                P.op('act', lambda e, s_=s_, c=c: e.copy(out=dst(c), in_=s_[:]), reads=[k_], writes=[dkey])
                            else:
                                P.op('pool', lambda e, s_=s_, c=c: e.tensor_copy(out=dst(c), in_=s_[:]), reads=[k_], writes=[dkey])
                    for ex in range(32):
                        pb_ = ex % 2
                        w1v = w1_d[ex].rearrange("(c p) f -> p c f", p=128)
                        w3v = w3_d[ex].rearrange("(c p) f -> p c f", p=128)
                        w2v = w2_d[ex].rearrange("(c p) d -> p c d", p=128)
                        wload(lambda c, pb_=pb_: W1b[pb_][:, c * 4:(c + 1) * 4, :].rearrange("p a f -> p (a f)"), 'W1b%d' % pb_,
                              lambda c, w1v=w1v: w1v[:, c * 4:(c + 1) * 4, :], 2)
                        wload(lambda c, pb_=pb_: W3b[pb_][:, c * 4:(c + 1) * 4, :].rearrange("p a f -> p (a f)"), 'W3b%d' % pb_,
                              lambda c, w3v=w3v: w3v[:, c * 4:(c + 1) * 4, :], 2)
                        wload(lambda c, pb_=pb_: W2b[pb_][:, c * 2:(c + 1) * 2, :].rearrange("p a f -> p (a f)"), 'W2b%d' % pb_,
                              lambda c, w2v=w2v: w2v[:, c * 2:(c + 1) * 2, :], 2)
                        for tbk in range(NT_H // 512):
                            tsl = slice(tbk * 512, (tbk + 1) * 512)
                            hw = hwT[tbk % 2]
                            hwk = 'hwT%d' % (tbk % 2)
                            for fc in range(4):
                                p1, p3 = psH1[fc % 2], psH3[fc % 2]
                                fsl = slice(fc * 128, (fc + 1) * 128)
                                for dc in range(8):
                                    P.op('pe', lambda e, dc=dc, fsl=fsl, tsl=tsl, p1=p1, pb_=pb_: e.matmul(
                                        p1[0], lhsT=W1b[pb_][:, dc, fsl], rhs=x1T[:, dc, tsl], start=(dc == 0), stop=(dc == 7)),
                                        reads=['W1b%d' % pb_, 'x1T'], writes=[p1[1]])
                                for dc in range(8):
                                    P.op('pe', lambda e, dc=dc, fsl=fsl, tsl=tsl, p3=p3, pb_=pb_: e.matmul(
                                        p3[0], lhsT=W3b[pb_][:, dc, fsl], rhs=x1T[:, dc, tsl], start=(dc == 0), stop=(dc == 7)),
                                        reads=['W3b%d' % pb_, 'x1T'], writes=[p3[1]])
                                sl_ = sil[fc % 2]
                                P.op('act', lambda e, sl_=sl_, p1=p1: e.activation(out=sl_[:], in_=p1[0], func=AF.Silu),
                                     reads=[p1[1]], writes=['sil%d' % (fc % 2)])
                                P.op('dve', lambda e, sl_=sl_, hw=hw, fc=fc, p3=p3: e.tensor_tensor(out=hw[:, fc, :], in0=p3[0], in1=sl_[:], op=ALU.mult),
                                     reads=[p3[1], 'sil%d' % (fc % 2)], writes=[hwk])
                            for tt in range(4):
                                tile_i = tbk * 4 + tt
                                for hx in range(2):
                                    pf = psF[hx]
                                    for fc in range(4):
                                        P.op('pe', lambda e, fc=fc, tt=tt, hx=hx, pf=pf, hw=hw, pb_=pb_: e.matmul(
                                            pf[0], lhsT=hw[:, fc, tt * 128:(tt + 1) * 128], rhs=W2b[pb_][:, fc, hx * 512:(hx + 1) * 512],
                                            start=(fc == 0), stop=(fc == 3)), reads=[hwk, 'W2b%d' % pb_], writes=[pf[1]])
                                    P.op('dve', lambda e, tile_i=tile_i, hx=hx, pf=pf, ex=ex: e.scalar_tensor_tensor(
                                        out=ACC[:, tile_i, hx * 512:(hx + 1) * 512], in0=pf[0], scalar=WtAll[:, tile_i, ex:ex + 1],
                                        in1=ACC[:, tile_i, hx * 512:(hx + 1) * 512], op0=ALU.mult, op1=ALU.add),
                                        reads=[pf[1], 'ACC%d' % tile_i, 'WtAll'], writes=['ACC%d' % tile_i])
                    P.flush()
                with ExitStack() as es:
                    P.barrier()
                    PSA.reset()
                    WPG = P.sb(es, 'WPG', [128, 8, D], BF16)
                    load_ln(['ln2_g', 'ln2_b'])
                    WPE = P.sb(es, 'WPE', [128, 2, D], BF16)
                    pTb = P.sb(es, 'pTb', [128, 2, NT_H], BF16)
                    with ExitStack() as es1:
                        load_cast(es1, WPG, 'WPG', lambda c: w_pg_d[c * 128:(c + 1) * 128, :], D, 8)
                        load_cast(es1, WPE, 'WPE', lambda c: w_pe_d[c * 128:(c + 1) * 128, :], D, 2)
                        load_cast(es1, pTb, 'pTb', lambda c: pT_d[c * 128:(c + 1) * 128, :], NT_H, 2, col0=tk0)
                        P.flush()
                    P.barrier()
                    Z2 = P.sb(es, 'Z2', [128, D])
                    sq_t = P.sb(es, 'sq_t2', [128, D])
                    stt = P.sb(es, 'stt2', [128, 2])
                    x2T = P.sb(es, 'x2T', [128, 8, 128], BF16)
                    sgp = P.sb(es, 'sgp', [128, 512])
                    ot_ = [P.sb(es, 'ot_', [128, D]) for _ in range(2)]
                    psTr = [PSA.alloc(512, 0), PSA.alloc(512, 1)]
                    psG = [PSA.alloc(512, 2), PSA.alloc(512, 3)]
                    psP = [PSA.alloc(512, 4), PSA.alloc(512, 5)]
                    for tile_i in range(NT_H // 128):
                        trow = tk0 + tile_i * 128
                        P.op('act', lambda e, tile_i=tile_i: e.copy(out=Z2[:], in_=ACC[:, tile_i, :]), reads=['ACC%d' % tile_i], writes=['Z2'])
                        layer_norm_tile(Z2, 'Z2', 2, stt, sq_t)
                        for dc in range(8):
                            pt = psTr[dc // 4]
                            P.op('pe', lambda e, dc=dc, pt=pt: e.matmul(pt[0][:, (dc % 4) * 128:(dc % 4 + 1) * 128],
                                                                        lhsT=Z2[:, dc * 128:(dc + 1) * 128], rhs=ident[:], start=True, stop=True),
                                 reads=['Z2', 'ident'], writes=[pt[1]])
                        for q in range(2):
                            pt = psTr[q]
                            evac3(x2T[:, q * 4:(q + 1) * 4, :].rearrange("p a t -> p (a t)"), pt[0], [pt[1]], ['x2T'])
                        o_t = ot_[tile_i % 2]
                        ok_ = 'ot_%d' % (tile_i % 2)
                        for hx in range(2):
                            for dc in range(8):
                                P.op('pe', lambda e, dc=dc, hx=hx: e.matmul(psG[hx][0], lhsT=x2T[:, dc, :], rhs=WPG[:, dc, hx * 512:(hx + 1) * 512],
                                                                            start=(dc == 0), stop=(dc == 7)), reads=['x2T', 'WPG'], writes=[psG[hx][1]])
                            for kc in range(2):
                                P.op('pe', lambda e, kc=kc, hx=hx, tile_i=tile_i: e.matmul(
                                    psP[hx][0], lhsT=pTb[:, kc, tile_i * 128:(tile_i + 1) * 128], rhs=WPE[:, kc, hx * 512:(hx + 1) * 512],
                                    start=(kc == 0), stop=(kc == 1)), reads=['pTb', 'WPE'], writes=[psP[hx][1]])
                            P.op('act', lambda e, hx=hx: e.activation(out=sgp[:], in_=psG[hx][0], func=AF.Sigmoid), reads=[psG[hx][1]], writes=['sgp'])
                            P.op('dve', lambda e, hx=hx, o_t=o_t: e.tensor_tensor(out=o_t[:, hx * 512:(hx + 1) * 512], in0=psP[hx][0], in1=sgp[:], op=ALU.mult),
                                 reads=[psP[hx][1], 'sgp'], writes=[ok_])
                            P.op('pool', lambda e, hx=hx, o_t=o_t: e.tensor_tensor(out=o_t[:, hx * 512:(hx + 1) * 512], in0=o_t[:, hx * 512:(hx + 1) * 512],
                                                                                   in1=Z2[:, hx * 512:(hx + 1) * 512], op=ALU.add),
                                 reads=[ok_, 'Z2'], writes=[ok_])
                        P.dma(out_d[trow:trow + 128, :], o_t[:], reads=[ok_], writes=['dram_out_%d' % trow])
                    P.flush()
            for hf_ in range(TOWN // NT_H):
                do_half(hf_)
            P.flush()
        print('NOPS', P.nops, {k: len(v) for k, v in P.streams.items()}, flush=True)
        P.emit()
    return nc


def make_in_maps(inputs):
    f32 = lambda a: np.ascontiguousarray(np.asarray(a, np.float32))
    x = f32(inputs['x'])
    consts = host_consts()
    pv = np.zeros((128, NPV), np.float32)
    pv[:, PV['w0']:PV['w0'] + 4] = colvec(inputs['rw_w0'][0], 4)
    pv[:, PV['a0']:PV['a0'] + 4] = colvec(inputs['rw_a0'][0], 4)
    pv[:, PV['k_k']:PV['k_k'] + 4] = colvec(inputs['rw_k_k'][0], 4)
    pv[:, PV['k_a']:PV['k_a'] + 4] = colvec(inputs['rw_k_a'][0], 4)
    pv[:, PV['r_k']:PV['r_k'] + 4] = colvec(np.asarray(inputs['rw_r_k'][0]).reshape(512), 4)
    pv[:, PV['lb0']:PV['lb0'] + 4] = colvec(inputs['hg_lb_logits'][0], 4)
    pv[:, PV['lb1']:PV['lb1'] + 4] = colvec(inputs['hg_lb_logits'][1], 4)
    rows = np.zeros((1, NROWS), np.float32)

    def setrow(name, v):
        o, n = ROWS[name]
        rows[0, o:o + n] = np.asarray(v, np.float32).reshape(n)
    setrow('mu', inputs['rw_mu'][0])
    setrow('gn_w', inputs['rw_gn_w'][0])
    setrow('gn_b', inputs['rw_gn_b'][0])
    setrow('hg_nw', inputs['hg_norm_w'][0])
    setrow('ln1_g', inputs['ln1_g'][0])
    setrow('ln1_b', inputs['ln1_b'][0])
    setrow('ln2_g', inputs['ln2_g'][0])
    setrow('ln2_b', inputs['ln2_b'][0])
    setrow('rb', np.concatenate([np.asarray(inputs['router_g_b'][0]).reshape(4), np.asarray(inputs['router_e_b'][0]).reshape(32)]))
    gw = np.asarray(inputs['rw_gn_w'][0], np.float32).reshape(4, 2, 1, 64)
    gb = np.asarray(inputs['rw_gn_b'][0], np.float32).reshape(4, 2, 1, 64)
    gnw_t = np.ascontiguousarray(np.broadcast_to(gw, (4, 2, 64, 64)).reshape(4, 128, 64))
    gnb_t = np.ascontiguousarray(np.broadcast_to(gb, (4, 2, 64, 64)).reshape(4, 128, 64))
    shared = dict(consts)
    shared.update(pv=pv, rows=rows, gnw_t=gnw_t, gnb_t=gnb_t,
                  w_in=f32(inputs['w_in'][0]), rw_w_up=f32(inputs['rw_w_up'][0]), rw_a_up=f32(inputs['rw_a_up'][0]),
                  rw_g_up=f32(inputs['rw_g_up'][0]),
                  w_a_out=f32(inputs['w_a_out'][0]), w_b_out=f32(inputs['w_b_out'][0]), w_o=f32(inputs['w_o'][0]),
                  rw=np.ascontiguousarray(np.concatenate([f32(inputs['router_g_w'][0]), f32(inputs['router_e_w'][0])], 1)),
                  w1=f32(inputs['w1'][0]), w3=f32(inputs['w3'][0]), w2=f32(inputs['w2'][0]),
                  w_pe=f32(inputs['w_pe'][0]), w_pg=f32(inputs['w_pg'][0]))
    in_maps = []
    for c in range(8):
        b, half = c // 2, c % 2
        xw = np.zeros((D, T), np.float32)
        if half == 1:
            xw[:, :] = x[b].T
        else:
            xw[:, T - TOWN:] = x[b, :TOWN].T
        m = dict(shared)
        m['xT'] = xw
        m['xo'] = np.ascontiguousarray(x[b, half * TOWN:(half + 1) * TOWN])
        m['pT'] = np.ascontiguousarray(np.asarray(inputs['p'], np.float32)[0, b, half * TOWN:(half + 1) * TOWN].T)
        in_maps.append(m)
    return in_maps


_NC_CACHE = {}


def kernel(**inputs):
    if 'full' not in _NC_CACHE:
        _NC_CACHE['full'] = build('full')
    nc = _NC_CACHE['full']
    in_maps = make_in_maps(inputs)
    keep = set(['xT', 'w_in', 'pv', 'rows', 'ident', 'mask320', 'iu2', 'ii', 'onesblk', 'scanmask',
                'rw_w_up', 'rw_a_up', 'rw_g_up', 'gnw_t', 'gnb_t', 'xo', 'pT', 'w_a_out', 'w_b_out', 'w_o', 'rw',
                'w1', 'w3', 'w2', 'w_pe', 'w_pg'])
    in_maps = [{k: v for k, v in m.items() if k in keep} for m in in_maps]
    res = run_bass_kernel_spmd(nc, in_maps, core_ids=list(range(8)))
    out = np.zeros((4, T, D), np.float32)
    for c in range(8):
        b, half = c // 2, c % 2
        out[b, half * TOWN:(half + 1) * TOWN] = res.results[c]['out']
    return out
```

```python
import os
import numpy as np
from contextlib import ExitStack
import concourse.bass as bass
import concourse.mybir as mybir
from concourse.bass_utils import run_bass_kernel_spmd

F32 = mybir.dt.float32
BF16 = mybir.dt.bfloat16
ALU = mybir.AluOpType
AF = mybir.ActivationFunctionType
AX = mybir.AxisListType

NDSEM = 8
T = 4096
TOWN = 2048
D = 1024
NB = 256
NCH = NB // 64
NBLK = T // NB
OWNB = (T - TOWN) // NB
CDEC = 0.6065306597126334
ALPHA = 2.0 ** 0.25
N_IN = 5888
C_Q = 1792
C_GATE = 3840


class Prog:
    def __init__(self, nc, es):
        self.nc = nc
        self.es = es
        self.engs = ['pe', 'act', 'dve', 'pool', 'sp']
        self.streams = {e: [] for e in self.engs}
        self.prods = ['pe', 'act', 'dve', 'pool']
        self.count = {p: 0 for p in self.prods}
        self.sems = {p: es.enter_context(nc.semaphore('s_' + p)) for p in self.prods}
        self.waited = {}
        self.last_write = {}
        self.readers = {}
        self.ndma = 0
        self.uid = 0
        self.floor = {}

    def barrier(self):
        self.floor = dict(self.count)

    def sb(self, es, name, shape, dt=F32):
        self.uid += 1
        return es.enter_context(self.nc.sbuf_tensor('%s_%d' % (name, self.uid), list(shape), dt))

    def _deps(self, eng, prod, reads, writes):
        deps = {}
        writes = list(writes) + [k for k in reads if k.startswith('bank')]
        reads = [k for k in reads if not k.startswith('bank')]

        def add(p, n):
            if n > deps.get(p, 0):
                deps[p] = n
        for k in reads:
            if k in self.last_write:
                add(*self.last_write[k])
        for k in writes:
            if k in self.last_write:
                add(*self.last_write[k])
            for p, n in self.readers.get(k, {}).items():
                add(p, n)
        waits = []
        for p, n in self.floor.items():
            if n > deps.get(p, 0) and self.waited.get((eng, p), 0) < n:
                deps[p] = n
        for p, n in deps.items():
            if p == 'pe' and eng == 'pe' and self.floor.get('pe', 0) < n:
                continue
            if self.waited.get((eng, p), 0) >= n:
                continue
            self.waited[(eng, p)] = n
            waits.append((p, n))
        self.count[prod] += 1
        idx = self.count[prod]
        for k in writes:
            self.last_write[k] = (prod, idx)
            self.readers[k] = {}
        for k in reads:
            rd = self.readers.setdefault(k, {})
            if rd.get(prod, 0) < idx:
                rd[prod] = idx
        return waits

    def op(self, eng, fn, reads=(), writes=()):
        self.nops = getattr(self, 'nops', 0) + 1
        if self.nops > int(os.environ.get('KMAX', '100000000')):
            return
        waits = self._deps(eng, eng, reads, writes)
        self.streams[eng].append((waits, fn, eng))

    def dma(self, out, in_, reads=(), writes=(), eng='sp'):
        self.nops = getattr(self, 'nops', 0) + 1
        if self.nops > int(os.environ.get('KMAX', '100000000')):
            return
        skey = [k for k in list(writes) + list(reads) if not k.startswith('dram_')][0]
        prod = 'd_' + skey
        if prod not in self.sems:
            self.sems[prod] = self.es.enter_context(self.nc.semaphore('s_' + prod))
            self.count[prod] = 0
            self.prods.append(prod)
        self.ndma += 1
        waits = self._deps(eng, prod, reads, writes)
        self.streams[eng].append((waits, lambda e: e.dma_start(out=out, in_=in_), prod))

    def emit(self):
        self.flush(final=True)

    def flush(self, final=False):
        nc = self.nc
        if not final and not any(self.streams.values()):
            return
        streams = self.streams
        self.streams = {e: [] for e in self.engs}

        def run(ename, e):
            for waits, fn, prod in streams[ename]:
                for p, n in waits:
                    e.wait_ge(self.sems[p], n * 16 if p[0] == 'd' else n)
                ins = fn(e)
                ins.then_inc(self.sems[prod], 16 if prod[0] == 'd' else 1)
            if ename == 'sp' and final:
                for p in self.prods:
                    if self.count[p] > 0:
                        e.wait_ge(self.sems[p], self.count[p] * (16 if p[0] == 'd' else 1))
        with nc.Block() as block:
            @block.tensor
            def _(e):
                run('pe', e)

            @block.scalar
            def _(e):
                run('act', e)

            @block.vector
            def _(e):
                run('dve', e)

            @block.gpsimd
            def _(e):
                run('pool', e)

            @block.sync
            def _(e):
                run('sp', e)


class PsumAlloc:
    def __init__(self, P, es):
        self.banks = [es.enter_context(P.nc.psum_tensor('psb%d' % i, [128, 512], F32)) for i in range(8)]
        self.used = [0] * 8
        self.n = 0

    def alloc(self, cols, bank):
        b = bank
        assert self.used[b] + cols <= 512, (bank, cols, self.used[b])
        o = self.used[b]
        self.used[b] += cols
        return self.banks[b][:, o:o + cols], 'bank%d' % b

    def reset(self):
        self.used = [0] * 8


def colvec(v, n):
    return np.ascontiguousarray(np.asarray(v, np.float32).reshape(n, 128).T)


def host_consts():
    c = {}
    c['ident'] = np.eye(128, dtype=np.float32)
    su = np.triu(np.ones((64, 64), np.float32), 1)
    sl = np.tril(np.ones((64, 64), np.float32), -1)
    iu = np.triu(np.ones((64, 64), np.float32), 0)
    m = np.concatenate([su, sl, su, iu, iu], 1)
    c['mask320'] = np.concatenate([m, m], 0)
    c['iu2'] = np.concatenate([iu, iu], 0)
    i64 = np.eye(64, dtype=np.float32)
    ii = np.concatenate([i64, i64], 1)
    c['ii'] = np.concatenate([ii, ii], 0)
    ob = np.zeros((128, 128), np.float32)
    ob[:64, :64] = 1
    ob[64:, 64:] = 1
    c['onesblk'] = ob
    sm = np.ones((128, NB), np.float32)
    sm[:, ::64] = 0
    c['scanmask'] = sm
    sel = np.zeros((32, 32 * 128), np.float32)
    for e in range(32):
        sel[e, e * 128:(e + 1) * 128] = 1
    c['sel'] = sel
    return c


PV = {}
_o = 0
for _name, _n in [('w0', 4), ('a0', 4), ('k_k', 4), ('k_a', 4), ('r_k', 4), ('lb0', 4), ('lb1', 4)]:
    PV[_name] = _o
    _o += _n
NPV = _o
ROWS = {}
_o = 0
for _name, _n in [('mu', 1792), ('gn_w', 512), ('gn_b', 512), ('hg_nw', 512), ('ln1_g', 1024), ('ln1_b', 1024),
                  ('ln2_g', 1024), ('ln2_b', 1024), ('rb', 36)]:
    ROWS[_name] = (_o, _n)
    _o += _n
NROWS = _o


def build(stage='full', nblk=NBLK, ownb=OWNB):
    nc = bass.Bass("TRN2", target_bir_lowering=False)

    def din(name, shape, dt=F32):
        return nc.dram_tensor(name, list(shape), dt, kind="ExternalInput").ap()

    def dout(name, shape, dt=F32):
        return nc.dram_tensor(name, list(shape), dt, kind="ExternalOutput").ap()

    xT = din('xT', [D, T])
    w_in = din('w_in', [D, N_IN])
    pv_d = din('pv', [128, NPV])
    rows_d = din('rows', [1, NROWS])
    ident_d = din('ident', [128, 128])
    mask320_d = din('mask320', [128, 320])
    iu2_d = din('iu2', [128, 64])
    ii_d = din('ii', [128, 128])
    onesblk_d = din('onesblk', [128, 128])
    scanmask_d = din('scanmask', [128, NB])
    w_up_d = din('rw_w_up', [64, 512])
    a_up_d = din('rw_a_up', [64, 512])
    g_up_d = din('rw_g_up', [128, 512])
    gnw_d = din('gnw_t', [4, 128, 64])
    gnb_d = din('gnb_t', [4, 128, 64])

    if stage == 'full':
        xo_d = din('xo', [TOWN, D])
        pT_d = din('pT', [256, TOWN])
        w_a_out_d = din('w_a_out', [512, D])
        w_b_out_d = din('w_b_out', [512, D])
        w_o_d = din('w_o', [D, D])
        rw_d = din('rw', [D, 36])
        w1_d = din('w1', [32, D, 512])
        w3_d = din('w3', [32, D, 512])
        w2_d = din('w2', [32, 512, D])
        w_pe_d = din('w_pe', [256, D])
        w_pg_d = din('w_pg', [D, D])
    yaT_d = nc.dram_tensor('yaT_s', [512, TOWN], BF16, kind="Internal").ap()
    ybT_d = nc.dram_tensor('ybT_s', [512, TOWN], BF16, kind="Internal").ap()
    if stage == 'rwkv':
        ya_dbg = dout('ya_dbg', [TOWN // 64, 4, 128, 64])
    elif stage == 'hgrn':
        yb_dbg = dout('yb_dbg', [TOWN, 512])
    elif stage == 'fullA':
        yaT_o = dout('yaT_o', [512, TOWN], BF16)
        ybT_o = dout('ybT_o', [512, TOWN], BF16)
    else:
        out_d = dout('out', [TOWN, D])

    with ExitStack() as es0:
        P = Prog(nc, es0)
        PSA = PsumAlloc(P, es0)
        ident = P.sb(es0, 'ident', [128, 128])
        pvt = P.sb(es0, 'pv', [128, NPV])
        P.dma(ident[:], ident_d, writes=['ident'])
        P.dma(pvt[:], pv_d, writes=['pv'])

        def pcol(name, i):
            o = PV[name] + i
            return pvt[:, o:o + 1]

        with ExitStack() as es:
            PSA.reset()
            mask320 = P.sb(es, 'mask320', [128, 320])
            iit = P.sb(es, 'ii', [128, 128])
            onesblk = P.sb(es, 'onesblk', [128, 128])
            scanmask = P.sb(es, 'scanmask', [128, NB])
            P.dma(mask320[:], mask320_d, writes=['mask320'])
            P.dma(iit[:], ii_d, writes=['ii'])
            P.dma(onesblk[:], onesblk_d, writes=['onesblk'])
            P.dma(scanmask[:], scanmask_d, writes=['scanmask'])
            wA = P.sb(es, 'wA', [128, 8, 1792], BF16)
            wB = P.sb(es, 'wB', [128, 8, 1792], BF16)
            LW = P.sb(es, 'LW', [128, 512], BF16)
            LW2 = P.sb(es, 'LW2', [128, 512], BF16)
            GU = P.sb(es, 'GU', [128, 512], BF16)
            gnw = P.sb(es, 'gnw', [128, 4, 64])
            gnb = P.sb(es, 'gnb', [128, 4, 64])
            for hp in range(4):
                P.dma(gnw[:, hp, :], gnw_d[hp], writes=['gnw'])
                P.dma(gnb[:, hp, :], gnb_d[hp], writes=['gnb'])
            if os.environ.get('KSTOP') == '1':
                P.emit()
                return nc
            with ExitStack() as es1:
                MU = P.sb(es1, 'MU', [128, 1792])
                OMM = P.sb(es1, 'OMM', [128, 1792])
                o, n = ROWS['mu']
                P.dma(MU[:], rows_d[:, o:o + n].partition_broadcast(128), writes=['MU'])
                P.op('dve', lambda e: e.tensor_scalar(out=OMM[:], in0=MU[:], scalar1=-1.0, scalar2=1.0,
                                                      op0=ALU.mult, op1=ALU.add), reads=['MU'], writes=['OMM'])
                stg = [P.sb(es1, 'wstg', [128, 1792]) for _ in range(2)]
                for dc in range(8):
                    s = stg[dc % 2]
                    k = 'wstg%d' % (dc % 2)
                    P.dma(s[:], w_in[dc * 128:(dc + 1) * 128, 0:1792], writes=[k])
                    P.op('dve', lambda e, s=s, dc=dc: e.tensor_tensor(out=wA[:, dc, :], in0=s[:], in1=OMM[:], op=ALU.mult),
                         reads=[k, 'OMM'], writes=['wA'])
                    P.op('pool', lambda e, s=s, dc=dc: e.tensor_tensor(out=wB[:, dc, :], in0=s[:], in1=MU[:], op=ALU.mult),
                         reads=[k, 'MU'], writes=['wB'])
                if os.environ.get('KSTOP') == '2':
                    P.emit()
                    return nc
                ls = P.sb(es1, 'lstg', [128, 512])
                P.dma(ls[0:64, :], w_up_d, writes=['lstg'])
                P.dma(ls[64:128, :], a_up_d, writes=['lstg'])
                P.op('pool', lambda e: e.memset(LW[:], 0.0), writes=['LW'])
                P.op('pool', lambda e: e.memset(LW2[:], 0.0), writes=['LW'])
                P.op('dve', lambda e: e.tensor_copy(out=LW[0:64, :], in_=ls[0:64, :]), reads=['lstg'], writes=['LW'])
                P.op('dve', lambda e: e.tensor_copy(out=LW2[64:128, :], in_=ls[64:128, :]), reads=['lstg'], writes=['LW'])
                gs = P.sb(es1, 'gstg', [128, 512])
                P.dma(gs[:], g_up_d, writes=['gstg'])
                P.op('dve', lambda e: e.tensor_copy(out=GU[:], in_=gs[:]), reads=['gstg'], writes=['GU'])
                P.flush()
            if os.environ.get('KSTOP') == '3':
                P.emit()
                return nc
            if os.environ.get('KNOBAR') != '1':
                P.barrier()

            xstg = [P.sb(es, 'xstg', [128, NB + 1]) for _ in range(2)]
            xb = [P.sb(es, 'xb', [128, 8, NB], BF16) for _ in range(2)]
            xbs_ = [P.sb(es, 'xbs', [128, 8, NB], BF16) for _ in range(2)]
            FM = {}
            for nm in ['Rr', 'Kr', 'Vt', 'At', 'Bt', 'Kt', 'Rt', 'Rtb', 'rk', 'eG']:
                FM[nm] = [P.sb(es, nm, [128, NB], BF16 if nm in ('Vt', 'At', 'Bt', 'Kt', 'Rtb') else F32) for _ in range(4)]
            identb = P.sb(es, 'identb', [128, 128], BF16)
            P.op('dve', lambda e: e.tensor_copy(out=identb[:], in_=ident[:]), reads=['ident'], writes=['identb'])
            tmp = {nm: P.sb(es, nm, [128, NB]) for nm in ['sg', 'a', 'Gs', 'Gp', 'eGn', 'eGp', 'kk', 'sq', 'rn', 'kkn', 't1', 'kp']}
            LX = P.sb(es, 'LX', [128, NB], BF16)
            SGX = P.sb(es, 'SGX', [128, NB], BF16)
            HH = [[P.sb(es, 'H', [128, 64]) for _ in range(2)] for _ in range(4)]
            for hp in range(4):
                P.op('pool', lambda e, hp=hp: e.memset(HH[hp][0][:], 0.0), writes=['H%d_0' % hp])
            NPAR = 2
            TM = [P.sb(es, 'TM', [128, 4, 64], BF16) for _ in range(NPAR)]
            MM = [P.sb(es, 'MM', [128, 320], BF16) for _ in range(NPAR)]
            TT = [[P.sb(es, 'TT', [128, 64], BF16) for _ in range(2)] for _ in range(NPAR)]
            PP = [[P.sb(es, 'PP', [128, 128], BF16) for _ in range(2)] for _ in range(NPAR)]
            X2s = [P.sb(es, 'X2s', [128, 64], BF16) for _ in range(NPAR)]
            WW = [P.sb(es, 'WW', [128, 128]) for _ in range(NPAR)]
            Dg = [P.sb(es, 'Dg', [128, 64]) for _ in range(NPAR)]
            HDg = [P.sb(es, 'HDg', [128, 64]) for _ in range(NPAR)]
            Us = [P.sb(es, 'Us', [128, 64], BF16) for _ in range(NPAR)]
            Yb = [P.sb(es, 'Yb', [128, NCH, 64]) for _ in range(2)]
            Yc = [P.sb(es, 'Yc', [128, NCH, 64]) for _ in range(2)]
            st = [P.sb(es, 'st', [128, 4 * NCH]) for _ in range(2)]
            cfs = [P.sb(es, 'cfs', [128, NCH]) for _ in range(2)]
            yaTs = [P.sb(es, 'yaTs', [128, NB], BF16) for _ in range(2)]
            pj = [PSA.alloc(NB, 0), PSA.alloc(NB, 1)]
            pl = PSA.alloc(2 * NB, 2)
            pss = PSA.alloc(NB, 3)
            psY = [PSA.alloc(NB, 3)] * 2
            psT = [PSA.alloc(256, 4)] * 2
            psW = psT
            psD = [PSA.alloc(128, 4)] * 2
            psX = [PSA.alloc(64, 4)] * 2
            psU = [PSA.alloc(64, 4)] * 2
            psM = [PSA.alloc(320, 5)] * 2
            psE = [PSA.alloc(128, 5)] * 2
            psH = [PSA.alloc(64, 5)] * 2
            pgate = [PSA.alloc(NB, 6)] * 2
            pyt = [PSA.alloc(NB, 6)] * 2
            pcf = [PSA.alloc(NCH, 7)] * 2

            evq = [0]

            def evac_copy(out, in_, reads, writes):
                evq[0] += 1
                if evq[0] % 2:
                    P.op('act', lambda e: e.copy(out=out, in_=in_), reads=reads, writes=writes)
                else:
                    P.op('dve', lambda e: e.tensor_copy(out=out, in_=in_), reads=reads, writes=writes)

            for tb in range(nblk if stage != 'hgrn' else 0):
                own = tb >= ownb
                t0 = tb * NB
                xbt = xb[tb % 2]
                xbst = xbs_[tb % 2]
                xk = 'xb%d' % (tb % 2)
                for dc in range(8):
                    s = xstg[dc % 2]
                    k = 'xstg%d' % (dc % 2)
                    if tb == 0:
                        P.op('pool', lambda e, s=s: e.memset(s[:, 0:1], 0.0), writes=[k])
                        P.dma(s[:, 1:NB + 1], xT[dc * 128:(dc + 1) * 128, 0:NB], writes=[k])
                    else:
                        P.dma(s[:], xT[dc * 128:(dc + 1) * 128, t0 - 1:t0 + NB], writes=[k])
                    P.op('pool', lambda e, s=s, dc=dc, xbt=xbt: e.tensor_copy(out=xbt[:, dc, :], in_=s[:, 1:NB + 1]),
                         reads=[k], writes=[xk])
                    P.op('dve', lambda e, s=s, dc=dc, xbst=xbst: e.tensor_copy(out=xbst[:, dc, :], in_=s[:, 0:NB]),
                         reads=[k], writes=[xk])
                for pc in range(14):
                    pap, pk = pj[pc % 2]
                    for dc in range(8):
                        P.op('pe', lambda e, pap=pap, dc=dc, pc=pc, xbt=xbt: e.matmul(
                            pap, lhsT=wA[:, dc, pc * 128:(pc + 1) * 128], rhs=xbt[:, dc, :], start=(dc == 0), stop=False),
                            reads=['wA', xk], writes=[pk])
                    for dc in range(8):
                        P.op('pe', lambda e, pap=pap, dc=dc, pc=pc, xbst=xbst: e.matmul(
                            pap, lhsT=wB[:, dc, pc * 128:(pc + 1) * 128], rhs=xbst[:, dc, :], start=False, stop=(dc == 7)),
                            reads=['wB', xk], writes=[pk])
                    if pc < 12:
                        nm = ['Rr', 'Kr', 'Vt'][pc // 4]
                        hp = pc % 4
                        if nm == 'Rr' and not own:
                            pass
                        else:
                            evac_copy(FM[nm][hp][:], pap, [pk], ['%s%d' % (nm, hp)])
                    elif pc == 12:
                        P.op('act', lambda e, pap=pap: e.activation(out=LX[0:64, :], in_=pap[0:64, :], func=AF.Tanh),
                             reads=[pk], writes=['LX'])
                        P.op('dve', lambda e, pap=pap: e.tensor_copy(out=LX[64:128, :], in_=pap[64:128, :]),
                             reads=[pk], writes=['LX'])
                    else:
                        if own:
                            P.op('act', lambda e, pap=pap: e.activation(out=SGX[:], in_=pap, func=AF.Sigmoid),
                                 reads=[pk], writes=['SGX'])
                for hp in range(4):
                    Rr, Kr, Vt = FM['Rr'][hp], FM['Kr'][hp], FM['Vt'][hp]
                    At, Bt, Kt, Rt, rk, eG = (FM[n_][hp] for n_ in ['At', 'Bt', 'Kt', 'Rt', 'rk', 'eG'])
                    Rtb = FM['Rtb'][hp]
                    kn = lambda n_: '%s%d' % (n_, hp)
                    plap, plk = pl
                    P.op('pe', lambda e, hp=hp: e.matmul(plap[:, 0:NB], lhsT=LW[:, hp * 128:(hp + 1) * 128], rhs=LX[:],
                                                         start=True, stop=True), reads=['LW', 'LX'], writes=[plk])
                    P.op('pe', lambda e, hp=hp: e.matmul(plap[:, NB:2 * NB], lhsT=LW2[:, hp * 128:(hp + 1) * 128], rhs=LX[:],
                                                         start=True, stop=True), reads=['LW', 'LX'], writes=[plk])
                    sg, a, Gs, Gp, eGn, eGp, kk, sq, rn, kkn, t1, kp = (tmp[n_] for n_ in
                                                                         ['sg', 'a', 'Gs', 'Gp', 'eGn', 'eGp', 'kk', 'sq', 'rn', 'kkn', 't1', 'kp'])
                    P.op('act', lambda e, hp=hp: e.activation(out=sg[:], in_=plap[:, 0:NB], func=AF.Sigmoid, bias=pcol('w0', hp)),
                         reads=[plk, 'pv'], writes=['sg'])
                    P.op('act', lambda e, hp=hp: e.activation(out=a[:], in_=plap[:, NB:2 * NB], func=AF.Sigmoid, bias=pcol('a0', hp)),
                         reads=[plk, 'pv'], writes=['a'])
                    P.op('dve', lambda e: e.tensor_tensor_scan(out=Gs[:], data0=scanmask[:], data1=sg[:], initial=0.0,
                                                               op0=ALU.mult, op1=ALU.add), reads=['scanmask', 'sg'], writes=['Gs'])
                    P.op('pool', lambda e: e.tensor_tensor(out=Gp[:], in0=Gs[:], in1=sg[:], op=ALU.subtract),
                         reads=['Gs', 'sg'], writes=['Gp'])
                    P.op('act', lambda e, eG=eG: e.activation(out=eG[:], in_=Gs[:], func=AF.Exp, scale=-CDEC),
                         reads=['Gs'], writes=[kn('eG')])
                    P.op('act', lambda e: e.activation(out=eGn[:], in_=Gs[:], func=AF.Exp, scale=CDEC),
                         reads=['Gs'], writes=['eGn'])
                    P.op('act', lambda e: e.activation(out=eGp[:], in_=Gp[:], func=AF.Exp, scale=-CDEC),
                         reads=['Gp'], writes=['eGp'])
                    P.op('dve', lambda e, Kr=Kr, hp=hp: e.tensor_scalar(out=kk[:], in0=Kr[:], scalar1=pcol('k_k', hp), scalar2=None,
                                                                        op0=ALU.mult), reads=[kn('Kr'), 'pv'], writes=['kk'])
                    P.op('pool', lambda e: e.tensor_tensor(out=sq[:], in0=kk[:], in1=kk[:], op=ALU.mult), reads=['kk'], writes=['sq'])
                    psap, psk = pss
                    P.op('pe', lambda e: e.matmul(psap, lhsT=onesblk[:], rhs=sq[:], start=True, stop=True),
                         reads=['onesblk', 'sq'], writes=[psk])
                    P.op('act', lambda e: e.activation(out=rn[:], in_=psap, func=AF.Sqrt), reads=[psk], writes=['rn'])
                    P.op('dve', lambda e: e.tensor_scalar(out=rn[:], in0=rn[:], scalar1=1e-12, scalar2=None, op0=ALU.max),
                         reads=['rn'], writes=['rn'])
                    P.op('dve', lambda e: e.reciprocal(out=rn[:], in_=rn[:]), reads=['rn'], writes=['rn'])
                    P.op('dve', lambda e: e.tensor_tensor(out=kkn[:], in0=kk[:], in1=rn[:], op=ALU.mult),
                         reads=['kk', 'rn'], writes=['kkn'])
                    P.op('dve', lambda e, hp=hp: e.tensor_scalar(out=t1[:], in0=a[:], scalar1=-1.0, scalar2=pcol('k_a', hp),
                                                                 op0=ALU.add, op1=ALU.mult), reads=['a', 'pv'], writes=['t1'])
                    P.op('pool', lambda e: e.tensor_scalar(out=t1[:], in0=t1[:], scalar1=1.0, scalar2=None, op0=ALU.add),
                         reads=['t1'], writes=['t1'])
                    P.op('dve', lambda e, Kr=Kr: e.tensor_tensor(out=kp[:], in0=Kr[:], in1=t1[:], op=ALU.mult),
                         reads=[kn('Kr'), 't1'], writes=['kp'])
                    P.op('dve', lambda e, At=At: e.scalar_tensor_tensor(out=At[:], in0=kkn[:], scalar=-1.0, in1=eGp[:],
                                                                         op0=ALU.mult, op1=ALU.mult),
                         reads=['kkn', 'eGp'], writes=[kn('At')])
                    P.op('pool', lambda e: e.tensor_tensor(out=t1[:], in0=kkn[:], in1=a[:], op=ALU.mult),
                         reads=['kkn', 'a'], writes=['t1'])
                    P.op('pool', lambda e, Bt=Bt: e.tensor_tensor(out=Bt[:], in0=t1[:], in1=eGn[:], op=ALU.mult),
                         reads=['t1', 'eGn'], writes=[kn('Bt')])
                    P.op('dve', lambda e, Kt=Kt: e.tensor_tensor(out=Kt[:], in0=kp[:], in1=eGn[:], op=ALU.mult),
                         reads=['kp', 'eGn'], writes=[kn('Kt')])
                    if own:
                        P.op('pool', lambda e, Rt=Rt, Rr=Rr, eG=eG: e.tensor_tensor(out=Rt[:], in0=Rr[:], in1=eG[:], op=ALU.mult),
                             reads=[kn('Rr'), kn('eG')], writes=[kn('Rt')])
                        P.op('pool', lambda e, Rt=Rt, Rtb=Rtb: e.tensor_copy(out=Rtb[:], in_=Rt[:]), reads=[kn('Rt')], writes=[kn('Rtb')])
                        P.op('dve', lambda e, rk=rk, Rr=Rr, hp=hp: e.scalar_tensor_tensor(out=rk[:], in0=Rr[:], scalar=pcol('r_k', hp),
                                                                                           in1=kp[:], op0=ALU.mult, op1=ALU.mult),
                             reads=[kn('Rr'), 'kp', 'pv'], writes=[kn('rk')])
                    par = hp % NPAR
                    yb_i = hp % 2
                    for c in range(NCH):
                        gc = tb * NCH + c
                        cs = slice(c * 64, (c + 1) * 64)
                        Hc, Hn = HH[hp][gc % 2], HH[hp][(gc + 1) % 2]
                        hck, hnk = 'H%d_%d' % (hp, gc % 2), 'H%d_%d' % (hp, (gc + 1) % 2)
                        tmk, mmk = 'TM%d' % par, 'MM%d' % par
                        ptap, ptk = psT[par]
                        for i_, (X, xn) in enumerate([(Vt, 'Vt'), (At, 'At'), (Bt, 'Bt'), (Kt, 'Kt')]):
                            for h2 in range(2):
                                hs = slice(64 * h2, 64 * h2 + 64)
                                P.op('pe', lambda e, X=X, hs=hs, i_=i_, cs=cs, ptap=ptap: e.matmul(
                                    ptap[hs, i_ * 64:(i_ + 1) * 64], lhsT=X[hs, cs], rhs=identb[hs, hs], start=True, stop=True),
                                    reads=[kn(xn), 'identb'], writes=[ptk])
                        evac_copy(TM[par][:].rearrange("p a b -> p (a b)"), ptap, [ptk], [tmk])
                        pmap, pmk = psM[par]
                        prs = [(Bt, At, 'Bt', 'At'), (At, Bt, 'At', 'Bt'), (Kt, At, 'Kt', 'At')]
                        if own:
                            prs += [(Bt, Rtb, 'Bt', 'Rtb'), (Kt, Rtb, 'Kt', 'Rtb')]
                        for i_, (L_, R_, ln, rn_) in enumerate(prs):
                            for h2 in range(2):
                                hs = slice(64 * h2, 64 * h2 + 64)
                                P.op('pe', lambda e, L_=L_, R_=R_, hs=hs, i_=i_, cs=cs, pmap=pmap: e.matmul(
                                    pmap[hs, i_ * 64:(i_ + 1) * 64], lhsT=L_[hs, cs], rhs=R_[hs, cs], start=True, stop=True),
                                    reads=[kn(ln), kn(rn_)], writes=[pmk])
                        ncol = 64 * len(prs)
                        P.op('dve', lambda e, par=par, pmap=pmap, ncol=ncol: e.tensor_tensor(
                            out=MM[par][:, 0:ncol], in0=pmap[:, 0:ncol], in1=mask320[:, 0:ncol], op=ALU.mult),
                            reads=[pmk, 'mask320'], writes=[mmk])
                        ttk = ['TT%d_%d' % (par, i_) for i_ in range(2)]
                        ppk = ['PP%d_%d' % (par, i_) for i_ in range(2)]
                        P.op('pool', lambda e, par=par: e.tensor_tensor(out=TT[par][0][:], in0=MM[par][:, 0:64], in1=iit[:, 0:64], op=ALU.add),
                             reads=[mmk, 'ii'], writes=[ttk[0]])
                        Pcur, Pk = MM[par][:, 0:128], mmk
                        tcur = 0
                        pdap, pdk = psD[par]
                        peap, pek = psE[par]

                        def square(Pcur, Pk, dst, dstk):
                            for h2 in range(2):
                                hs = slice(64 * h2, 64 * h2 + 64)
                                P.op('pe', lambda e, Pcur=Pcur, hs=hs: e.matmul(
                                    pdap[hs, 0:64], lhsT=Pcur[hs, 64:128], rhs=Pcur[hs, 0:64], start=True, stop=True),
                                    reads=[Pk], writes=[pdk])
                                P.op('pe', lambda e, Pcur=Pcur, hs=hs: e.matmul(
                                    pdap[hs, 64:128], lhsT=Pcur[hs, 0:64], rhs=Pcur[hs, 64:128], start=True, stop=True),
                                    reads=[Pk], writes=[pdk])
                            evac_copy(dst[:], pdap, [pdk], [dstk])
                        square(Pcur, Pk, PP[par][0], ppk[0])
                        Pcur, Pk = PP[par][0][:], ppk[0]
                        for lvl in range(1, 6):
                            Tc = TT[par][tcur]
                            Tn = TT[par][1 - tcur]
                            for h2 in range(2):
                                hs = slice(64 * h2, 64 * h2 + 64)
                                P.op('pe', lambda e, Tc=Tc, Pcur=Pcur, hs=hs: e.matmul(
                                    peap[hs, 0:64], lhsT=Pcur[hs, 64:128], rhs=Tc[hs, :], start=True, stop=True),
                                    reads=[ttk[tcur], Pk], writes=[pek])
                            if lvl < 5:
                                square(Pcur, Pk, PP[par][lvl % 2], ppk[lvl % 2])
                            P.op('dve', lambda e, Tc=Tc, Tn=Tn: e.tensor_tensor(out=Tn[:], in0=peap[:, 0:64], in1=Tc[:], op=ALU.add),
                                 reads=[pek, ttk[tcur]], writes=[ttk[1 - tcur]])
                            if lvl < 5:
                                Pcur, Pk = PP[par][lvl % 2][:], ppk[lvl % 2]
                            tcur = 1 - tcur
                        Tt = TT[par][tcur]
                        tk = ttk[tcur]
                        pxap, pxk = psX[par]
                        for h2 in range(2):
                            hs = slice(64 * h2, 64 * h2 + 64)
                            P.op('pe', lambda e, hs=hs, par=par, pxap=pxap: e.matmul(
                                pxap[hs, :], lhsT=MM[par][hs, 128:192], rhs=TM[par][hs, 0, :], start=True, stop=True),
                                reads=[mmk, tmk], writes=[pxk])
                        evac_copy(X2s[par][:], pxap, [pxk], ['X2s%d' % par])
                        pwap, pwk = psW[par]
                        for h2 in range(2):
                            hs = slice(64 * h2, 64 * h2 + 64)
                            P.op('pe', lambda e, hs=hs, par=par, Tt=Tt, pwap=pwap: e.matmul(
                                pwap[hs, 0:64], lhsT=Tt[hs, :], rhs=X2s[par][hs, :], start=True, stop=True),
                                reads=[tk, 'X2s%d' % par], writes=[pwk])
                            P.op('pe', lambda e, hs=hs, par=par, Tt=Tt, pwap=pwap: e.matmul(
                                pwap[hs, 64:128], lhsT=TM[par][hs, 1, :], rhs=Tt[hs, :], start=True, stop=True),
                                reads=[tk, tmk], writes=[pwk])
                            P.op('pe', lambda e, hs=hs, par=par, pwap=pwap: e.matmul(
                                pwap[hs, 128:192], lhsT=TM[par][hs, 3, :], rhs=TM[par][hs, 0, :], start=True, stop=True),
                                reads=[tmk], writes=[pwk])
                        wwk = 'WW%d' % par
                        evac_copy(WW[par][:], pwap[:, 0:128], [pwk], [wwk])
                        gam = eG[:, c * 64 + 63:c * 64 + 64]
                        P.op('dve', lambda e, par=par, gam=gam, pwap=pwap: e.tensor_scalar(out=Dg[par][:], in0=pwap[:, 128:192], scalar1=gam,
                                                                                            scalar2=None, op0=ALU.mult),
                             reads=[pwk, kn('eG')], writes=['Dg%d' % par])
                        P.op('dve', lambda e, par=par, gam=gam, Hc=Hc: e.scalar_tensor_tensor(
                            out=HDg[par][:], in0=Hc[:], scalar=gam, in1=Dg[par][:], op0=ALU.mult, op1=ALU.add),
                            reads=[hck, kn('eG'), 'Dg%d' % par], writes=['HDg%d' % par])
                        puap, puk = psU[par]
                        for h2 in range(2):
                            hs = slice(64 * h2, 64 * h2 + 64)
                            P.op('pe', lambda e, hs=hs, par=par, Hc=Hc, puap=puap: e.matmul(
                                puap[hs, :], lhsT=WW[par][hs, 64:128], rhs=Hc[hs, :], start=True, stop=True),
                                reads=[wwk, hck], writes=[puk])
                        P.op('dve', lambda e, par=par, puap=puap: e.tensor_tensor(out=Us[par][:], in0=puap, in1=WW[par][:, 0:64], op=ALU.add),
                             reads=[puk, wwk], writes=['Us%d' % par])
                        if own:
                            pyap, pyk = psY[yb_i]
                            for h2 in range(2):
                                hs = slice(64 * h2, 64 * h2 + 64)
                                P.op('pe', lambda e, hs=hs, Rt=Rt, cs=cs, Hc=Hc, pyap=pyap: e.matmul(
                                    pyap[hs, cs], lhsT=Rt[hs, cs], rhs=Hc[hs, :], start=True, stop=False),
                                    reads=[kn('Rt'), hck], writes=[pyk])
                                P.op('pe', lambda e, hs=hs, par=par, cs=cs, pyap=pyap: e.matmul(
                                    pyap[hs, cs], lhsT=MM[par][hs, 192:256], rhs=Us[par][hs, :], start=False, stop=False),
                                    reads=[mmk, 'Us%d' % par], writes=[pyk])
                                P.op('pe', lambda e, hs=hs, par=par, cs=cs, pyap=pyap: e.matmul(
                                    pyap[hs, cs], lhsT=MM[par][hs, 256:320], rhs=TM[par][hs, 0, :], start=False, stop=True),
                                    reads=[mmk, tmk], writes=[pyk])
                            pcap, pck = pcf[yb_i]
                            pgap, pgk = pgate[yb_i]
                            for h2 in range(2):
                                hs = slice(64 * h2, 64 * h2 + 64)
                                P.op('pe', lambda e, hs=hs, rk=rk, cs=cs, c=c, pcap=pcap: e.matmul(
                                    pcap[hs, c:c + 1], lhsT=rk[hs, cs], rhs=onesblk[hs, 64 * (hs.start // 64):64 * (hs.start // 64) + 1],
                                    start=True, stop=True), reads=[kn('rk'), 'onesblk'], writes=[pck])
                                h = 2 * hp + h2
                                P.op('pe', lambda e, hs=hs, cs=cs, h=h, pgap=pgap: e.matmul(
                                    pgap[hs, cs], lhsT=SGX[:, cs], rhs=GU[:, h * 64:(h + 1) * 64], start=True, stop=True),
                                    reads=['SGX', 'GU'], writes=[pgk])
                            P.op('pool', lambda e, par=par, c=c, yb_i=yb_i: e.tensor_copy(out=Yc[yb_i][:, c, :], in_=TM[par][:, 0, :]),
                                 reads=[tmk], writes=['Yc%d' % yb_i])
                        phap, phk = psH[par]
                        for h2 in range(2):
                            hs = slice(64 * h2, 64 * h2 + 64)
                            P.op('pe', lambda e, hs=hs, par=par, phap=phap: e.matmul(
                                phap[hs, :], lhsT=TM[par][hs, 2, :], rhs=Us[par][hs, :], start=True, stop=True),
                                reads=[tmk, 'Us%d' % par], writes=[phk])
                        P.op('dve', lambda e, par=par, gam=gam, Hn=Hn, phap=phap: e.scalar_tensor_tensor(
                            out=Hn[:], in0=phap, scalar=gam, in1=HDg[par][:], op0=ALU.mult, op1=ALU.add),
                            reads=[phk, kn('eG'), 'HDg%d' % par], writes=[hnk])
                    if own:
                        pyap, pyk = psY[yb_i]
                        pcap, pck = pcf[yb_i]
                        pgap, pgk = pgate[yb_i]
                        Y = Yb[yb_i]
                        yk = 'Yb%d' % yb_i
                        V3 = Yc[yb_i]
                        vk = 'Yc%d' % yb_i
                        S = st[yb_i]
                        sk = 'st%d' % yb_i
                        y3 = lambda ap: ap.rearrange("p (c v) -> p c v", v=64)
                        bc = lambda ap: ap.unsqueeze(2).to_broadcast([128, NCH, 64])
                        evac_copy(Y[:].rearrange("p c v -> p (c v)"), pyap, [pyk], [yk])
                        P.op('dve', lambda e, Y=Y, S=S: e.tensor_reduce(out=S[:, 0:NCH], in_=Y[:], axis=AX.X, op=ALU.add),
                             reads=[yk], writes=[sk])
                        P.op('dve', lambda e, S=S: e.tensor_scalar(out=S[:, 0:NCH], in0=S[:, 0:NCH], scalar1=1.0 / 64, scalar2=None, op0=ALU.mult),
                             reads=[sk], writes=[sk])
                        P.op('dve', lambda e, Y=Y, S=S: e.tensor_tensor(out=Y[:], in0=Y[:], in1=bc(S[:, 0:NCH]), op=ALU.subtract),
                             reads=[yk, sk], writes=[yk])
                        sqb = tmp['sq']
                        P.op('pool', lambda e, Y=Y: e.tensor_tensor(out=y3(sqb[:]), in0=Y[:], in1=Y[:], op=ALU.mult),
                             reads=[yk], writes=['sq'])
                        P.op('dve', lambda e, S=S: e.tensor_reduce(out=S[:, NCH:2 * NCH], in_=y3(sqb[:]), axis=AX.X, op=ALU.add),
                             reads=['sq'], writes=[sk])
                        P.op('dve', lambda e, S=S: e.tensor_scalar(out=S[:, NCH:2 * NCH], in0=S[:, NCH:2 * NCH], scalar1=1.0 / 64, scalar2=64e-5,
                                                                   op0=ALU.mult, op1=ALU.add), reads=[sk], writes=[sk])
                        P.op('act', lambda e, S=S: e.activation(out=S[:, NCH:2 * NCH], in_=S[:, NCH:2 * NCH], func=AF.Sqrt),
                             reads=[sk], writes=[sk])
                        P.op('dve', lambda e, S=S: e.reciprocal(out=S[:, NCH:2 * NCH], in_=S[:, NCH:2 * NCH]), reads=[sk], writes=[sk])
                        P.op('dve', lambda e, Y=Y, S=S: e.tensor_tensor(out=Y[:], in0=Y[:], in1=bc(S[:, NCH:2 * NCH]), op=ALU.mult),
                             reads=[yk, sk], writes=[yk])
                        gw = gnw[:, hp, :].unsqueeze(1).to_broadcast([128, NCH, 64])
                        gb = gnb[:, hp, :].unsqueeze(1).to_broadcast([128, NCH, 64])
                        P.op('pool', lambda e, Y=Y, gw=gw: e.tensor_tensor(out=Y[:], in0=Y[:], in1=gw, op=ALU.mult),
                             reads=[yk, 'gnw'], writes=[yk])
                        P.op('pool', lambda e, Y=Y, gb=gb: e.tensor_tensor(out=Y[:], in0=Y[:], in1=gb, op=ALU.add),
                             reads=[yk, 'gnb'], writes=[yk])
                        cf = cfs[yb_i]
                        ck = 'cfs%d' % yb_i
                        evac_copy(cf[:], pcap, [pck], [ck])
                        P.op('dve', lambda e, V3=V3, cf=cf: e.tensor_tensor(out=V3[:], in0=V3[:], in1=bc(cf[:]), op=ALU.mult),
                             reads=[vk, ck], writes=[vk])
                        P.op('pool', lambda e, Y=Y, V3=V3: e.tensor_tensor(out=Y[:], in0=Y[:], in1=V3[:], op=ALU.add),
                             reads=[yk, vk], writes=[yk])
                        P.op('dve', lambda e, Y=Y, pgap=pgap: e.tensor_tensor(out=Y[:].rearrange("p c v -> p (c v)"),
                                                                              in0=Y[:].rearrange("p c v -> p (c v)"), in1=pgap, op=ALU.mult),
                             reads=[yk, pgk], writes=[yk])
                        ob = tb - ownb
                        if stage == 'rwkv':
                            for c in range(NCH):
                                P.dma(ya_dbg[ob * NCH + c, hp], Y[:, c, :], reads=[yk])
                        ptap2, ptk2 = pyt[yb_i]
                        for c in range(NCH):
                            for h2 in range(2):
                                hs = slice(64 * h2, 64 * h2 + 64)
                                P.op('pe', lambda e, Y=Y, hs=hs, c=c, ptap2=ptap2: e.matmul(
                                    ptap2[hs, c * 64:(c + 1) * 64], lhsT=Y[hs, c, :], rhs=ident[hs, hs], start=True, stop=True),
                                    reads=[yk, 'ident'], writes=[ptk2])
                        yT = yaTs[yb_i]
                        ytk = 'yaTs%d' % yb_i
                        evac_copy(yT[:], ptap2, [ptk2], [ytk])
                        P.dma(yaT_d[hp * 128:(hp + 1) * 128, ob * NB:(ob + 1) * NB], yT[:], reads=[ytk], writes=['dram_yaT_%d_%d' % (hp, ob)])
            P.flush()
        if stage in ('hgrn', 'full', 'fullA'):
          P.barrier()
          with ExitStack() as es:
            PSA.reset()
            scanmask_h = P.sb(es, 'scanmask_h', [128, NB])
            iu2 = P.sb(es, 'iu2', [128, 64])
            P.dma(scanmask_h[:], scanmask_d, writes=['scanmask_h'])
            P.dma(iu2[:], iu2_d, writes=['iu2'])
            wH = P.sb(es, 'wH', [128, 8, 2048], BF16)
            NWB = P.sb(es, 'NWB', [128, 512])
            o_, n_ = ROWS['hg_nw']
            P.dma(NWB[:], rows_d[:, o_:o_ + n_].partition_broadcast(128), writes=['NWB'])
            lbt = P.sb(es, 'lbt', [128, 12])
            P.op('dve', lambda e: e.tensor_tensor(out=lbt[:, 0:4], in0=pvt[:, PV['lb0']:PV['lb0'] + 4],
                                                  in1=pvt[:, PV['lb1']:PV['lb1'] + 4], op=ALU.subtract),
                 reads=['pv'], writes=['lbt'])
            P.op('act', lambda e: e.activation(out=lbt[:, 0:4], in_=lbt[:, 0:4], func=AF.Sigmoid), reads=['lbt'], writes=['lbt'])
            P.op('dve', lambda e: e.tensor_scalar(out=lbt[:, 4:8], in0=lbt[:, 0:4], scalar1=-1.0, scalar2=1.0, op0=ALU.mult, op1=ALU.add),
                 reads=['lbt'], writes=['lbt'])
            P.op('dve', lambda e: e.tensor_scalar(out=lbt[:, 8:12], in0=lbt[:, 4:8], scalar1=-1.0, scalar2=None, op0=ALU.mult),
                 reads=['lbt'], writes=['lbt'])
            with ExitStack() as es1:
                stg = [P.sb(es1, 'whstg', [128, 2048]) for _ in range(2)]
                for dc in range(8):
                    s = stg[dc % 2]
                    k = 'whstg%d' % (dc % 2)
                    P.dma(s[:], w_in[dc * 128:(dc + 1) * 128, C_Q:C_GATE], writes=[k])
                    P.op('dve' if dc % 2 else 'pool', lambda e, s=s, dc=dc: e.tensor_copy(out=wH[:, dc, :], in_=s[:]),
                         reads=[k], writes=['wH'])
                P.flush()
            P.barrier()
            xstg = [P.sb(es, 'hxstg', [128, NB]) for _ in range(2)]
            xb = [P.sb(es, 'hxb', [128, 8, NB], BF16) for _ in range(2)]
            NSET = 2
            fm = [{nm: P.sb(es, 'h' + nm, [128, NB]) for nm in ['q', 'sf', 'lf', 'k', 'b', 'eb', 'enb', 'qt', 'kt']} for _ in range(NSET)]
            Vtok = [[P.sb(es, 'Vtok', [128, 512]) for _ in range(NB // 128)] for _ in range(2)]
            SGo = [P.sb(es, 'SGo', [128, 512]) for _ in range(NB // 128)]
            KTs = [P.sb(es, 'KTs', [128, 128]) for _ in range(2)]
            ATs = [P.sb(es, 'ATs', [128, 64]) for _ in range(2)]
            Dgs = [[P.sb(es, 'hDg', [128, 128]) for _ in range(2)] for _ in range(2)]
            SS = [[P.sb(es, 'hS', [128, 128]) for _ in range(2)] for _ in range(4)]
            for h in range(4):
                P.op('pool', lambda e, h=h: e.memset(SS[h][0][:], 0.0), writes=['hS%d_0' % h])
            Ot = P.sb(es, 'Ot', [128, 4, 128])
            Osq = P.sb(es, 'Osq', [128, 4, 128])
            ost = P.sb(es, 'ost', [128, 8])
            ybTs = P.sb(es, 'ybTs', [128, 4, 128], BF16)
            pj = [PSA.alloc(NB, 0), PSA.alloc(NB, 1)]
            pv_ = PSA.alloc(512, 2)
            pog = PSA.alloc(512, 3)
            pK = PSA.alloc(128, 4)
            pA = PSA.alloc(64, 4)
            pD = PSA.alloc(256, 5)
            pO = [PSA.alloc(512, 6), PSA.alloc(512, 7)]
            pT = pv_
            evq2 = [0]

            def evac2(out, in_, reads, writes):
                evq2[0] += 1
                if evq2[0] % 2:
                    P.op('act', lambda e: e.copy(out=out, in_=in_), reads=reads, writes=writes)
                else:
                    P.op('dve', lambda e: e.tensor_copy(out=out, in_=in_), reads=reads, writes=writes)

            for tb in range(nblk):
                own = tb >= ownb
                ob = tb - ownb
                t0 = tb * NB
                xbt = xb[tb % 2]
                xk = 'hxb%d' % (tb % 2)
                for dc in range(8):
                    s = xstg[dc % 2]
                    k = 'hxstg%d' % (dc % 2)
                    P.dma(s[:], xT[dc * 128:(dc + 1) * 128, t0:t0 + NB], writes=[k])
                    P.op('pool' if dc % 2 else 'dve', lambda e, s=s, dc=dc, xbt=xbt: e.tensor_copy(out=xbt[:, dc, :], in_=s[:]),
                         reads=[k], writes=[xk])
                vt = Vtok[tb % 2]
                for tt in range(NB // 128):
                    vk = 'Vtok%d_%d' % (tb % 2, tt)
                    pap, pk = pv_
                    for dc in range(8):
                        P.op('pe', lambda e, pap=pap, dc=dc, tt=tt, xbt=xbt: e.matmul(
                            pap, lhsT=xbt[:, dc, tt * 128:(tt + 1) * 128], rhs=wH[:, dc, 1024:1536], start=(dc == 0), stop=(dc == 7)),
                            reads=['wH', xk], writes=[pk])
                    evac2(vt[tt][:], pap, [pk], [vk])
                    if own:
                        pap, pk = pog
                        for dc in range(8):
                            P.op('pe', lambda e, pap=pap, dc=dc, tt=tt, xbt=xbt: e.matmul(
                                pap, lhsT=xbt[:, dc, tt * 128:(tt + 1) * 128], rhs=wH[:, dc, 1536:2048], start=(dc == 0), stop=(dc == 7)),
                                reads=['wH', xk], writes=[pk])
                        P.op('act', lambda e, pap=pap, tt=tt: e.activation(out=SGo[tt][:], in_=pap, func=AF.Sigmoid),
                             reads=[pk], writes=['SGo%d' % tt])
                for h in range(4):
                    F = fm[h % NSET]
                    fk = lambda n_: 'h%s%d' % (n_, h % NSET)
                    pap, pk = pj[0]
                    if own:
                        for dc in range(8):
                            P.op('pe', lambda e, pap=pap, dc=dc, h=h, xbt=xbt: e.matmul(
                                pap, lhsT=wH[:, dc, h * 128:(h + 1) * 128], rhs=xbt[:, dc, :], start=(dc == 0), stop=(dc == 7)),
                                reads=['wH', xk], writes=[pk])
                        P.op('act', lambda e, pap=pap, F=F: e.activation(out=F['q'][:], in_=pap, func=AF.Silu), reads=[pk], writes=[fk('q')])
                    pap, pk = pj[1]
                    for dc in range(8):
                        P.op('pe', lambda e, pap=pap, dc=dc, h=h, xbt=xbt: e.matmul(
                            pap, lhsT=wH[:, dc, 512 + h * 128:512 + (h + 1) * 128], rhs=xbt[:, dc, :], start=(dc == 0), stop=(dc == 7)),
                            reads=['wH', xk], writes=[pk])
                    P.op('act', lambda e, pap=pap, F=F: e.activation(out=F['sf'][:], in_=pap, func=AF.Sigmoid), reads=[pk], writes=[fk('sf')])
                    P.op('act', lambda e, F=F, h=h: e.activation(out=F['lf'][:], in_=F['sf'][:], func=AF.Ln, bias=lbt[:, h:h + 1],
                                                                 scale=lbt[:, 4 + h:5 + h]), reads=[fk('sf'), 'lbt'], writes=[fk('lf')])
                    P.op('dve', lambda e, F=F, h=h: e.tensor_scalar(out=F['k'][:], in0=F['sf'][:], scalar1=lbt[:, 8 + h:9 + h],
                                                                    scalar2=lbt[:, 4 + h:5 + h], op0=ALU.mult, op1=ALU.add),
                         reads=[fk('sf'), 'lbt'], writes=[fk('k')])
                    P.op('dve', lambda e, F=F: e.tensor_tensor_scan(out=F['b'][:], data0=scanmask_h[:], data1=F['lf'][:], initial=0.0,
                                                                    op0=ALU.mult, op1=ALU.add), reads=['scanmask_h', fk('lf')], writes=[fk('b')])
                    P.op('act', lambda e, F=F: e.activation(out=F['eb'][:], in_=F['b'][:], func=AF.Exp), reads=[fk('b')], writes=[fk('eb')])
                    P.op('act', lambda e, F=F: e.activation(out=F['enb'][:], in_=F['b'][:], func=AF.Exp, scale=-1.0),
                         reads=[fk('b')], writes=[fk('enb')])
                    P.op('pool', lambda e, F=F: e.tensor_tensor(out=F['kt'][:], in0=F['k'][:], in1=F['enb'][:], op=ALU.mult),
                         reads=[fk('k'), fk('enb')], writes=[fk('kt')])
                    if own:
                        P.op('pool', lambda e, F=F: e.tensor_tensor(out=F['qt'][:], in0=F['q'][:], in1=F['eb'][:], op=ALU.mult),
                             reads=[fk('q'), fk('eb')], writes=[fk('qt')])
                    for cp in range(NB // 128):
                        vk = 'Vtok%d_%d' % (tb % 2, cp)
                        for c2 in range(2):
                            c = 2 * cp + c2
                            gc = tb * NCH + c
                            cs = slice(c * 64, (c + 1) * 64)
                            ps_ = slice(64 * c2, 64 * c2 + 64)
                            Sc, Sn = SS[h][gc % 2], SS[h][(gc + 1) % 2]
                            sck, snk = 'hS%d_%d' % (h, gc % 2), 'hS%d_%d' % (h, (gc + 1) % 2)
                            kts, ktk = KTs[c2], 'KTs%d' % c2
                            ats, atk = ATs[c2], 'ATs%d' % c2
                            dgs, dgk = Dgs[h % 2][c2], 'hDg%d_%d' % (h % 2, c2)
                            pkap, pkk = pK
                            P.op('pe', lambda e, F=F, cs=cs, ps_=ps_, pkap=pkap: e.matmul(
                                pkap[ps_, :], lhsT=F['kt'][:, cs], rhs=ident[:], start=True, stop=True),
                                reads=[fk('kt'), 'ident'], writes=[pkk])
                            evac2(kts[ps_, :], pkap[ps_, :], [pkk], [ktk])
                            if own:
                                paap, pak = pA
                                P.op('pe', lambda e, F=F, cs=cs, ps_=ps_, paap=paap: e.matmul(
                                    paap[ps_, :], lhsT=F['kt'][:, cs], rhs=F['qt'][:, cs], start=True, stop=True),
                                    reads=[fk('kt'), fk('qt')], writes=[pak])
                                P.op('dve', lambda e, ats=ats, ps_=ps_, paap=paap: e.tensor_tensor(
                                    out=ats[ps_, :], in0=paap[ps_, :], in1=iu2[ps_, :], op=ALU.mult),
                                    reads=[pak, 'iu2'], writes=[atk])
                            pdap, pdk = pD
                            P.op('pe', lambda e, kts=kts, ps_=ps_, cp=cp, h=h, c2=c2, pdap=pdap, vt=vt: e.matmul(
                                pdap[:, c2 * 128:(c2 + 1) * 128], lhsT=kts[ps_, :], rhs=vt[cp][ps_, h * 128:(h + 1) * 128],
                                start=True, stop=True), reads=[ktk, vk], writes=[pdk])
                            gl = F['eb'][:, c * 64 + 63:c * 64 + 64]
                            P.op('dve', lambda e, dgs=dgs, pdap=pdap, c2=c2, gl=gl: e.tensor_scalar(
                                out=dgs[:], in0=pdap[:, c2 * 128:(c2 + 1) * 128], scalar1=gl, scalar2=None, op0=ALU.mult),
                                reads=[pdk, fk('eb')], writes=[dgk])
                            if own:
                                poap, pok = pO[cp]
                                P.op('pe', lambda e, ats=ats, ps_=ps_, cp=cp, h=h, poap=poap, vt=vt: e.matmul(
                                    poap[ps_, h * 128:(h + 1) * 128], lhsT=ats[ps_, :], rhs=vt[cp][ps_, h * 128:(h + 1) * 128],
                                    start=True, stop=False), reads=[atk, vk], writes=[pok])
                                P.op('pe', lambda e, F=F, cs=cs, ps_=ps_, h=h, Sc=Sc, poap=poap: e.matmul(
                                    poap[ps_, h * 128:(h + 1) * 128], lhsT=F['qt'][:, cs], rhs=Sc[:], start=False, stop=True),
                                    reads=[fk('qt'), sck], writes=[pok])
                            P.op('dve', lambda e, Sc=Sc, Sn=Sn, gl=gl, dgs=dgs: e.scalar_tensor_tensor(
                                out=Sn[:], in0=Sc[:], scalar=gl, in1=dgs[:], op0=ALU.mult, op1=ALU.add),
                                reads=[sck, fk('eb'), dgk], writes=[snk])
                if own:
                    for cp in range(NB // 128):
                        poap, pok = pO[cp]
                        evac2(Ot[:].rearrange("p h v -> p (h v)"), poap, [pok], ['Ot'])
                        P.op('pool', lambda e: e.tensor_tensor(out=Osq[:], in0=Ot[:], in1=Ot[:], op=ALU.mult), reads=['Ot'], writes=['Osq'])
                        P.op('dve', lambda e: e.tensor_reduce(out=ost[:, 0:4], in_=Osq[:], axis=AX.X, op=ALU.add), reads=['Osq'], writes=['ost'])
                        P.op('dve', lambda e: e.tensor_scalar(out=ost[:, 0:4], in0=ost[:, 0:4], scalar1=1.0 / 128, scalar2=1e-6,
                                                              op0=ALU.mult, op1=ALU.add), reads=['ost'], writes=['ost'])
                        P.op('act', lambda e: e.activation(out=ost[:, 0:4], in_=ost[:, 0:4], func=AF.Sqrt), reads=['ost'], writes=['ost'])
                        P.op('dve', lambda e: e.reciprocal(out=ost[:, 0:4], in_=ost[:, 0:4]), reads=['ost'], writes=['ost'])
                        P.op('dve', lambda e: e.tensor_tensor(out=Ot[:], in0=Ot[:], in1=ost[:, 0:4].unsqueeze(2).to_broadcast([128, 4, 128]),
                                                              op=ALU.mult), reads=['Ot', 'ost'], writes=['Ot'])
                        P.op('pool', lambda e: e.tensor_tensor(out=Ot[:].rearrange("p h v -> p (h v)"), in0=Ot[:].rearrange("p h v -> p (h v)"),
                                                               in1=NWB[:], op=ALU.mult), reads=['Ot', 'NWB'], writes=['Ot'])
                        P.op('dve', lambda e, cp=cp: e.tensor_tensor(out=Ot[:].rearrange("p h v -> p (h v)"), in0=Ot[:].rearrange("p h v -> p (h v)"),
                                                                     in1=SGo[cp][:], op=ALU.mult), reads=['Ot', 'SGo%d' % cp], writes=['Ot'])
                        tok0 = ob * NB + cp * 128
                        if stage == 'hgrn':
                            P.dma(yb_dbg[tok0:tok0 + 128, :], Ot[:].rearrange("p h v -> p (h v)"), reads=['Ot'])
                        ptap, ptk = pT
                        for h in range(4):
                            P.op('pe', lambda e, h=h, ptap=ptap: e.matmul(ptap[:, h * 128:(h + 1) * 128], lhsT=Ot[:, h, :], rhs=ident[:],
                                                                         start=True, stop=True), reads=['Ot', 'ident'], writes=[ptk])
                        evac2(ybTs[:].rearrange("p h t -> p (h t)"), ptap, [ptk], ['ybTs'])
                        P.dma(ybT_d.rearrange("(h p) t -> p h t", p=128)[:, :, tok0:tok0 + 128], ybTs[:], reads=['ybTs'],
                              writes=['dram_ybT_%d' % (tok0 // 128)])
            P.flush()
        if stage == 'fullA':
            with ExitStack() as es:
                P.barrier()
                bt = P.sb(es, 'bt', [128, 4, TOWN], BF16)
                for src, dst, kp_ in [(yaT_d, yaT_o, 'a'), (ybT_d, ybT_o, 'b')]:
                    rk_ = (['dram_yaT_%d_%d' % (hp_, o__) for hp_ in range(4) for o__ in range(nblk - ownb)] if kp_ == 'a'
                           else ['dram_ybT_%d' % j_ for j_ in range(TOWN // 128)])
                    P.dma(bt[:], src.rearrange("(c p) t -> p c t", p=128), reads=rk_, writes=['bt'])
                    P.dma(dst.rearrange("(c p) t -> p c t", p=128), bt[:], reads=['bt'], writes=['dram_o' + kp_])
                P.flush()
        if stage == 'full':
          NT_H = 1024
          own_off = T - TOWN
          with ExitStack() as esB:
            P.barrier()
            PSA.reset()
            ACC = P.sb(esB, 'ACC', [128, NT_H // 128, D])
            x1T = P.sb(esB, 'x1T', [128, 8, NT_H], BF16)
            WtAll = P.sb(esB, 'WtAll', [128, NT_H // 128, 32])
            LNB = P.sb(esB, 'LNB', [128, 2, D])

            def load_ln(names):
                for i_, nm in enumerate(names):
                    o_, n_ = ROWS[nm]
                    P.dma(LNB[:, i_, :], rows_d[:, o_:o_ + n_].partition_broadcast(128), writes=['LNB'])
            RBB = P.sb(esB, 'RBB', [128, 36])
            o_, n_ = ROWS['rb']
            P.dma(RBB[:], rows_d[:, o_:o_ + n_].partition_broadcast(128), writes=['RBB'])
            evq3 = [0]

            def evac3(out, in_, reads, writes):
                evq3[0] += 1
                if evq3[0] % 2:
                    P.op('act', lambda e: e.copy(out=out, in_=in_), reads=reads, writes=writes)
                else:
                    P.op('dve', lambda e: e.tensor_copy(out=out, in_=in_), reads=reads, writes=writes)

            def load_cast(es_, dst, dkey, src_rows, ncols, nchunk, col0=0, cast_engs=('dve', 'pool')):
                stg_ = [P.sb(es_, 'lcs', [128, 1024]) for _ in range(2)]
                n_ = 0
                for c in range(nchunk):
                    for c1 in range(0, ncols, 1024):
                        s_ = stg_[n_ % 2]
                        k_ = 'lcs%d' % (n_ % 2)
                        P.dma(s_[:], src_rows(c)[:, col0 + c1:col0 + c1 + 1024], writes=[k_])
                        P.op(cast_engs[n_ % len(cast_engs)], lambda e, s_=s_, c=c, c1=c1: e.tensor_copy(out=dst[:, c, c1:c1 + 1024], in_=s_[:]),
                             reads=[k_], writes=[dkey])
                        n_ += 1

            def layer_norm_tile(Zt, zk, gi, stt, sq_t):
                P.op('dve', lambda e: e.tensor_reduce(out=stt[:, 0:1], in_=Zt[:], axis=AX.X, op=ALU.add), reads=[zk], writes=['stt'])
                P.op('dve', lambda e: e.tensor_scalar(out=stt[:, 0:1], in0=stt[:, 0:1], scalar1=1.0 / D, scalar2=None, op0=ALU.mult),
                     reads=['stt'], writes=['stt'])
                P.op('dve', lambda e: e.tensor_scalar(out=Zt[:], in0=Zt[:], scalar1=stt[:, 0:1], scalar2=None, op0=ALU.subtract),
                     reads=[zk, 'stt'], writes=[zk])
                P.op('pool', lambda e: e.tensor_tensor(out=sq_t[:], in0=Zt[:], in1=Zt[:], op=ALU.mult), reads=[zk], writes=['sq_t'])
                P.op('dve', lambda e: e.tensor_reduce(out=stt[:, 1:2], in_=sq_t[:], axis=AX.X, op=ALU.add), reads=['sq_t'], writes=['stt'])
                P.op('dve', lambda e: e.tensor_scalar(out=stt[:, 1:2], in0=stt[:, 1:2], scalar1=1.0 / D, scalar2=1e-5, op0=ALU.mult, op1=ALU.add),
                     reads=['stt'], writes=['stt'])
                P.op('act', lambda e: e.activation(out=stt[:, 1:2], in_=stt[:, 1:2], func=AF.Sqrt), reads=['stt'], writes=['stt'])
                P.op('dve', lambda e: e.reciprocal(out=stt[:, 1:2], in_=stt[:, 1:2]), reads=['stt'], writes=['stt'])
                P.op('dve', lambda e: e.tensor_scalar(out=Zt[:], in0=Zt[:], scalar1=stt[:, 1:2], scalar2=None, op0=ALU.mult),
                     reads=[zk, 'stt'], writes=[zk])
                P.op('pool', lambda e: e.tensor_tensor(out=Zt[:], in0=Zt[:], in1=LNB[:, 0, :], op=ALU.mult), reads=[zk, 'LNB'], writes=[zk])
                P.op('dve', lambda e: e.tensor_tensor(out=Zt[:], in0=Zt[:], in1=LNB[:, 1, :], op=ALU.add), reads=[zk, 'LNB'], writes=[zk])

            def do_half(hf):
                tk0 = hf * NT_H
                with ExitStack() as es:
                    P.barrier()
                    PSA.reset()
                    WA = P.sb(es, 'WA', [128, 4, D], BF16)
                    WB = P.sb(es, 'WB', [128, 4, D], BF16)
                    WG = P.sb(es, 'WG', [128, 8, 2048], BF16)
                    WO = P.sb(es, 'WO', [128, 8, D], BF16)
                    RW = P.sb(es, 'RW', [128, 8, 36])
                    load_ln(['ln1_g', 'ln1_b'])
                    with ExitStack() as es1:
                        load_cast(es1, WA, 'WA', lambda c: w_a_out_d[c * 128:(c + 1) * 128, :], D, 4)
                        load_cast(es1, WB, 'WB', lambda c: w_b_out_d[c * 128:(c + 1) * 128, :], D, 4)
                        load_cast(es1, WO, 'WO', lambda c: w_o_d[c * 128:(c + 1) * 128, :], D, 8)
                        load_cast(es1, WG, 'WG', lambda c: w_in[c * 128:(c + 1) * 128, :], 2048, 8, col0=C_GATE)
                        for c in range(8):
                            P.dma(RW[:, c, :], rw_d[c * 128:(c + 1) * 128, :], writes=['RW'])
                        P.flush()
                    P.barrier()
                    yaTb = P.sb(es, 'yaTb', [128, 4, 512], BF16)
                    ybTb = P.sb(es, 'ybTb', [128, 4, 512], BF16)
                    xgs = [P.sb(es, 'xgs', [128, 512]) for _ in range(2)]
                    xgb = P.sb(es, 'xgb', [128, 8, 512], BF16)
                    sga = P.sb(es, 'sga', [128, 512])
                    sgb = P.sb(es, 'sgb', [128, 512])
                    m1 = P.sb(es, 'm1', [128, 512])
                    m2 = P.sb(es, 'm2', [128, 512])
                    mT = P.sb(es, 'mT', [128, 8, 512], BF16)
                    xo_t = P.sb(es, 'xo_t', [128, D])
                    Zt = P.sb(es, 'Zt', [128, D])
                    sq_t = P.sb(es, 'sq_t', [128, D])
                    stt = P.sb(es, 'stt', [128, 2])
                    x1Tf = P.sb(es, 'x1Tf', [128, 8, 128])
                    Lg = P.sb(es, 'Lg', [128, 36])
                    rt = P.sb(es, 'rt', [128, 64])
                    psA = PSA.alloc(512, 0)
                    psB = PSA.alloc(512, 1)
                    psGa = PSA.alloc(512, 2)
                    psGb = PSA.alloc(512, 3)
                    psO = [PSA.alloc(512, 4), PSA.alloc(512, 5)]
                    psTr = [PSA.alloc(512, 6), PSA.alloc(512, 7)]
                    psR = psA
                    for tbk in range(NT_H // 512):
                        tb0 = tk0 + tbk * 512
                        P.dma(yaTb[:], yaT_d.rearrange("(c p) t -> p c t", p=128)[:, :, tb0:tb0 + 512],
                              reads=['dram_yaT_%d_%d' % (hp_, tb0 // NB + j_) for hp_ in range(4) for j_ in range(512 // NB)], writes=['yaTb'])
                        P.dma(ybTb[:], ybT_d.rearrange("(c p) t -> p c t", p=128)[:, :, tb0:tb0 + 512],
                              reads=['dram_ybT_%d' % (tb0 // 128 + j_) for j_ in range(4)], writes=['ybTb'])
                        for dc in range(8):
                            s_ = xgs[dc % 2]
                            k_ = 'xgs%d' % (dc % 2)
                            P.dma(s_[:], xT[dc * 128:(dc + 1) * 128, own_off + tb0:own_off + tb0 + 512], writes=[k_])
                            P.op('pool' if dc % 2 else 'dve', lambda e, s_=s_, dc=dc: e.tensor_copy(out=xgb[:, dc, :], in_=s_[:]),
                                 reads=[k_], writes=['xgb'])
                        for dco in range(8):
                            cso = slice(dco * 128, (dco + 1) * 128)
                            for fc in range(4):
                                P.op('pe', lambda e, fc=fc, cso=cso: e.matmul(psA[0], lhsT=WA[:, fc, cso], rhs=yaTb[:, fc, :],
                                                                             start=(fc == 0), stop=(fc == 3)), reads=['WA', 'yaTb'], writes=[psA[1]])
                            for fc in range(4):
                                P.op('pe', lambda e, fc=fc, cso=cso: e.matmul(psB[0], lhsT=WB[:, fc, cso], rhs=ybTb[:, fc, :],
                                                                             start=(fc == 0), stop=(fc == 3)), reads=['WB', 'ybTb'], writes=[psB[1]])
                            for dc in range(8):
                                P.op('pe', lambda e, dc=dc, dco=dco: e.matmul(psGa[0], lhsT=WG[:, dc, dco * 128:(dco + 1) * 128], rhs=xgb[:, dc, :],
                                                                             start=(dc == 0), stop=(dc == 7)), reads=['WG', 'xgb'], writes=[psGa[1]])
                            for dc in range(8):
                                P.op('pe', lambda e, dc=dc, dco=dco: e.matmul(psGb[0], lhsT=WG[:, dc, 1024 + dco * 128:1024 + (dco + 1) * 128],
                                                                             rhs=xgb[:, dc, :], start=(dc == 0), stop=(dc == 7)),
                                     reads=['WG', 'xgb'], writes=[psGb[1]])
                            P.op('act', lambda e: e.activation(out=sga[:], in_=psGa[0], func=AF.Sigmoid), reads=[psGa[1]], writes=['sga'])
                            P.op('act', lambda e: e.activation(out=sgb[:], in_=psGb[0], func=AF.Sigmoid), reads=[psGb[1]], writes=['sgb'])
                            P.op('dve', lambda e: e.tensor_tensor(out=m1[:], in0=psA[0], in1=sga[:], op=ALU.mult), reads=[psA[1], 'sga'], writes=['m1'])
                            P.op('dve', lambda e: e.tensor_tensor(out=m2[:], in0=psB[0], in1=sgb[:], op=ALU.mult), reads=[psB[1], 'sgb'], writes=['m2'])
                            P.op('pool', lambda e, dco=dco: e.tensor_tensor(out=mT[:, dco, :], in0=m1[:], in1=m2[:], op=ALU.add),
                                 reads=['m1', 'm2'], writes=['mT'])
                        for tt in range(4):
                            tile_i = tbk * 4 + tt
                            trow = tb0 + tt * 128
                            P.dma(xo_t[:], xo_d[trow:trow + 128, :], writes=['xo_t'])
                            for hh in range(2):
                                for dc in range(8):
                                    P.op('pe', lambda e, dc=dc, tt=tt, hh=hh: e.matmul(
                                        psO[hh][0], lhsT=mT[:, dc, tt * 128:(tt + 1) * 128], rhs=WO[:, dc, hh * 512:(hh + 1) * 512],
                                        start=(dc == 0), stop=(dc == 7)), reads=['mT', 'WO'], writes=[psO[hh][1]])
                                P.op('dve', lambda e, hh=hh: e.scalar_tensor_tensor(
                                    out=Zt[:, hh * 512:(hh + 1) * 512], in0=xo_t[:, hh * 512:(hh + 1) * 512], scalar=ALPHA, in1=psO[hh][0],
                                    op0=ALU.mult, op1=ALU.add), reads=['xo_t', psO[hh][1]], writes=['Zt'])
                            layer_norm_tile(Zt, 'Zt', 0, stt, sq_t)
                            P.op('act', lambda e, tile_i=tile_i: e.mul(out=ACC[:, tile_i, :], in_=Zt[:], mul=ALPHA), reads=['Zt'], writes=['ACC%d' % tile_i])
                            for dc in range(8):
                                pt = psTr[dc // 4]
                                P.op('pe', lambda e, dc=dc, pt=pt: e.matmul(pt[0][:, (dc % 4) * 128:(dc % 4 + 1) * 128],
                                                                            lhsT=Zt[:, dc * 128:(dc + 1) * 128], rhs=ident[:], start=True, stop=True),
                                     reads=['Zt', 'ident'], writes=[pt[1]])
                            for q in range(2):
                                pt = psTr[q]
                                P.op('act', lambda e, q=q, pt=pt: e.copy(out=x1Tf[:, q * 4:(q + 1) * 4, :].rearrange("p a t -> p (a t)"), in_=pt[0]),
                                     reads=[pt[1]], writes=['x1Tf'])
                                P.op('dve', lambda e, q=q, pt=pt, tile_i=tile_i: e.tensor_copy(
                                    out=x1T[:, q * 4:(q + 1) * 4, tile_i * 128:(tile_i + 1) * 128],
                                    in_=pt[0].rearrange("p (a t) -> p a t", t=128)), reads=[pt[1]], writes=['x1T'])
                            for dc in range(8):
                                P.op('pe', lambda e, dc=dc: e.matmul(psR[0][:, 0:36], lhsT=x1Tf[:, dc, :], rhs=RW[:, dc, :],
                                                                     start=(dc == 0), stop=(dc == 7)), reads=['x1Tf', 'RW'], writes=[psR[1]])
                            P.op('dve', lambda e: e.tensor_tensor(out=Lg[:], in0=psR[0][:, 0:36], in1=RBB[:], op=ALU.add),
                                 reads=[psR[1], 'RBB'], writes=['Lg'])
                            RT = lambda a, b: rt[:, a:b]
                            dv = lambda fn, rd=('Lg', 'rt'): P.op('dve', fn, reads=list(rd), writes=['rt'])
                            dv(lambda e: e.tensor_reduce(out=RT(0, 1), in_=Lg[:, 0:4], axis=AX.X, op=ALU.max))
                            dv(lambda e: e.tensor_scalar(out=RT(1, 2), in0=RT(0, 1), scalar1=-1.0, scalar2=None, op0=ALU.mult))
                            P.op('act', lambda e: e.activation(out=RT(16, 20), in_=Lg[:, 0:4], func=AF.Exp, bias=RT(1, 2)), reads=['Lg', 'rt'], writes=['rt'])
                            dv(lambda e: e.tensor_reduce(out=RT(2, 3), in_=RT(16, 20), axis=AX.X, op=ALU.add))
                            dv(lambda e: e.reciprocal(out=RT(3, 4), in_=RT(2, 3)))
                            dv(lambda e: e.tensor_scalar(out=RT(16, 20), in0=Lg[:, 0:4], scalar1=RT(0, 1), scalar2=None, op0=ALU.is_equal))
                            P.op('dve', lambda e: e.tensor_tensor(out=sq_t[:, 0:32].rearrange("p (g j) -> p g j", j=8),
                                                                  in0=Lg[:, 4:36].rearrange("p (g j) -> p g j", j=8),
                                                                  in1=RT(16, 20).unsqueeze(2).to_broadcast([128, 4, 8]), op=ALU.mult),
                                 reads=['Lg', 'rt'], writes=['sq_t'])
                            P.op('dve', lambda e: e.tensor_reduce(out=RT(24, 32), in_=sq_t[:, 0:32].rearrange("p (g j) -> p j g", j=8), axis=AX.X, op=ALU.add),
                                 reads=['sq_t', 'rt'], writes=['rt', 'sq_t'])
                            dv(lambda e: e.tensor_reduce(out=RT(4, 5), in_=RT(24, 32), axis=AX.X, op=ALU.max))
                            dv(lambda e: e.tensor_scalar(out=RT(32, 40), in0=RT(24, 32), scalar1=RT(4, 5), scalar2=None, op0=ALU.is_equal))
                            dv(lambda e: e.scalar_tensor_tensor(out=RT(40, 48), in0=RT(32, 40), scalar=-1e30, in1=RT(24, 32), op0=ALU.mult, op1=ALU.add))
                            dv(lambda e: e.tensor_reduce(out=RT(5, 6), in_=RT(40, 48), axis=AX.X, op=ALU.max))
                            dv(lambda e: e.tensor_scalar(out=RT(40, 48), in0=RT(40, 48), scalar1=RT(5, 6), scalar2=None, op0=ALU.is_equal))
                            dv(lambda e: e.tensor_tensor(out=RT(6, 7), in0=RT(4, 5), in1=RT(5, 6), op=ALU.subtract))
                            P.op('act', lambda e: e.activation(out=RT(7, 8), in_=RT(6, 7), func=AF.Sigmoid), reads=['rt'], writes=['rt'])
                            dv(lambda e: e.tensor_tensor(out=RT(8, 9), in0=RT(7, 8), in1=RT(3, 4), op=ALU.mult))
                            dv(lambda e: e.tensor_tensor(out=RT(9, 10), in0=RT(3, 4), in1=RT(8, 9), op=ALU.subtract))
                            dv(lambda e: e.tensor_scalar(out=RT(48, 56), in0=RT(32, 40), scalar1=RT(8, 9), scalar2=None, op0=ALU.mult))
                            dv(lambda e: e.scalar_tensor_tensor(out=RT(48, 56), in0=RT(40, 48), scalar=RT(9, 10), in1=RT(48, 56), op0=ALU.mult, op1=ALU.add))
                            P.op('dve', lambda e, tile_i=tile_i: e.tensor_tensor(out=WtAll[:, tile_i, :].rearrange("p (g j) -> p g j", j=8),
                                                                  in0=RT(16, 20).unsqueeze(2).to_broadcast([128, 4, 8]),
                                                                  in1=RT(48, 56).unsqueeze(1).to_broadcast([128, 4, 8]), op=ALU.mult),
                                 reads=['rt'], writes=['WtAll'])
                    P.flush()
                with ExitStack() as es:
                    P.barrier()
                    PSA.reset()
                    W1b = [P.sb(es, 'W1b', [128, 8, 512], BF16) for _ in range(2)]
                    W3b = [P.sb(es, 'W3b', [128, 8, 512], BF16) for _ in range(2)]
                    W2b = [P.sb(es, 'W2b', [128, 4, D], BF16) for _ in range(2)]
                    wst = [P.sb(es, 'wst', [128, 2048]) for _ in range(3)]
                    sil = [P.sb(es, 'sil', [128, 512]) for _ in range(2)]
                    hwT = [P.sb(es, 'hwT', [128, 4, 512], BF16) for _ in range(2)]
                    psH1 = [PSA.alloc(512, 1), PSA.alloc(512, 2)]
                    psH3 = [PSA.alloc(512, 3), PSA.alloc(512, 4)]
                    psF = [PSA.alloc(512, 5), PSA.alloc(512, 6)]
                    wq = [0]
                    ceng = ['pool', 'act']

                    def wload(dst, dkey, src3, nck):
                        for c in range(nck):
                            i_ = wq[0] % 3
                            wq[0] += 1
                            s_ = wst[i_]
                            k_ = 'wst%d' % i_
                            P.dma(s_[:].rearrange("p (a f) -> p a f", a=src3(c).shape[1]), src3(c), writes=[k_])
                            en = ceng[wq[0] % 2]
                            if en == 'act':
                                P.op('act', lambda e, s_=s_, c=c: e.copy(out=dst(c), in_=s_[:]), reads=[k_], writes=[dkey])
                            else:
                                P.op('pool', lambda e, s_=s_, c=c: e.tensor_copy(out=dst(c), in_=s_[:]), reads=[k_], writes=[dkey])
                    for ex in range(32):
                        pb_ = ex % 2
                        w1v = w1_d[ex].rearrange("(c p) f -> p c f", p=128)
                        w3v = w3_d[ex].rearrange("(c p) f -> p c f", p=128)
                        w2v = w2_d[ex].rearrange("(c p) d -> p c d", p=128)
                        wload(lambda c, pb_=pb_: W1b[pb_][:, c * 4:(c + 1) * 4, :].rearrange("p a f -> p (a f)"), 'W1b%d' % pb_,
                              lambda c, w1v=w1v: w1v[:, c * 4:(c + 1) * 4, :], 2)
                        wload(lambda c, pb_=pb_: W3b[pb_][:, c * 4:(c + 1) * 4, :].rearrange("p a f -> p (a f)"), 'W3b%d' % pb_,
                              lambda c, w3v=w3v: w3v[:, c * 4:(c + 1) * 4, :], 2)
                        wload(lambda c, pb_=pb_: W2b[pb_][:, c * 2:(c + 1) * 2, :].rearrange("p a f -> p (a f)"), 'W2b%d' % pb_,
                              lambda c, w2v=w2v: w2v[:, c * 2:(c + 1) * 2, :], 2)
                        for tbk in range(NT_H // 512):
                            tsl = slice(tbk * 512, (tbk + 1) * 512)
                            hw = hwT[tbk % 2]
                            hwk = 'hwT%d' % (tbk % 2)
                            for fc in range(4):
                                p1, p3 = psH1[fc % 2], psH3[fc % 2]
                                fsl = slice(fc * 128, (fc + 1) * 128)
                                for dc in range(8):
                                    P.op('pe', lambda e, dc=dc, fsl=fsl, tsl=tsl, p1=p1, pb_=pb_: e.matmul(
                                        p1[0], lhsT=W1b[pb_][:, dc, fsl], rhs=x1T[:, dc, tsl], start=(dc == 0), stop=(dc == 7)),
                                        reads=['W1b%d' % pb_, 'x1T'], writes=[p1[1]])
                                for dc in range(8):
                                    P.op('pe', lambda e, dc=dc, fsl=fsl, tsl=tsl, p3=p3, pb_=pb_: e.matmul(
                                        p3[0], lhsT=W3b[pb_][:, dc, fsl], rhs=x1T[:, dc, tsl], start=(dc == 0), stop=(dc == 7)),
                                        reads=['W3b%d' % pb_, 'x1T'], writes=[p3[1]])
                                sl_ = sil[fc % 2]
                                P.op('act', lambda e, sl_=sl_, p1=p1: e.activation(out=sl_[:], in_=p1[0], func=AF.Silu),
                                     reads=[p1[1]], writes=['sil%d' % (fc % 2)])
                                P.op('dve', lambda e, sl_=sl_, hw=hw, fc=fc, p3=p3: e.tensor_tensor(out=hw[:, fc, :], in0=p3[0], in1=sl_[:], op=ALU.mult),
                                     reads=[p3[1], 'sil%d' % (fc % 2)], writes=[hwk])
                            for tt in range(4):
                                tile_i = tbk * 4 + tt
                                for hx in range(2):
                                    pf = psF[hx]
                                    for fc in range(4):
                                        P.op('pe', lambda e, fc=fc, tt=tt, hx=hx, pf=pf, hw=hw, pb_=pb_: e.matmul(
                                            pf[0], lhsT=hw[:, fc, tt * 128:(tt + 1) * 128], rhs=W2b[pb_][:, fc, hx * 512:(hx + 1) * 512],
                                            start=(fc == 0), stop=(fc == 3)), reads=[hwk, 'W2b%d' % pb_], writes=[pf[1]])
                                    P.op('dve', lambda e, tile_i=tile_i, hx=hx, pf=pf, ex=ex: e.scalar_tensor_tensor(
                                        out=ACC[:, tile_i, hx * 512:(hx + 1) * 512], in0=pf[0], scalar=WtAll[:, tile_i, ex:ex + 1],
                                        in1=ACC[:, tile_i, hx * 512:(hx + 1) * 512], op0=ALU.mult, op1=ALU.add),
                                        reads=[pf[1], 'ACC%d' % tile_i, 'WtAll'], writes=['ACC%d' % tile_i])
                    P.flush()
                with ExitStack() as es:
                    P.barrier()
                    PSA.reset()
                    WPG = P.sb(es, 'WPG', [128, 8, D], BF16)
                    load_ln(['ln2_g', 'ln2_b'])
                    WPE = P.sb(es, 'WPE', [128, 2, D], BF16)
                    pTb = P.sb(es, 'pTb', [128, 2, NT_H], BF16)
                    with ExitStack() as es1:
                        load_cast(es1, WPG, 'WPG', lambda c: w_pg_d[c * 128:(c + 1) * 128, :], D, 8)
                        load_cast(es1, WPE, 'WPE', lambda c: w_pe_d[c * 128:(c + 1) * 128, :], D, 2)
                        load_cast(es1, pTb, 'pTb', lambda c: pT_d[c * 128:(c + 1) * 128, :], NT_H, 2, col0=tk0)
                        P.flush()
                    P.barrier()
                    Z2 = P.sb(es, 'Z2', [128, D])
                    sq_t = P.sb(es, 'sq_t2', [128, D])
                    stt = P.sb(es, 'stt2', [128, 2])
                    x2T = P.sb(es, 'x2T', [128, 8, 128], BF16)
                    sgp = P.sb(es, 'sgp', [128, 512])
                    ot_ = [P.sb(es, 'ot_', [128, D]) for _ in range(2)]
                    psTr = [PSA.alloc(512, 0), PSA.alloc(512, 1)]
                    psG = [PSA.alloc(512, 2), PSA.alloc(512, 3)]
                    psP = [PSA.alloc(512, 4), PSA.alloc(512, 5)]
                    for tile_i in range(NT_H // 128):
                        trow = tk0 + tile_i * 128
                        P.op('act', lambda e, tile_i=tile_i: e.copy(out=Z2[:], in_=ACC[:, tile_i, :]), reads=['ACC%d' % tile_i], writes=['Z2'])
                        layer_norm_tile(Z2, 'Z2', 2, stt, sq_t)
                        for dc in range(8):
                            pt = psTr[dc // 4]
                            P.op('pe', lambda e, dc=dc, pt=pt: e.matmul(pt[0][:, (dc % 4) * 128:(dc % 4 + 1) * 128],
                                                                        lhsT=Z2[:, dc * 128:(dc + 1) * 128], rhs=ident[:], start=True, stop=True),
                                 reads=['Z2', 'ident'], writes=[pt[1]])
                        for q in range(2):
                            pt = psTr[q]
                            evac3(x2T[:, q * 4:(q + 1) * 4, :].rearrange("p a t -> p (a t)"), pt[0], [pt[1]], ['x2T'])
                        o_t = ot_[tile_i % 2]
                        ok_ = 'ot_%d' % (tile_i % 2)
                        for hx in range(2):
                            for dc in range(8):
                                P.op('pe', lambda e, dc=dc, hx=hx: e.matmul(psG[hx][0], lhsT=x2T[:, dc, :], rhs=WPG[:, dc, hx * 512:(hx + 1) * 512],
                                                                            start=(dc == 0), stop=(dc == 7)), reads=['x2T', 'WPG'], writes=[psG[hx][1]])
                            for kc in range(2):
                                P.op('pe', lambda e, kc=kc, hx=hx, tile_i=tile_i: e.matmul(
                                    psP[hx][0], lhsT=pTb[:, kc, tile_i * 128:(tile_i + 1) * 128], rhs=WPE[:, kc, hx * 512:(hx + 1) * 512],
                                    start=(kc == 0), stop=(kc == 1)), reads=['pTb', 'WPE'], writes=[psP[hx][1]])
                            P.op('act', lambda e, hx=hx: e.activation(out=sgp[:], in_=psG[hx][0], func=AF.Sigmoid), reads=[psG[hx][1]], writes=['sgp'])
                            P.op('dve', lambda e, hx=hx, o_t=o_t: e.tensor_tensor(out=o_t[:, hx * 512:(hx + 1) * 512], in0=psP[hx][0], in1=sgp[:], op=ALU.mult),
                                 reads=[psP[hx][1], 'sgp'], writes=[ok_])
                            P.op('pool', lambda e, hx=hx, o_t=o_t: e.tensor_tensor(out=o_t[:, hx * 512:(hx + 1) * 512], in0=o_t[:, hx * 512:(hx + 1) * 512],
                                                                                   in1=Z2[:, hx * 512:(hx + 1) * 512], op=ALU.add),
                                 reads=[ok_, 'Z2'], writes=[ok_])
                        P.dma(out_d[trow:trow + 128, :], o_t[:], reads=[ok_], writes=['dram_out_%d' % trow])
                    P.flush()
            for hf_ in range(TOWN // NT_H):
                do_half(hf_)
            P.flush()
        print('NOPS', P.nops, {k: len(v) for k, v in P.streams.items()}, flush=True)
        P.emit()
    return nc


def make_in_maps(inputs):
    f32 = lambda a: np.ascontiguousarray(np.asarray(a, np.float32))
    x = f32(inputs['x'])
    consts = host_consts()
    pv = np.zeros((128, NPV), np.float32)
    pv[:, PV['w0']:PV['w0'] + 4] = colvec(inputs['rw_w0'][0], 4)
    pv[:, PV['a0']:PV['a0'] + 4] = colvec(inputs['rw_a0'][0], 4)
    pv[:, PV['k_k']:PV['k_k'] + 4] = colvec(inputs['rw_k_k'][0], 4)
    pv[:, PV['k_a']:PV['k_a'] + 4] = colvec(inputs['rw_k_a'][0], 4)
    pv[:, PV['r_k']:PV['r_k'] + 4] = colvec(np.asarray(inputs['rw_r_k'][0]).reshape(512), 4)
    pv[:, PV['lb0']:PV['lb0'] + 4] = colvec(inputs['hg_lb_logits'][0], 4)
    pv[:, PV['lb1']:PV['lb1'] + 4] = colvec(inputs['hg_lb_logits'][1], 4)
    rows = np.zeros((1, NROWS), np.float32)

    def setrow(name, v):
        o, n = ROWS[name]
        rows[0, o:o + n] = np.asarray(v, np.float32).reshape(n)
    setrow('mu', inputs['rw_mu'][0])
    setrow('gn_w', inputs['rw_gn_w'][0])
    setrow('gn_b', inputs['rw_gn_b'][0])
    setrow('hg_nw', inputs['hg_norm_w'][0])
    setrow('ln1_g', inputs['ln1_g'][0])
    setrow('ln1_b', inputs['ln1_b'][0])
    setrow('ln2_g', inputs['ln2_g'][0])
    setrow('ln2_b', inputs['ln2_b'][0])
    setrow('rb', np.concatenate([np.asarray(inputs['router_g_b'][0]).reshape(4), np.asarray(inputs['router_e_b'][0]).reshape(32)]))
    gw = np.asarray(inputs['rw_gn_w'][0], np.float32).reshape(4, 2, 1, 64)
    gb = np.asarray(inputs['rw_gn_b'][0], np.float32).reshape(4, 2, 1, 64)
    gnw_t = np.ascontiguousarray(np.broadcast_to(gw, (4, 2, 64, 64)).reshape(4, 128, 64))
    gnb_t = np.ascontiguousarray(np.broadcast_to(gb, (4, 2, 64, 64)).reshape(4, 128, 64))
    shared = dict(consts)
    shared.update(pv=pv, rows=rows, gnw_t=gnw_t, gnb_t=gnb_t,
                  w_in=f32(inputs['w_in'][0]), rw_w_up=f32(inputs['rw_w_up'][0]), rw_a_up=f32(inputs['rw_a_up'][0]),
                  rw_g_up=f32(inputs['rw_g_up'][0]),
                  w_a_out=f32(inputs['w_a_out'][0]), w_b_out=f32(inputs['w_b_out'][0]), w_o=f32(inputs['w_o'][0]),
                  rw=np.ascontiguousarray(np.concatenate([f32(inputs['router_g_w'][0]), f32(inputs['router_e_w'][0])], 1)),
                  w1=f32(inputs['w1'][0]), w3=f32(inputs['w3'][0]), w2=f32(inputs['w2'][0]),
                  w_pe=f32(inputs['w_pe'][0]), w_pg=f32(inputs['w_pg'][0]))
    in_maps = []
    for c in range(8):
        b, half = c // 2, c % 2
        xw = np.zeros((D, T), np.float32)
        if half == 1:
            xw[:, :] = x[b].T
        else:
            xw[:, T - TOWN:] = x[b, :TOWN].T
        m = dict(shared)
        m['xT'] = xw
        m['xo'] = np.ascontiguousarray(x[b, half * TOWN:(half + 1) * TOWN])
        m['pT'] = np.ascontiguousarray(np.asarray(inputs['p'], np.float32)[0, b, half * TOWN:(half + 1) * TOWN].T)
        in_maps.append(m)
    return in_maps


_NC_CACHE = {}


def kernel(**inputs):
    if 'full' not in _NC_CACHE:
        _NC_CACHE['full'] = build('full')
    nc = _NC_CACHE['full']
    in_maps = make_in_maps(inputs)
    keep = set(['xT', 'w_in', 'pv', 'rows', 'ident', 'mask320', 'iu2', 'ii', 'onesblk', 'scanmask',
                'rw_w_up', 'rw_a_up', 'rw_g_up', 'gnw_t', 'gnb_t', 'xo', 'pT', 'w_a_out', 'w_b_out', 'w_o', 'rw',
                'w1', 'w3', 'w2', 'w_pe', 'w_pg'])
    in_maps = [{k: v for k, v in m.items() if k in keep} for m in in_maps]
    res = run_bass_kernel_spmd(nc, in_maps, core_ids=list(range(8)))
    out = np.zeros((4, T, D), np.float32)
    for c in range(8):
        b, half = c // 2, c % 2
        out[b, half * TOWN:(half + 1) * TOWN] = res.results[c]['out']
    return out
```

```python
import os
import numpy as np
from contextlib import ExitStack
import concourse.bass as bass
import concourse.mybir as mybir
from concourse.bass_utils import run_bass_kernel_spmd

F32 = mybir.dt.float32
BF16 = mybir.dt.bfloat16
ALU = mybir.AluOpType
AF = mybir.ActivationFunctionType
AX = mybir.AxisListType

NDSEM = 8
T = 4096
TOWN = 2048
D = 1024
NB = 256
NCH = NB // 64
NBLK = T // NB
OWNB = (T - TOWN) // NB
CDEC = 0.6065306597126334
ALPHA = 2.0 ** 0.25
N_IN = 5888
C_Q = 1792
C_GATE = 3840


class Prog:
    def __init__(self, nc, es):
        self.nc = nc
        self.es = es
        self.engs = ['pe', 'act', 'dve', 'pool', 'sp']
        self.streams = {e: [] for e in self.engs}
        self.prods = ['pe', 'act', 'dve', 'pool']
        self.count = {p: 0 for p in self.prods}
        self.sems = {p: es.enter_context(nc.semaphore('s_' + p)) for p in self.prods}
        self.waited = {}
        self.last_write = {}
        self.readers = {}
        self.ndma = 0
        self.uid = 0
        self.floor = {}

    def barrier(self):
        self.floor = dict(self.count)

    def sb(self, es, name, shape, dt=F32):
        self.uid += 1
        return es.enter_context(self.nc.sbuf_tensor('%s_%d' % (name, self.uid), list(shape), dt))

    def _deps(self, eng, prod, reads, writes):
        deps = {}
        writes = list(writes) + [k for k in reads if k.startswith('bank')]
        reads = [k for k in reads if not k.startswith('bank')]

        def add(p, n):
            if n > deps.get(p, 0):
                deps[p] = n
        for k in reads:
            if k in self.last_write:
                add(*self.last_write[k])
        for k in writes:
            if k in self.last_write:
                add(*self.last_write[k])
            for p, n in self.readers.get(k, {}).items():
                add(p, n)
        waits = []
        for p, n in self.floor.items():
            if n > deps.get(p, 0) and self.waited.get((eng, p), 0) < n:
                deps[p] = n
        for p, n in deps.items():
            if p == 'pe' and eng == 'pe' and self.floor.get('pe', 0) < n:
                continue
            if self.waited.get((eng, p), 0) >= n:
                continue
            self.waited[(eng, p)] = n
            waits.append((p, n))
        self.count[prod] += 1
        idx = self.count[prod]
        for k in writes:
            self.last_write[k] = (prod, idx)
            self.readers[k] = {}
        for k in reads:
            rd = self.readers.setdefault(k, {})
            if rd.get(prod, 0) < idx:
                rd[prod] = idx
        return waits

    def op(self, eng, fn, reads=(), writes=()):
        self.nops = getattr(self, 'nops', 0) + 1
        if self.nops > int(os.environ.get('KMAX', '100000000')):
            return
        waits = self._deps(eng, eng, reads, writes)
        self.streams[eng].append((waits, fn, eng))

    def dma(self, out, in_, reads=(), writes=(), eng='sp'):
        self.nops = getattr(self, 'nops', 0) + 1
        if self.nops > int(os.environ.get('KMAX', '100000000')):
            return
        skey = [k for k in list(writes) + list(reads) if not k.startswith('dram_')][0]
        prod = 'd_' + skey
        if prod not in self.sems:
            self.sems[prod] = self.es.enter_context(self.nc.semaphore('s_' + prod))
            self.count[prod] = 0
            self.prods.append(prod)
        self.ndma += 1
        waits = self._deps(eng, prod, reads, writes)
        self.streams[eng].append((waits, lambda e: e.dma_start(out=out, in_=in_), prod))

    def emit(self):
        self.flush(final=True)

    def flush(self, final=False):
        nc = self.nc
        if not final and not any(self.streams.values()):
            return
        streams = self.streams
        self.streams = {e: [] for e in self.engs}

        def run(ename, e):
            for waits, fn, prod in streams[ename]:
                for p, n in waits:
                    e.wait_ge(self.sems[p], n * 16 if p[0] == 'd' else n)
                ins = fn(e)
                ins.then_inc(self.sems[prod], 16 if prod[0] == 'd' else 1)
            if ename == 'sp' and final:
                for p in self.prods:
                    if self.count[p] > 0:
                        e.wait_ge(self.sems[p], self.count[p] * (16 if p[0] == 'd' else 1))
        with nc.Block() as block:
            @block.tensor
            def _(e):
                run('pe', e)

            @block.scalar
            def _(e):
                run('act', e)

            @block.vector
            def _(e):
                run('dve', e)

            @block.gpsimd
            def _(e):
                run('pool', e)

            @block.sync
            def _(e):
                run('sp', e)


class PsumAlloc:
    def __init__(self, P, es):
        self.banks = [es.enter_context(P.nc.psum_tensor('psb%d' % i, [128, 512], F32)) for i in range(8)]
        self.used = [0] * 8
        self.n = 0

    def alloc(self, cols, bank):
        b = bank
        assert self.used[b] + cols <= 512, (bank, cols, self.used[b])
        o = self.used[b]
        self.used[b] += cols
        return self.banks[b][:, o:o + cols], 'bank%d' % b

    def reset(self):
        self.used = [0] * 8


def colvec(v, n):
    return np.ascontiguousarray(np.asarray(v, np.float32).reshape(n, 128).T)


def host_consts():
    c = {}
    c['ident'] = np.eye(128, dtype=np.float32)
    su = np.triu(np.ones((64, 64), np.float32), 1)
    sl = np.tril(np.ones((64, 64), np.float32), -1)
    iu = np.triu(np.ones((64, 64), np.float32), 0)
    m = np.concatenate([su, sl, su, iu, iu], 1)
    c['mask320'] = np.concatenate([m, m], 0)
    c['iu2'] = np.concatenate([iu, iu], 0)
    i64 = np.eye(64, dtype=np.float32)
    ii = np.concatenate([i64, i64], 1)
    c['ii'] = np.concatenate([ii, ii], 0)
    ob = np.zeros((128, 128), np.float32)
    ob[:64, :64] = 1
    ob[64:, 64:] = 1
    c['onesblk'] = ob
    sm = np.ones((128, NB), np.float32)
    sm[:, ::64] = 0
    c['scanmask'] = sm
    sel = np.zeros((32, 32 * 128), np.float32)
    for e in range(32):
        sel[e, e * 128:(e + 1) * 128] = 1
    c['sel'] = sel
    return c


PV = {}
_o = 0
for _name, _n in [('w0', 4), ('a0', 4), ('k_k', 4), ('k_a', 4), ('r_k', 4), ('lb0', 4), ('lb1', 4)]:
    PV[_name] = _o
    _o += _n
NPV = _o
ROWS = {}
_o = 0
for _name, _n in [('mu', 1792), ('gn_w', 512), ('gn_b', 512), ('hg_nw', 512), ('ln1_g', 1024), ('ln1_b', 1024),
                  ('ln2_g', 1024), ('ln2_b', 1024), ('rb', 36)]:
    ROWS[_name] = (_o, _n)
    _o += _n
NROWS = _o


def build(stage='full', nblk=NBLK, ownb=OWNB):
    nc = bass.Bass("TRN2", target_bir_lowering=False)

    def din(name, shape, dt=F32):
        return nc.dram_tensor(name, list(shape), dt, kind="ExternalInput").ap()

    def dout(name, shape, dt=F32):
        return nc.dram_tensor(name, list(shape), dt, kind="ExternalOutput").ap()

    xT = din('xT', [D, T])
    w_in = din('w_in', [D, N_IN])
    pv_d = din('pv', [128, NPV])
    rows_d = din('rows', [1, NROWS])
    ident_d = din('ident', [128, 128])
    mask320_d = din('mask320', [128, 320])
    iu2_d = din('iu2', [128, 64])
    ii_d = din('ii', [128, 128])
    onesblk_d = din('onesblk', [128, 128])
    scanmask_d = din('scanmask', [128, NB])
    w_up_d = din('rw_w_up', [64, 512])
    a_up_d = din('rw_a_up', [64, 512])
    g_up_d = din('rw_g_up', [128, 512])
    gnw_d = din('gnw_t', [4, 128, 64])
    gnb_d = din('gnb_t', [4, 128, 64])

    if stage == 'full':
        xo_d = din('xo', [TOWN, D])
        pT_d = din('pT', [256, TOWN])
        w_a_out_d = din('w_a_out', [512, D])
        w_b_out_d = din('w_b_out', [512, D])
        w_o_d = din('w_o', [D, D])
        rw_d = din('rw', [D, 36])
        w1_d = din('w1', [32, D, 512])
        w3_d = din('w3', [32, D, 512])
        w2_d = din('w2', [32, 512, D])
        w_pe_d = din('w_pe', [256, D])
        w_pg_d = din('w_pg', [D, D])
    yaT_d = nc.dram_tensor('yaT_s', [512, TOWN], BF16, kind="Internal").ap()
    ybT_d = nc.dram_tensor('ybT_s', [512, TOWN], BF16, kind="Internal").ap()
    if stage == 'rwkv':
        ya_dbg = dout('ya_dbg', [TOWN // 64, 4, 128, 64])
    elif stage == 'hgrn':
        yb_dbg = dout('yb_dbg', [TOWN, 512])
    elif stage == 'fullA':
        yaT_o = dout('yaT_o', [512, TOWN], BF16)
        ybT_o = dout('ybT_o', [512, TOWN], BF16)
    else:
        out_d = dout('out', [TOWN, D])

    with ExitStack() as es0:
        P = Prog(nc, es0)
        PSA = PsumAlloc(P, es0)
        ident = P.sb(es0, 'ident', [128, 128])
        pvt = P.sb(es0, 'pv', [128, NPV])
        P.dma(ident[:], ident_d, writes=['ident'])
        P.dma(pvt[:], pv_d, writes=['pv'])

        def pcol(name, i):
            o = PV[name] + i
            return pvt[:, o:o + 1]

        with ExitStack() as es:
            PSA.reset()
            mask320 = P.sb(es, 'mask320', [128, 320])
            iit = P.sb(es, 'ii', [128, 128])
            onesblk = P.sb(es, 'onesblk', [128, 128])
            scanmask = P.sb(es, 'scanmask', [128, NB])
            P.dma(mask320[:], mask320_d, writes=['mask320'])
            P.dma(iit[:], ii_d, writes=['ii'])
            P.dma(onesblk[:], onesblk_d, writes=['onesblk'])
            P.dma(scanmask[:], scanmask_d, writes=['scanmask'])
            wA = P.sb(es, 'wA', [128, 8, 1792], BF16)
            wB = P.sb(es, 'wB', [128, 8, 1792], BF16)
            LW = P.sb(es, 'LW', [128, 512], BF16)
            LW2 = P.sb(es, 'LW2', [128, 512], BF16)
            GU = P.sb(es, 'GU', [128, 512], BF16)
            gnw = P.sb(es, 'gnw', [128, 4, 64])
            gnb = P.sb(es, 'gnb', [128, 4, 64])
            for hp in range(4):
                P.dma(gnw[:, hp, :], gnw_d[hp], writes=['gnw'])
                P.dma(gnb[:, hp, :], gnb_d[hp], writes=['gnb'])
            if os.environ.get('KSTOP') == '1':
                P.emit()
                return nc
            with ExitStack() as es1:
                MU = P.sb(es1, 'MU', [128, 1792])
                OMM = P.sb(es1, 'OMM', [128, 1792])
                o, n = ROWS['mu']
                P.dma(MU[:], rows_d[:, o:o + n].partition_broadcast(128), writes=['MU'])
                P.op('dve', lambda e: e.tensor_scalar(out=OMM[:], in0=MU[:], scalar1=-1.0, scalar2=1.0,
                                                      op0=ALU.mult, op1=ALU.add), reads=['MU'], writes=['OMM'])
                stg = [P.sb(es1, 'wstg', [128, 1792]) for _ in range(2)]
                for dc in range(8):
                    s = stg[dc % 2]
                    k = 'wstg%d' % (dc % 2)
                    P.dma(s[:], w_in[dc * 128:(dc + 1) * 128, 0:1792], writes=[k])
                    P.op('dve', lambda e, s=s, dc=dc: e.tensor_tensor(out=wA[:, dc, :], in0=s[:], in1=OMM[:], op=ALU.mult),
                         reads=[k, 'OMM'], writes=['wA'])
                    P.op('pool', lambda e, s=s, dc=dc: e.tensor_tensor(out=wB[:, dc, :], in0=s[:], in1=MU[:], op=ALU.mult),
                         reads=[k, 'MU'], writes=['wB'])
                if os.environ.get('KSTOP') == '2':
                    P.emit()
                    return nc
                ls = P.sb(es1, 'lstg', [128, 512])
                P.dma(ls[0:64, :], w_up_d, writes=['lstg'])
                P.dma(ls[64:128, :], a_up_d, writes=['lstg'])
                P.op('pool', lambda e: e.memset(LW[:], 0.0), writes=['LW'])
                P.op('pool', lambda e: e.memset(LW2[:], 0.0), writes=['LW'])
                P.op('dve', lambda e: e.tensor_copy(out=LW[0:64, :], in_=ls[0:64, :]), reads=['lstg'], writes=['LW'])
                P.op('dve', lambda e: e.tensor_copy(out=LW2[64:128, :], in_=ls[64:128, :]), reads=['lstg'], writes=['LW'])
                gs = P.sb(es1, 'gstg', [128, 512])
                P.dma(gs[:], g_up_d, writes=['gstg'])
                P.op('dve', lambda e: e.tensor_copy(out=GU[:], in_=gs[:]), reads=['gstg'], writes=['GU'])
                P.flush()
            if os.environ.get('KSTOP') == '3':
                P.emit()
                return nc
            if os.environ.get('KNOBAR') != '1':
                P.barrier()

            xstg = [P.sb(es, 'xstg', [128, NB + 1]) for _ in range(2)]
            xb = [P.sb(es, 'xb', [128, 8, NB], BF16) for _ in range(2)]
            xbs_ = [P.sb(es, 'xbs', [128, 8, NB], BF16) for _ in range(2)]
            FM = {}
            for nm in ['Rr', 'Kr', 'Vt', 'At', 'Bt', 'Kt', 'Rt', 'Rtb', 'rk', 'eG']:
                FM[nm] = [P.sb(es, nm, [128, NB], BF16 if nm in ('Vt', 'At', 'Bt', 'Kt', 'Rtb') else F32) for _ in range(4)]
            identb = P.sb(es, 'identb', [128, 128], BF16)
            P.op('dve', lambda e: e.tensor_copy(out=identb[:], in_=ident[:]), reads=['ident'], writes=['identb'])
            tmp = {nm: P.sb(es, nm, [128, NB]) for nm in ['sg', 'a', 'Gs', 'Gp', 'eGn', 'eGp', 'kk', 'sq', 'rn', 'kkn', 't1', 'kp']}
            LX = P.sb(es, 'LX', [128, NB], BF16)
            SGX = P.sb(es, 'SGX', [128, NB], BF16)
            HH = [[P.sb(es, 'H', [128, 64]) for _ in range(2)] for _ in range(4)]
            for hp in range(4):
                P.op('pool', lambda e, hp=hp: e.memset(HH[hp][0][:], 0.0), writes=['H%d_0' % hp])
            NPAR = 2
            TM = [P.sb(es, 'TM', [128, 4, 64], BF16) for _ in range(NPAR)]
            MM = [P.sb(es, 'MM', [128, 320], BF16) for _ in range(NPAR)]
            TT = [[P.sb(es, 'TT', [128, 64], BF16) for _ in range(2)] for _ in range(NPAR)]
            PP = [[P.sb(es, 'PP', [128, 128], BF16) for _ in range(2)] for _ in range(NPAR)]
            X2s = [P.sb(es, 'X2s', [128, 64], BF16) for _ in range(NPAR)]
            WW = [P.sb(es, 'WW', [128, 128]) for _ in range(NPAR)]
            Dg = [P.sb(es, 'Dg', [128, 64]) for _ in range(NPAR)]
            HDg = [P.sb(es, 'HDg', [128, 64]) for _ in range(NPAR)]
            Us = [P.sb(es, 'Us', [128, 64], BF16) for _ in range(NPAR)]
            Yb = [P.sb(es, 'Yb', [128, NCH, 64]) for _ in range(2)]
            Yc = [P.sb(es, 'Yc', [128, NCH, 64]) for _ in range(2)]
            st = [P.sb(es, 'st', [128, 4 * NCH]) for _ in range(2)]
            cfs = [P.sb(es, 'cfs', [128, NCH]) for _ in range(2)]
            yaTs = [P.sb(es, 'yaTs', [128, NB], BF16) for _ in range(2)]
            pj = [PSA.alloc(NB, 0), PSA.alloc(NB, 3)]
            pss = PSA.alloc(NB, 0)
            pl = PSA.alloc(2 * NB, 2)
            psY = [PSA.alloc(NB, 3)] * 2
            psT = [PSA.alloc(256, 4), PSA.alloc(256, 1)]
            psW = psT
            psD = [PSA.alloc(128, 4), PSA.alloc(128, 1)]
            psX = [PSA.alloc(64, 4), PSA.alloc(64, 1)]
            psU = [PSA.alloc(64, 4), PSA.alloc(64, 1)]
            psM = [PSA.alloc(320, 5), PSA.alloc(320, 7)]
            psE = [PSA.alloc(64, 5), PSA.alloc(64, 7)]
            psH = [PSA.alloc(64, 5), PSA.alloc(64, 7)]
            pgate = [PSA.alloc(NB, 6)] * 2
            pyt = [PSA.alloc(NB, 6)] * 2
            pcf = [PSA.alloc(NCH, 7)] * 2

            evq = [0]

            def evac_copy(out, in_, reads, writes):
                evq[0] += 1
                if evq[0] % 2:
                    P.op('act', lambda e: e.copy(out=out, in_=in_), reads=reads, writes=writes)
                else:
                    P.op('dve', lambda e: e.tensor_copy(out=out, in_=in_), reads=reads, writes=writes)

            for tb in range(nblk if stage != 'hgrn' else 0):
                own = tb >= ownb
                t0 = tb * NB
                xbt = xb[tb % 2]
                xbst = xbs_[tb % 2]
                xk = 'xb%d' % (tb % 2)
                for dc in range(8):
                    s = xstg[dc % 2]
                    k = 'xstg%d' % (dc % 2)
                    if tb == 0:
                        P.op('pool', lambda e, s=s: e.memset(s[:, 0:1], 0.0), writes=[k])
                        P.dma(s[:, 1:NB + 1], xT[dc * 128:(dc + 1) * 128, 0:NB], writes=[k])
                    else:
                        P.dma(s[:], xT[dc * 128:(dc + 1) * 128, t0 - 1:t0 + NB], writes=[k])
                    P.op('pool', lambda e, s=s, dc=dc, xbt=xbt: e.tensor_copy(out=xbt[:, dc, :], in_=s[:, 1:NB + 1]),
                         reads=[k], writes=[xk])
                    P.op('dve', lambda e, s=s, dc=dc, xbst=xbst: e.tensor_copy(out=xbst[:, dc, :], in_=s[:, 0:NB]),
                         reads=[k], writes=[xk])
                for pc in range(14):
                    pap, pk = pj[pc % 2]
                    for dc in range(8):
                        P.op('pe', lambda e, pap=pap, dc=dc, pc=pc, xbt=xbt: e.matmul(
                            pap, lhsT=wA[:, dc, pc * 128:(pc + 1) * 128], rhs=xbt[:, dc, :], start=(dc == 0), stop=False),
                            reads=['wA', xk], writes=[pk])
                    for dc in range(8):
                        P.op('pe', lambda e, pap=pap, dc=dc, pc=pc, xbst=xbst: e.matmul(
                            pap, lhsT=wB[:, dc, pc * 128:(pc + 1) * 128], rhs=xbst[:, dc, :], start=False, stop=(dc == 7)),
                            reads=['wB', xk], writes=[pk])
                    if pc < 12:
                        nm = ['Rr', 'Kr', 'Vt'][pc // 4]
                        hp = pc % 4
                        if nm == 'Rr' and not own:
                            pass
                        else:
                            evac_copy(FM[nm][hp][:], pap, [pk], ['%s%d' % (nm, hp)])
                    elif pc == 12:
                        P.op('act', lambda e, pap=pap: e.activation(out=LX[0:64, :], in_=pap[0:64, :], func=AF.Tanh),
                             reads=[pk], writes=['LX'])
                        P.op('dve', lambda e, pap=pap: e.tensor_copy(out=LX[64:128, :], in_=pap[64:128, :]),
                             reads=[pk], writes=['LX'])
                    else:
                        if own:
                            P.op('act', lambda e, pap=pap: e.activation(out=SGX[:], in_=pap, func=AF.Sigmoid),
                                 reads=[pk], writes=['SGX'])
                for hp in range(4):
                    Rr, Kr, Vt = FM['Rr'][hp], FM['Kr'][hp], FM['Vt'][hp]
                    At, Bt, Kt, Rt, rk, eG = (FM[n_][hp] for n_ in ['At', 'Bt', 'Kt', 'Rt', 'rk', 'eG'])
                    Rtb = FM['Rtb'][hp]
                    kn = lambda n_: '%s%d' % (n_, hp)
                    plap, plk = pl
                    P.op('pe', lambda e, hp=hp: e.matmul(plap[:, 0:NB], lhsT=LW[:, hp * 128:(hp + 1) * 128], rhs=LX[:],
                                                         start=True, stop=True), reads=['LW', 'LX'], writes=[plk])
                    P.op('pe', lambda e, hp=hp: e.matmul(plap[:, NB:2 * NB], lhsT=LW2[:, hp * 128:(hp + 1) * 128], rhs=LX[:],
                                                         start=True, stop=True), reads=['LW', 'LX'], writes=[plk])
                    sg, a, Gs, Gp, eGn, eGp, kk, sq, rn, kkn, t1, kp = (tmp[n_] for n_ in
                                                                         ['sg', 'a', 'Gs', 'Gp', 'eGn', 'eGp', 'kk', 'sq', 'rn', 'kkn', 't1', 'kp'])
                    P.op('act', lambda e, hp=hp: e.activation(out=sg[:], in_=plap[:, 0:NB], func=AF.Sigmoid, bias=pcol('w0', hp)),
                         reads=[plk, 'pv'], writes=['sg'])
                    P.op('act', lambda e, hp=hp: e.activation(out=a[:], in_=plap[:, NB:2 * NB], func=AF.Sigmoid, bias=pcol('a0', hp)),
                         reads=[plk, 'pv'], writes=['a'])
                    P.op('dve', lambda e: e.tensor_tensor_scan(out=Gs[:], data0=scanmask[:], data1=sg[:], initial=0.0,
                                                               op0=ALU.mult, op1=ALU.add), reads=['scanmask', 'sg'], writes=['Gs'])
                    P.op('pool', lambda e: e.tensor_tensor(out=Gp[:], in0=Gs[:], in1=sg[:], op=ALU.subtract),
                         reads=['Gs', 'sg'], writes=['Gp'])
                    P.op('act', lambda e, eG=eG: e.activation(out=eG[:], in_=Gs[:], func=AF.Exp, scale=-CDEC),
                         reads=['Gs'], writes=[kn('eG')])
                    P.op('act', lambda e: e.activation(out=eGn[:], in_=Gs[:], func=AF.Exp, scale=CDEC),
                         reads=['Gs'], writes=['eGn'])
                    P.op('act', lambda e: e.activation(out=eGp[:], in_=Gp[:], func=AF.Exp, scale=-CDEC),
                         reads=['Gp'], writes=['eGp'])
                    P.op('dve', lambda e, Kr=Kr, hp=hp: e.tensor_scalar(out=kk[:], in0=Kr[:], scalar1=pcol('k_k', hp), scalar2=None,
                                                                        op0=ALU.mult), reads=[kn('Kr'), 'pv'], writes=['kk'])
                    P.op('pool', lambda e: e.tensor_tensor(out=sq[:], in0=kk[:], in1=kk[:], op=ALU.mult), reads=['kk'], writes=['sq'])
                    psap, psk = pss
                    P.op('pe', lambda e: e.matmul(psap, lhsT=onesblk[:], rhs=sq[:], start=True, stop=True),
                         reads=['onesblk', 'sq'], writes=[psk])
                    P.op('act', lambda e: e.activation(out=rn[:], in_=psap, func=AF.Sqrt), reads=[psk], writes=['rn'])
                    P.op('dve', lambda e: e.tensor_scalar(out=rn[:], in0=rn[:], scalar1=1e-12, scalar2=None, op0=ALU.max),
                         reads=['rn'], writes=['rn'])
                    P.op('dve', lambda e: e.reciprocal(out=rn[:], in_=rn[:]), reads=['rn'], writes=['rn'])
                    P.op('dve', lambda e: e.tensor_tensor(out=kkn[:], in0=kk[:], in1=rn[:], op=ALU.mult),
                         reads=['kk', 'rn'], writes=['kkn'])
                    P.op('dve', lambda e, hp=hp: e.tensor_scalar(out=t1[:], in0=a[:], scalar1=-1.0, scalar2=pcol('k_a', hp),
                                                                 op0=ALU.add, op1=ALU.mult), reads=['a', 'pv'], writes=['t1'])
                    P.op('pool', lambda e: e.tensor_scalar(out=t1[:], in0=t1[:], scalar1=1.0, scalar2=None, op0=ALU.add),
                         reads=['t1'], writes=['t1'])
                    P.op('dve', lambda e, Kr=Kr: e.tensor_tensor(out=kp[:], in0=Kr[:], in1=t1[:], op=ALU.mult),
                         reads=[kn('Kr'), 't1'], writes=['kp'])
                    P.op('dve', lambda e, At=At: e.scalar_tensor_tensor(out=At[:], in0=kkn[:], scalar=-1.0, in1=eGp[:],
                                                                         op0=ALU.mult, op1=ALU.mult),
                         reads=['kkn', 'eGp'], writes=[kn('At')])
                    P.op('pool', lambda e: e.tensor_tensor(out=t1[:], in0=kkn[:], in1=a[:], op=ALU.mult),
                         reads=['kkn', 'a'], writes=['t1'])
                    P.op('pool', lambda e, Bt=Bt: e.tensor_tensor(out=Bt[:], in0=t1[:], in1=eGn[:], op=ALU.mult),
                         reads=['t1', 'eGn'], writes=[kn('Bt')])
                    P.op('dve', lambda e, Kt=Kt: e.tensor_tensor(out=Kt[:], in0=kp[:], in1=eGn[:], op=ALU.mult),
                         reads=['kp', 'eGn'], writes=[kn('Kt')])
                    if own:
                        P.op('pool', lambda e, Rt=Rt, Rr=Rr, eG=eG: e.tensor_tensor(out=Rt[:], in0=Rr[:], in1=eG[:], op=ALU.mult),
                             reads=[kn('Rr'), kn('eG')], writes=[kn('Rt')])
                        P.op('pool', lambda e, Rt=Rt, Rtb=Rtb: e.tensor_copy(out=Rtb[:], in_=Rt[:]), reads=[kn('Rt')], writes=[kn('Rtb')])
                        P.op('dve', lambda e, rk=rk, Rr=Rr, hp=hp: e.scalar_tensor_tensor(out=rk[:], in0=Rr[:], scalar=pcol('r_k', hp),
                                                                                           in1=kp[:], op0=ALU.mult, op1=ALU.mult),
                             reads=[kn('Rr'), 'kp', 'pv'], writes=[kn('rk')])
                    yb_i = hp % 2
                    def chunk_ops(c, par):
                        gc = tb * NCH + c
                        cs = slice(c * 64, (c + 1) * 64)
                        Hc, Hn = HH[hp][gc % 2], HH[hp][(gc + 1) % 2]
                        hck, hnk = 'H%d_%d' % (hp, gc % 2), 'H%d_%d' % (hp, (gc + 1) % 2)
                        tmk, mmk = 'TM%d' % par, 'MM%d' % par
                        ptap, ptk = psT[par]
                        for i_, (X, xn) in enumerate([(Vt, 'Vt'), (At, 'At'), (Bt, 'Bt'), (Kt, 'Kt')]):
                            for h2 in range(2):
                                hs = slice(64 * h2, 64 * h2 + 64)
                                P.op('pe', lambda e, X=X, hs=hs, i_=i_, cs=cs, ptap=ptap: e.matmul(
                                    ptap[hs, i_ * 64:(i_ + 1) * 64], lhsT=X[hs, cs], rhs=identb[hs, hs], start=True, stop=True),
                                    reads=[kn(xn), 'identb'], writes=[ptk])
                        evac_copy(TM[par][:].rearrange("p a b -> p (a b)"), ptap, [ptk], [tmk])
                        yield 'pre'
                        pmap, pmk = psM[par]
                        prs = [(Bt, At, 'Bt', 'At'), (At, Bt, 'At', 'Bt'), (Kt, At, 'Kt', 'At')]
                        if own:
                            prs += [(Bt, Rtb, 'Bt', 'Rtb'), (Kt, Rtb, 'Kt', 'Rtb')]
                        for i_, (L_, R_, ln, rn_) in enumerate(prs):
                            for h2 in range(2):
                                hs = slice(64 * h2, 64 * h2 + 64)
                                P.op('pe', lambda e, L_=L_, R_=R_, hs=hs, i_=i_, cs=cs, pmap=pmap: e.matmul(
                                    pmap[hs, i_ * 64:(i_ + 1) * 64], lhsT=L_[hs, cs], rhs=R_[hs, cs], start=True, stop=True),
                                    reads=[kn(ln), kn(rn_)], writes=[pmk])
                        ncol = 64 * len(prs)
                        P.op('dve', lambda e, par=par, pmap=pmap, ncol=ncol: e.tensor_tensor(
                            out=MM[par][:, 0:ncol], in0=pmap[:, 0:ncol], in1=mask320[:, 0:ncol], op=ALU.mult),
                            reads=[pmk, 'mask320'], writes=[mmk])
                        yield 'pre'
                        ttk = ['TT%d_%d' % (par, i_) for i_ in range(2)]
                        ppk = ['PP%d_%d' % (par, i_) for i_ in range(2)]
                        P.op('pool', lambda e, par=par: e.tensor_tensor(out=TT[par][0][:], in0=MM[par][:, 0:64], in1=iit[:, 0:64], op=ALU.add),
                             reads=[mmk, 'ii'], writes=[ttk[0]])
                        Pcur, Pk = MM[par][:, 0:128], mmk
                        tcur = 0
                        pdap, pdk = psD[par]
                        peap, pek = psE[par]

                        def square(Pcur, Pk, dst, dstk):
                            for h2 in range(2):
                                hs = slice(64 * h2, 64 * h2 + 64)
                                P.op('pe', lambda e, Pcur=Pcur, hs=hs: e.matmul(
                                    pdap[hs, 0:64], lhsT=Pcur[hs, 64:128], rhs=Pcur[hs, 0:64], start=True, stop=True),
                                    reads=[Pk], writes=[pdk])
                                P.op('pe', lambda e, Pcur=Pcur, hs=hs: e.matmul(
                                    pdap[hs, 64:128], lhsT=Pcur[hs, 0:64], rhs=Pcur[hs, 64:128], start=True, stop=True),
                                    reads=[Pk], writes=[pdk])
                            evac_copy(dst[:], pdap, [pdk], [dstk])
                        square(Pcur, Pk, PP[par][0], ppk[0])
                        yield 'pre'
                        Pcur, Pk = PP[par][0][:], ppk[0]
                        for lvl in range(1, 6):
                            Tc = TT[par][tcur]
                            Tn = TT[par][1 - tcur]
                            for h2 in range(2):
                                hs = slice(64 * h2, 64 * h2 + 64)
                                P.op('pe', lambda e, Tc=Tc, Pcur=Pcur, hs=hs: e.matmul(
                                    peap[hs, 0:64], lhsT=Pcur[hs, 64:128], rhs=Tc[hs, :], start=True, stop=True),
                                    reads=[ttk[tcur], Pk], writes=[pek])
                            if lvl < 5:
                                square(Pcur, Pk, PP[par][lvl % 2], ppk[lvl % 2])
                            P.op('dve', lambda e, Tc=Tc, Tn=Tn: e.tensor_tensor(out=Tn[:], in0=peap[:, 0:64], in1=Tc[:], op=ALU.add),
                                 reads=[pek, ttk[tcur]], writes=[ttk[1 - tcur]])
                            if lvl < 5:
                                Pcur, Pk = PP[par][lvl % 2][:], ppk[lvl % 2]
                            tcur = 1 - tcur
                            yield 'pre'
                        Tt = TT[par][tcur]
                        tk = ttk[tcur]
                        pxap, pxk = psX[par]
                        for h2 in range(2):
                            hs = slice(64 * h2, 64 * h2 + 64)
                            P.op('pe', lambda e, hs=hs, par=par, pxap=pxap: e.matmul(
                                pxap[hs, :], lhsT=MM[par][hs, 128:192], rhs=TM[par][hs, 0, :], start=True, stop=True),
                                reads=[mmk, tmk], writes=[pxk])
                        evac_copy(X2s[par][:], pxap, [pxk], ['X2s%d' % par])
                        yield 'pre'
                        pwap, pwk = psW[par]
                        for h2 in range(2):
                            hs = slice(64 * h2, 64 * h2 + 64)
                            P.op('pe', lambda e, hs=hs, par=par, Tt=Tt, pwap=pwap: e.matmul(
                                pwap[hs, 0:64], lhsT=Tt[hs, :], rhs=X2s[par][hs, :], start=True, stop=True),
                                reads=[tk, 'X2s%d' % par], writes=[pwk])
                            P.op('pe', lambda e, hs=hs, par=par, Tt=Tt, pwap=pwap: e.matmul(
                                pwap[hs, 64:128], lhsT=TM[par][hs, 1, :], rhs=Tt[hs, :], start=True, stop=True),
                                reads=[tk, tmk], writes=[pwk])
                            P.op('pe', lambda e, hs=hs, par=par, pwap=pwap: e.matmul(
                                pwap[hs, 128:192], lhsT=TM[par][hs, 3, :], rhs=TM[par][hs, 0, :], start=True, stop=True),
                                reads=[tmk], writes=[pwk])
                        wwk = 'WW%d' % par
                        evac_copy(WW[par][:], pwap[:, 0:128], [pwk], [wwk])
                        gam = eG[:, c * 64 + 63:c * 64 + 64]
                        P.op('dve', lambda e, par=par, gam=gam, pwap=pwap: e.tensor_scalar(out=Dg[par][:], in0=pwap[:, 128:192], scalar1=gam,
                                                                                            scalar2=None, op0=ALU.mult),
                             reads=[pwk, kn('eG')], writes=['Dg%d' % par])
                        yield 'pre'
                        yield 'seq'
                        P.op('dve', lambda e, par=par, gam=gam, Hc=Hc: e.scalar_tensor_tensor(
                            out=HDg[par][:], in0=Hc[:], scalar=gam, in1=Dg[par][:], op0=ALU.mult, op1=ALU.add),
                            reads=[hck, kn('eG'), 'Dg%d' % par], writes=['HDg%d' % par])
                        puap, puk = psU[par]
                        for h2 in range(2):
                            hs = slice(64 * h2, 64 * h2 + 64)
                            P.op('pe', lambda e, hs=hs, par=par, Hc=Hc, puap=puap: e.matmul(
                                puap[hs, :], lhsT=WW[par][hs, 64:128], rhs=Hc[hs, :], start=True, stop=True),
                                reads=[wwk, hck], writes=[puk])
                        P.op('dve', lambda e, par=par, puap=puap: e.tensor_tensor(out=Us[par][:], in0=puap, in1=WW[par][:, 0:64], op=ALU.add),
                             reads=[puk, wwk], writes=['Us%d' % par])
                        if own:
                            pyap, pyk = psY[yb_i]
                            for h2 in range(2):
                                hs = slice(64 * h2, 64 * h2 + 64)
                                P.op('pe', lambda e, hs=hs, Rt=Rt, cs=cs, Hc=Hc, pyap=pyap: e.matmul(
                                    pyap[hs, cs], lhsT=Rt[hs, cs], rhs=Hc[hs, :], start=True, stop=False),
                                    reads=[kn('Rt'), hck], writes=[pyk])
                                P.op('pe', lambda e, hs=hs, par=par, cs=cs, pyap=pyap: e.matmul(
                                    pyap[hs, cs], lhsT=MM[par][hs, 192:256], rhs=Us[par][hs, :], start=False, stop=False),
                                    reads=[mmk, 'Us%d' % par], writes=[pyk])
                                P.op('pe', lambda e, hs=hs, par=par, cs=cs, pyap=pyap: e.matmul(
                                    pyap[hs, cs], lhsT=MM[par][hs, 256:320], rhs=TM[par][hs, 0, :], start=False, stop=True),
                                    reads=[mmk, tmk], writes=[pyk])
                            pcap, pck = pcf[yb_i]
                            pgap, pgk = pgate[yb_i]
                            for h2 in range(2):
                                hs = slice(64 * h2, 64 * h2 + 64)
                                P.op('pe', lambda e, hs=hs, rk=rk, cs=cs, c=c, pcap=pcap: e.matmul(
                                    pcap[hs, c:c + 1], lhsT=rk[hs, cs], rhs=onesblk[hs, 64 * (hs.start // 64):64 * (hs.start // 64) + 1],
                                    start=True, stop=True), reads=[kn('rk'), 'onesblk'], writes=[pck])
                                h = 2 * hp + h2
                                P.op('pe', lambda e, hs=hs, cs=cs, h=h, pgap=pgap: e.matmul(
                                    pgap[hs, cs], lhsT=SGX[:, cs], rhs=GU[:, h * 64:(h + 1) * 64], start=True, stop=True),
                                    reads=['SGX', 'GU'], writes=[pgk])
                            P.op('pool', lambda e, par=par, c=c, yb_i=yb_i: e.tensor_copy(out=Yc[yb_i][:, c, :], in_=TM[par][:, 0, :]),
                                 reads=[tmk], writes=['Yc%d' % yb_i])
                        phap, phk = psH[par]
                        for h2 in range(2):
                            hs = slice(64 * h2, 64 * h2 + 64)
                            P.op('pe', lambda e, hs=hs, par=par, phap=phap: e.matmul(
                                phap[hs, :], lhsT=TM[par][hs, 2, :], rhs=Us[par][hs, :], start=True, stop=True),
                                reads=[tmk, 'Us%d' % par], writes=[phk])
                        P.op('dve', lambda e, par=par, gam=gam, Hn=Hn, phap=phap: e.scalar_tensor_tensor(
                            out=Hn[:], in0=phap, scalar=gam, in1=HDg[par][:], op0=ALU.mult, op1=ALU.add),
                            reads=[phk, kn('eG'), 'HDg%d' % par], writes=[hnk])
                    for c0 in range(0, NCH, 2):
                        gens = [chunk_ops(c0, 0), chunk_ops(c0 + 1, 1)]
                        pre_done = [False, False]
                        while not all(pre_done):
                            for gi_, g_ in enumerate(gens):
                                if not pre_done[gi_]:
                                    if next(g_) == 'seq':
                                        pre_done[gi_] = True
                        for g_ in gens:
                            for _ in g_:
                                pass
                    if own:
                        pyap, pyk = psY[yb_i]
                        pcap, pck = pcf[yb_i]
                        pgap, pgk = pgate[yb_i]
                        Y = Yb[yb_i]
                        yk = 'Yb%d' % yb_i
                        V3 = Yc[yb_i]
                        vk = 'Yc%d' % yb_i
                        S = st[yb_i]
                        sk = 'st%d' % yb_i
                        y3 = lambda ap: ap.rearrange("p (c v) -> p c v", v=64)
                        bc = lambda ap: ap.unsqueeze(2).to_broadcast([128, NCH, 64])
                        evac_copy(Y[:].rearrange("p c v -> p (c v)"), pyap, [pyk], [yk])
                        P.op('dve', lambda e, Y=Y, S=S: e.tensor_reduce(out=S[:, 0:NCH], in_=Y[:], axis=AX.X, op=ALU.add),
                             reads=[yk], writes=[sk])
                        P.op('dve', lambda e, S=S: e.tensor_scalar(out=S[:, 0:NCH], in0=S[:, 0:NCH], scalar1=1.0 / 64, scalar2=None, op0=ALU.mult),
                             reads=[sk], writes=[sk])
                        P.op('dve', lambda e, Y=Y, S=S: e.tensor_tensor(out=Y[:], in0=Y[:], in1=bc(S[:, 0:NCH]), op=ALU.subtract),
                             reads=[yk, sk], writes=[yk])
                        sqb = tmp['sq']
                        P.op('pool', lambda e, Y=Y: e.tensor_tensor(out=y3(sqb[:]), in0=Y[:], in1=Y[:], op=ALU.mult),
                             reads=[yk], writes=['sq'])
                        P.op('dve', lambda e, S=S: e.tensor_reduce(out=S[:, NCH:2 * NCH], in_=y3(sqb[:]), axis=AX.X, op=ALU.add),
                             reads=['sq'], writes=[sk])
                        P.op('dve', lambda e, S=S: e.tensor_scalar(out=S[:, NCH:2 * NCH], in0=S[:, NCH:2 * NCH], scalar1=1.0 / 64, scalar2=64e-5,
                                                                   op0=ALU.mult, op1=ALU.add), reads=[sk], writes=[sk])
                        P.op('act', lambda e, S=S: e.activation(out=S[:, NCH:2 * NCH], in_=S[:, NCH:2 * NCH], func=AF.Sqrt),
                             reads=[sk], writes=[sk])
                        P.op('dve', lambda e, S=S: e.reciprocal(out=S[:, NCH:2 * NCH], in_=S[:, NCH:2 * NCH]), reads=[sk], writes=[sk])
                        P.op('dve', lambda e, Y=Y, S=S: e.tensor_tensor(out=Y[:], in0=Y[:], in1=bc(S[:, NCH:2 * NCH]), op=ALU.mult),
                             reads=[yk, sk], writes=[yk])
                        gw = gnw[:, hp, :].unsqueeze(1).to_broadcast([128, NCH, 64])
                        gb = gnb[:, hp, :].unsqueeze(1).to_broadcast([128, NCH, 64])
                        P.op('pool', lambda e, Y=Y, gw=gw: e.tensor_tensor(out=Y[:], in0=Y[:], in1=gw, op=ALU.mult),
                             reads=[yk, 'gnw'], writes=[yk])
                        P.op('pool', lambda e, Y=Y, gb=gb: e.tensor_tensor(out=Y[:], in0=Y[:], in1=gb, op=ALU.add),
                             reads=[yk, 'gnb'], writes=[yk])
                        cf = cfs[yb_i]
                        ck = 'cfs%d' % yb_i
                        evac_copy(cf[:], pcap, [pck], [ck])
                        P.op('dve', lambda e, V3=V3, cf=cf: e.tensor_tensor(out=V3[:], in0=V3[:], in1=bc(cf[:]), op=ALU.mult),
                             reads=[vk, ck], writes=[vk])
                        P.op('pool', lambda e, Y=Y, V3=V3: e.tensor_tensor(out=Y[:], in0=Y[:], in1=V3[:], op=ALU.add),
                             reads=[yk, vk], writes=[yk])
                        P.op('dve', lambda e, Y=Y, pgap=pgap: e.tensor_tensor(out=Y[:].rearrange("p c v -> p (c v)"),
                                                                              in0=Y[:].rearrange("p c v -> p (c v)"), in1=pgap, op=ALU.mult),
                             reads=[yk, pgk], writes=[yk])
                        ob = tb - ownb
                        if stage == 'rwkv':
                            for c in range(NCH):
                                P.dma(ya_dbg[ob * NCH + c, hp], Y[:, c, :], reads=[yk])
                        ptap2, ptk2 = pyt[yb_i]
                        for c in range(NCH):
                            for h2 in range(2):
                                hs = slice(64 * h2, 64 * h2 + 64)
                                P.op('pe', lambda e, Y=Y, hs=hs, c=c, ptap2=ptap2: e.matmul(
                                    ptap2[hs, c * 64:(c + 1) * 64], lhsT=Y[hs, c, :], rhs=ident[hs, hs], start=True, stop=True),
                                    reads=[yk, 'ident'], writes=[ptk2])
                        yT = yaTs[yb_i]
                        ytk = 'yaTs%d' % yb_i
                        evac_copy(yT[:], ptap2, [ptk2], [ytk])
                        P.dma(yaT_d[hp * 128:(hp + 1) * 128, ob * NB:(ob + 1) * NB], yT[:], reads=[ytk], writes=['dram_yaT_%d_%d' % (hp, ob)])
            P.flush()
        if stage in ('hgrn', 'full', 'fullA'):
          P.barrier()
          with ExitStack() as es:
            PSA.reset()
            scanmask_h = P.sb(es, 'scanmask_h', [128, NB])
            iu2 = P.sb(es, 'iu2', [128, 64])
            P.dma(scanmask_h[:], scanmask_d, writes=['scanmask_h'])
            P.dma(iu2[:], iu2_d, writes=['iu2'])
            wH = P.sb(es, 'wH', [128, 8, 2048], BF16)
            NWB = P.sb(es, 'NWB', [128, 512])
            o_, n_ = ROWS['hg_nw']
            P.dma(NWB[:], rows_d[:, o_:o_ + n_].partition_broadcast(128), writes=['NWB'])
            lbt = P.sb(es, 'lbt', [128, 12])
            P.op('dve', lambda e: e.tensor_tensor(out=lbt[:, 0:4], in0=pvt[:, PV['lb0']:PV['lb0'] + 4],
                                                  in1=pvt[:, PV['lb1']:PV['lb1'] + 4], op=ALU.subtract),
                 reads=['pv'], writes=['lbt'])
            P.op('act', lambda e: e.activation(out=lbt[:, 0:4], in_=lbt[:, 0:4], func=AF.Sigmoid), reads=['lbt'], writes=['lbt'])
            P.op('dve', lambda e: e.tensor_scalar(out=lbt[:, 4:8], in0=lbt[:, 0:4], scalar1=-1.0, scalar2=1.0, op0=ALU.mult, op1=ALU.add),
                 reads=['lbt'], writes=['lbt'])
            P.op('dve', lambda e: e.tensor_scalar(out=lbt[:, 8:12], in0=lbt[:, 4:8], scalar1=-1.0, scalar2=None, op0=ALU.mult),
                 reads=['lbt'], writes=['lbt'])
            with ExitStack() as es1:
                stg = [P.sb(es1, 'whstg', [128, 2048]) for _ in range(2)]
                for dc in range(8):
                    s = stg[dc % 2]
                    k = 'whstg%d' % (dc % 2)
                    P.dma(s[:], w_in[dc * 128:(dc + 1) * 128, C_Q:C_GATE], writes=[k])
                    P.op('dve' if dc % 2 else 'pool', lambda e, s=s, dc=dc: e.tensor_copy(out=wH[:, dc, :], in_=s[:]),
                         reads=[k], writes=['wH'])
                P.flush()
            P.barrier()
            xstg = [P.sb(es, 'hxstg', [128, NB]) for _ in range(2)]
            xb = [P.sb(es, 'hxb', [128, 8, NB], BF16) for _ in range(2)]
            NSET = 4
            fm = [{nm: P.sb(es, 'h' + nm, [128, NB]) for nm in ['q', 'sf', 'lf', 'k', 'b', 'eb', 'enb', 'qt', 'kt']} for _ in range(NSET)]
            Vtok = [[P.sb(es, 'Vtok', [128, 512]) for _ in range(NB // 128)] for _ in range(2)]
            SGo = [P.sb(es, 'SGo', [128, 512]) for _ in range(NB // 128)]
            KTs = [[P.sb(es, 'KTs', [128, 128]) for _ in range(2)] for _ in range(4)]
            ATs = [[P.sb(es, 'ATs', [128, 64]) for _ in range(2)] for _ in range(4)]
            Dgs = [[P.sb(es, 'hDg', [128, 128]) for _ in range(2)] for _ in range(4)]
            SS = [[P.sb(es, 'hS', [128, 128]) for _ in range(2)] for _ in range(4)]
            for h in range(4):
                P.op('pool', lambda e, h=h: e.memset(SS[h][0][:], 0.0), writes=['hS%d_0' % h])
            Ot = P.sb(es, 'Ot', [128, 4, 128])
            Osq = P.sb(es, 'Osq', [128, 4, 128])
            ost = P.sb(es, 'ost', [128, 8])
            ybTs = P.sb(es, 'ybTs', [128, 4, 128], BF16)
            pj = [PSA.alloc(NB, 0), PSA.alloc(NB, 1)]
            pv_ = PSA.alloc(512, 2)
            pog = PSA.alloc(512, 3)
            pK = [PSA.alloc(128, 4), PSA.alloc(128, 5)]
            pA = [PSA.alloc(64, 4), PSA.alloc(64, 5)]
            pD = [PSA.alloc(256, 4), PSA.alloc(256, 5)]
            pO = [PSA.alloc(512, 6), PSA.alloc(512, 7)]
            pT = pv_
            evq2 = [0]

            def evac2(out, in_, reads, writes):
                evq2[0] += 1
                if evq2[0] % 2:
                    P.op('act', lambda e: e.copy(out=out, in_=in_), reads=reads, writes=writes)
                else:
                    P.op('dve', lambda e: e.tensor_copy(out=out, in_=in_), reads=reads, writes=writes)

            for tb in range(nblk):
                own = tb >= ownb
                ob = tb - ownb
                t0 = tb * NB
                xbt = xb[tb % 2]
                xk = 'hxb%d' % (tb % 2)
                for dc in range(8):
                    s = xstg[dc % 2]
                    k = 'hxstg%d' % (dc % 2)
                    P.dma(s[:], xT[dc * 128:(dc + 1) * 128, t0:t0 + NB], writes=[k])
                    P.op('pool' if dc % 2 else 'dve', lambda e, s=s, dc=dc, xbt=xbt: e.tensor_copy(out=xbt[:, dc, :], in_=s[:]),
                         reads=[k], writes=[xk])
                vt = Vtok[tb % 2]
                for tt in range(NB // 128):
                    vk = 'Vtok%d_%d' % (tb % 2, tt)
                    pap, pk = pv_
                    for dc in range(8):
                        P.op('pe', lambda e, pap=pap, dc=dc, tt=tt, xbt=xbt: e.matmul(
                            pap, lhsT=xbt[:, dc, tt * 128:(tt + 1) * 128], rhs=wH[:, dc, 1024:1536], start=(dc == 0), stop=(dc == 7)),
                            reads=['wH', xk], writes=[pk])
                    evac2(vt[tt][:], pap, [pk], [vk])
                    if own:
                        pap, pk = pog
                        for dc in range(8):
                            P.op('pe', lambda e, pap=pap, dc=dc, tt=tt, xbt=xbt: e.matmul(
                                pap, lhsT=xbt[:, dc, tt * 128:(tt + 1) * 128], rhs=wH[:, dc, 1536:2048], start=(dc == 0), stop=(dc == 7)),
                                reads=['wH', xk], writes=[pk])
                        P.op('act', lambda e, pap=pap, tt=tt: e.activation(out=SGo[tt][:], in_=pap, func=AF.Sigmoid),
                             reads=[pk], writes=['SGo%d' % tt])
                for h in range(4):
                    F = fm[h % NSET]
                    fk = lambda n_: 'h%s%d' % (n_, h % NSET)
                    pap, pk = pj[0]
                    if own:
                        for dc in range(8):
                            P.op('pe', lambda e, pap=pap, dc=dc, h=h, xbt=xbt: e.matmul(
                                pap, lhsT=wH[:, dc, h * 128:(h + 1) * 128], rhs=xbt[:, dc, :], start=(dc == 0), stop=(dc == 7)),
                                reads=['wH', xk], writes=[pk])
                        P.op('act', lambda e, pap=pap, F=F: e.activation(out=F['q'][:], in_=pap, func=AF.Silu), reads=[pk], writes=[fk('q')])
                    pap, pk = pj[1]
                    for dc in range(8):
                        P.op('pe', lambda e, pap=pap, dc=dc, h=h, xbt=xbt: e.matmul(
                            pap, lhsT=wH[:, dc, 512 + h * 128:512 + (h + 1) * 128], rhs=xbt[:, dc, :], start=(dc == 0), stop=(dc == 7)),
                            reads=['wH', xk], writes=[pk])
                    P.op('act', lambda e, pap=pap, F=F: e.activation(out=F['sf'][:], in_=pap, func=AF.Sigmoid), reads=[pk], writes=[fk('sf')])
                    P.op('act', lambda e, F=F, h=h: e.activation(out=F['lf'][:], in_=F['sf'][:], func=AF.Ln, bias=lbt[:, h:h + 1],
                                                                 scale=lbt[:, 4 + h:5 + h]), reads=[fk('sf'), 'lbt'], writes=[fk('lf')])
                    P.op('dve', lambda e, F=F, h=h: e.tensor_scalar(out=F['k'][:], in0=F['sf'][:], scalar1=lbt[:, 8 + h:9 + h],
                                                                    scalar2=lbt[:, 4 + h:5 + h], op0=ALU.mult, op1=ALU.add),
                         reads=[fk('sf'), 'lbt'], writes=[fk('k')])
                    P.op('dve', lambda e, F=F: e.tensor_tensor_scan(out=F['b'][:], data0=scanmask_h[:], data1=F['lf'][:], initial=0.0,
                                                                    op0=ALU.mult, op1=ALU.add), reads=['scanmask_h', fk('lf')], writes=[fk('b')])
                    P.op('act', lambda e, F=F: e.activation(out=F['eb'][:], in_=F['b'][:], func=AF.Exp), reads=[fk('b')], writes=[fk('eb')])
                    P.op('act', lambda e, F=F: e.activation(out=F['enb'][:], in_=F['b'][:], func=AF.Exp, scale=-1.0),
                         reads=[fk('b')], writes=[fk('enb')])
                    P.op('pool', lambda e, F=F: e.tensor_tensor(out=F['kt'][:], in0=F['k'][:], in1=F['enb'][:], op=ALU.mult),
                         reads=[fk('k'), fk('enb')], writes=[fk('kt')])
                    if own:
                        P.op('pool', lambda e, F=F: e.tensor_tensor(out=F['qt'][:], in0=F['q'][:], in1=F['eb'][:], op=ALU.mult),
                             reads=[fk('q'), fk('eb')], writes=[fk('qt')])
                for cp in range(NB // 128):
                    vk = 'Vtok%d_%d' % (tb % 2, cp)
                    for c2 in range(2):
                      for h in range(4):
                            F = fm[h]
                            fk = lambda n_, h=h: 'h%s%d' % (n_, h)
                            c = 2 * cp + c2
                            gc = tb * NCH + c
                            cs = slice(c * 64, (c + 1) * 64)
                            ps_ = slice(64 * c2, 64 * c2 + 64)
                            Sc, Sn = SS[h][gc % 2], SS[h][(gc + 1) % 2]
                            sck, snk = 'hS%d_%d' % (h, gc % 2), 'hS%d_%d' % (h, (gc + 1) % 2)
                            kts, ktk = KTs[h][c2], 'KTs%d_%d' % (h, c2)
                            ats, atk = ATs[h][c2], 'ATs%d_%d' % (h, c2)
                            dgs, dgk = Dgs[h][c2], 'hDg%d_%d' % (h, c2)
                            pkap, pkk = pK[h % 2]
                            P.op('pe', lambda e, F=F, cs=cs, ps_=ps_, pkap=pkap: e.matmul(
                                pkap[ps_, :], lhsT=F['kt'][:, cs], rhs=ident[:], start=True, stop=True),
                                reads=[fk('kt'), 'ident'], writes=[pkk])
                            evac2(kts[ps_, :], pkap[ps_, :], [pkk], [ktk])
                            if own:
                                paap, pak = pA[h % 2]
                                P.op('pe', lambda e, F=F, cs=cs, ps_=ps_, paap=paap: e.matmul(
                                    paap[ps_, :], lhsT=F['kt'][:, cs], rhs=F['qt'][:, cs], start=True, stop=True),
                                    reads=[fk('kt'), fk('qt')], writes=[pak])
                                P.op('dve', lambda e, ats=ats, ps_=ps_, paap=paap: e.tensor_tensor(
                                    out=ats[ps_, :], in0=paap[ps_, :], in1=iu2[ps_, :], op=ALU.mult),
                                    reads=[pak, 'iu2'], writes=[atk])
                            pdap, pdk = pD[h % 2]
                            P.op('pe', lambda e, kts=kts, ps_=ps_, cp=cp, h=h, c2=c2, pdap=pdap, vt=vt: e.matmul(
                                pdap[:, c2 * 128:(c2 + 1) * 128], lhsT=kts[ps_, :], rhs=vt[cp][ps_, h * 128:(h + 1) * 128],
                                start=True, stop=True), reads=[ktk, vk], writes=[pdk])
                            gl = F['eb'][:, c * 64 + 63:c * 64 + 64]
                            P.op('dve', lambda e, dgs=dgs, pdap=pdap, c2=c2, gl=gl: e.tensor_scalar(
                                out=dgs[:], in0=pdap[:, c2 * 128:(c2 + 1) * 128], scalar1=gl, scalar2=None, op0=ALU.mult),
                                reads=[pdk, fk('eb')], writes=[dgk])
                            if own:
                                poap, pok = pO[cp]
                                P.op('pe', lambda e, ats=ats, ps_=ps_, cp=cp, h=h, poap=poap, vt=vt: e.matmul(
                                    poap[ps_, h * 128:(h + 1) * 128], lhsT=ats[ps_, :], rhs=vt[cp][ps_, h * 128:(h + 1) * 128],
                                    start=True, stop=False), reads=[atk, vk], writes=[pok])
                                P.op('pe', lambda e, F=F, cs=cs, ps_=ps_, h=h, Sc=Sc, poap=poap: e.matmul(
                                    poap[ps_, h * 128:(h + 1) * 128], lhsT=F['qt'][:, cs], rhs=Sc[:], start=False, stop=True),
                                    reads=[fk('qt'), sck], writes=[pok])
                            P.op('dve', lambda e, Sc=Sc, Sn=Sn, gl=gl, dgs=dgs: e.scalar_tensor_tensor(
                                out=Sn[:], in0=Sc[:], scalar=gl, in1=dgs[:], op0=ALU.mult, op1=ALU.add),
                                reads=[sck, fk('eb'), dgk], writes=[snk])
                if own:
                    for cp in range(NB // 128):
                        poap, pok = pO[cp]
                        evac2(Ot[:].rearrange("p h v -> p (h v)"), poap, [pok], ['Ot'])
                        P.op('pool', lambda e: e.tensor_tensor(out=Osq[:], in0=Ot[:], in1=Ot[:], op=ALU.mult), reads=['Ot'], writes=['Osq'])
                        P.op('dve', lambda e: e.tensor_reduce(out=ost[:, 0:4], in_=Osq[:], axis=AX.X, op=ALU.add), reads=['Osq'], writes=['ost'])
                        P.op('dve', lambda e: e.tensor_scalar(out=ost[:, 0:4], in0=ost[:, 0:4], scalar1=1.0 / 128, scalar2=1e-6,
                                                              op0=ALU.mult, op1=ALU.add), reads=['ost'], writes=['ost'])
                        P.op('act', lambda e: e.activation(out=ost[:, 0:4], in_=ost[:, 0:4], func=AF.Sqrt), reads=['ost'], writes=['ost'])
                        P.op('dve', lambda e: e.reciprocal(out=ost[:, 0:4], in_=ost[:, 0:4]), reads=['ost'], writes=['ost'])
                        P.op('dve', lambda e: e.tensor_tensor(out=Ot[:], in0=Ot[:], in1=ost[:, 0:4].unsqueeze(2).to_broadcast([128, 4, 128]),
                                                              op=ALU.mult), reads=['Ot', 'ost'], writes=['Ot'])
                        P.op('pool', lambda e: e.tensor_tensor(out=Ot[:].rearrange("p h v -> p (h v)"), in0=Ot[:].rearrange("p h v -> p (h v)"),
                                                               in1=NWB[:], op=ALU.mult), reads=['Ot', 'NWB'], writes=['Ot'])
                        P.op('dve', lambda e, cp=cp: e.tensor_tensor(out=Ot[:].rearrange("p h v -> p (h v)"), in0=Ot[:].rearrange("p h v -> p (h v)"),
                                                                     in1=SGo[cp][:], op=ALU.mult), reads=['Ot', 'SGo%d' % cp], writes=['Ot'])
                        tok0 = ob * NB + cp * 128
                        if stage == 'hgrn':
                            P.dma(yb_dbg[tok0:tok0 + 128, :], Ot[:].rearrange("p h v -> p (h v)"), reads=['Ot'])
                        ptap, ptk = pT
                        for h in range(4):
                            P.op('pe', lambda e, h=h, ptap=ptap: e.matmul(ptap[:, h * 128:(h + 1) * 128], lhsT=Ot[:, h, :], rhs=ident[:],
                                                                         start=True, stop=True), reads=['Ot', 'ident'], writes=[ptk])
                        evac2(ybTs[:].rearrange("p h t -> p (h t)"), ptap, [ptk], ['ybTs'])
                        P.dma(ybT_d.rearrange("(h p) t -> p h t", p=128)[:, :, tok0:tok0 + 128], ybTs[:], reads=['ybTs'],
                              writes=['dram_ybT_%d' % (tok0 // 128)])
            P.flush()
        if stage == 'fullA':
            with ExitStack() as es:
                P.barrier()
                bt = P.sb(es, 'bt', [128, 4, TOWN], BF16)
                for src, dst, kp_ in [(yaT_d, yaT_o, 'a'), (ybT_d, ybT_o, 'b')]:
                    rk_ = (['dram_yaT_%d_%d' % (hp_, o__) for hp_ in range(4) for o__ in range(nblk - ownb)] if kp_ == 'a'
                           else ['dram_ybT_%d' % j_ for j_ in range(TOWN // 128)])
                    P.dma(bt[:], src.rearrange("(c p) t -> p c t", p=128), reads=rk_, writes=['bt'])
                    P.dma(dst.rearrange("(c p) t -> p c t", p=128), bt[:], reads=['bt'], writes=['dram_o' + kp_])
                P.flush()
        if stage == 'full':
          NT_H = 1024
          own_off = T - TOWN
          with ExitStack() as esB:
            P.barrier()
            PSA.reset()
            ACC = P.sb(esB, 'ACC', [128, NT_H // 128, D])
            x1T = P.sb(esB, 'x1T', [128, 8, NT_H], BF16)
            WtAll = P.sb(esB, 'WtAll', [128, NT_H // 128, 32])
            LNB = P.sb(esB, 'LNB', [128, 2, D])

            def load_ln(names):
                for i_, nm in enumerate(names):
                    o_, n_ = ROWS[nm]
                    P.dma(LNB[:, i_, :], rows_d[:, o_:o_ + n_].partition_broadcast(128), writes=['LNB'])
            RBB = P.sb(esB, 'RBB', [128, 36])
            o_, n_ = ROWS['rb']
            P.dma(RBB[:], rows_d[:, o_:o_ + n_].partition_broadcast(128), writes=['RBB'])
            evq3 = [0]

            def evac3(out, in_, reads, writes):
                evq3[0] += 1
                if evq3[0] % 2:
                    P.op('act', lambda e: e.copy(out=out, in_=in_), reads=reads, writes=writes)
                else:
                    P.op('dve', lambda e: e.tensor_copy(out=out, in_=in_), reads=reads, writes=writes)

            def load_cast(es_, dst, dkey, src_rows, ncols, nchunk, col0=0, cast_engs=('dve', 'pool')):
                stg_ = [P.sb(es_, 'lcs', [128, 1024]) for _ in range(2)]
                n_ = 0
                for c in range(nchunk):
                    for c1 in range(0, ncols, 1024):
                        s_ = stg_[n_ % 2]
                        k_ = 'lcs%d' % (n_ % 2)
                        P.dma(s_[:], src_rows(c)[:, col0 + c1:col0 + c1 + 1024], writes=[k_])
                        P.op(cast_engs[n_ % len(cast_engs)], lambda e, s_=s_, c=c, c1=c1: e.tensor_copy(out=dst[:, c, c1:c1 + 1024], in_=s_[:]),
                             reads=[k_], writes=[dkey])
                        n_ += 1

            def layer_norm_tile(Zt, zk, gi, stt, sq_t):
                P.op('dve', lambda e: e.tensor_reduce(out=stt[:, 0:1], in_=Zt[:], axis=AX.X, op=ALU.add), reads=[zk], writes=['stt'])
                P.op('dve', lambda e: e.tensor_scalar(out=stt[:, 0:1], in0=stt[:, 0:1], scalar1=1.0 / D, scalar2=None, op0=ALU.mult),
                     reads=['stt'], writes=['stt'])
                P.op('dve', lambda e: e.tensor_scalar(out=Zt[:], in0=Zt[:], scalar1=stt[:, 0:1], scalar2=None, op0=ALU.subtract),
                     reads=[zk, 'stt'], writes=[zk])
                P.op('pool', lambda e: e.tensor_tensor(out=sq_t[:], in0=Zt[:], in1=Zt[:], op=ALU.mult), reads=[zk], writes=['sq_t'])
                P.op('dve', lambda e: e.tensor_reduce(out=stt[:, 1:2], in_=sq_t[:], axis=AX.X, op=ALU.add), reads=['sq_t'], writes=['stt'])
                P.op('dve', lambda e: e.tensor_scalar(out=stt[:, 1:2], in0=stt[:, 1:2], scalar1=1.0 / D, scalar2=1e-5, op0=ALU.mult, op1=ALU.add),
                     reads=['stt'], writes=['stt'])
                P.op('act', lambda e: e.activation(out=stt[:, 1:2], in_=stt[:, 1:2], func=AF.Sqrt), reads=['stt'], writes=['stt'])
                P.op('dve', lambda e: e.reciprocal(out=stt[:, 1:2], in_=stt[:, 1:2]), reads=['stt'], writes=['stt'])
                P.op('dve', lambda e: e.tensor_scalar(out=Zt[:], in0=Zt[:], scalar1=stt[:, 1:2], scalar2=None, op0=ALU.mult),
                     reads=[zk, 'stt'], writes=[zk])
                P.op('pool', lambda e: e.tensor_tensor(out=Zt[:], in0=Zt[:], in1=LNB[:, 0, :], op=ALU.mult), reads=[zk, 'LNB'], writes=[zk])
                P.op('dve', lambda e: e.tensor_tensor(out=Zt[:], in0=Zt[:], in1=LNB[:, 1, :], op=ALU.add), reads=[zk, 'LNB'], writes=[zk])

            def do_half(hf):
                tk0 = hf * NT_H
                with ExitStack() as es:
                    P.barrier()
                    PSA.reset()
                    WA = P.sb(es, 'WA', [128, 4, D], BF16)
                    WB = P.sb(es, 'WB', [128, 4, D], BF16)
                    WG = P.sb(es, 'WG', [128, 8, 2048], BF16)
                    WO = P.sb(es, 'WO', [128, 8, D], BF16)
                    RW = P.sb(es, 'RW', [128, 8, 36])
                    load_ln(['ln1_g', 'ln1_b'])
                    with ExitStack() as es1:
                        load_cast(es1, WA, 'WA', lambda c: w_a_out_d[c * 128:(c + 1) * 128, :], D, 4)
                        load_cast(es1, WB, 'WB', lambda c: w_b_out_d[c * 128:(c + 1) * 128, :], D, 4)
                        load_cast(es1, WO, 'WO', lambda c: w_o_d[c * 128:(c + 1) * 128, :], D, 8)
                        load_cast(es1, WG, 'WG', lambda c: w_in[c * 128:(c + 1) * 128, :], 2048, 8, col0=C_GATE)
                        for c in range(8):
                            P.dma(RW[:, c, :], rw_d[c * 128:(c + 1) * 128, :], writes=['RW'])
                        P.flush()
                    P.barrier()
                    yaTb = P.sb(es, 'yaTb', [128, 4, 512], BF16)
                    ybTb = P.sb(es, 'ybTb', [128, 4, 512], BF16)
                    xgs = [P.sb(es, 'xgs', [128, 512]) for _ in range(2)]
                    xgb = P.sb(es, 'xgb', [128, 8, 512], BF16)
                    sga = P.sb(es, 'sga', [128, 512])
                    sgb = P.sb(es, 'sgb', [128, 512])
                    m1 = P.sb(es, 'm1', [128, 512])
                    m2 = P.sb(es, 'm2', [128, 512])
                    mT = P.sb(es, 'mT', [128, 8, 512], BF16)
                    xo_t = P.sb(es, 'xo_t', [128, D])
                    Zt = P.sb(es, 'Zt', [128, D])
                    sq_t = P.sb(es, 'sq_t', [128, D])
                    stt = P.sb(es, 'stt', [128, 2])
                    x1Tf = P.sb(es, 'x1Tf', [128, 8, 128])
                    Lg = P.sb(es, 'Lg', [128, 36])
                    rt = P.sb(es, 'rt', [128, 64])
                    psA = PSA.alloc(512, 0)
                    psB = PSA.alloc(512, 1)
                    psGa = PSA.alloc(512, 2)
                    psGb = PSA.alloc(512, 3)
                    psO = [PSA.alloc(512, 4), PSA.alloc(512, 5)]
                    psTr = [PSA.alloc(512, 6), PSA.alloc(512, 7)]
                    psR = psA
                    for tbk in range(NT_H // 512):
                        tb0 = tk0 + tbk * 512
                        P.dma(yaTb[:], yaT_d.rearrange("(c p) t -> p c t", p=128)[:, :, tb0:tb0 + 512],
                              reads=['dram_yaT_%d_%d' % (hp_, tb0 // NB + j_) for hp_ in range(4) for j_ in range(512 // NB)], writes=['yaTb'])
                        P.dma(ybTb[:], ybT_d.rearrange("(c p) t -> p c t", p=128)[:, :, tb0:tb0 + 512],
                              reads=['dram_ybT_%d' % (tb0 // 128 + j_) for j_ in range(4)], writes=['ybTb'])
                        for dc in range(8):
                            s_ = xgs[dc % 2]
                            k_ = 'xgs%d' % (dc % 2)
                            P.dma(s_[:], xT[dc * 128:(dc + 1) * 128, own_off + tb0:own_off + tb0 + 512], writes=[k_])
                            P.op('pool' if dc % 2 else 'dve', lambda e, s_=s_, dc=dc: e.tensor_copy(out=xgb[:, dc, :], in_=s_[:]),
                                 reads=[k_], writes=['xgb'])
                        for dco in range(8):
                            cso = slice(dco * 128, (dco + 1) * 128)
                            for fc in range(4):
                                P.op('pe', lambda e, fc=fc, cso=cso: e.matmul(psA[0], lhsT=WA[:, fc, cso], rhs=yaTb[:, fc, :],
                                                                             start=(fc == 0), stop=(fc == 3)), reads=['WA', 'yaTb'], writes=[psA[1]])
                            for fc in range(4):
                                P.op('pe', lambda e, fc=fc, cso=cso: e.matmul(psB[0], lhsT=WB[:, fc, cso], rhs=ybTb[:, fc, :],
                                                                             start=(fc == 0), stop=(fc == 3)), reads=['WB', 'ybTb'], writes=[psB[1]])
                            for dc in range(8):
                                P.op('pe', lambda e, dc=dc, dco=dco: e.matmul(psGa[0], lhsT=WG[:, dc, dco * 128:(dco + 1) * 128], rhs=xgb[:, dc, :],
                                                                             start=(dc == 0), stop=(dc == 7)), reads=['WG', 'xgb'], writes=[psGa[1]])
                            for dc in range(8):
                                P.op('pe', lambda e, dc=dc, dco=dco: e.matmul(psGb[0], lhsT=WG[:, dc, 1024 + dco * 128:1024 + (dco + 1) * 128],
                                                                             rhs=xgb[:, dc, :], start=(dc == 0), stop=(dc == 7)),
                                     reads=['WG', 'xgb'], writes=[psGb[1]])
                            P.op('act', lambda e: e.activation(out=sga[:], in_=psGa[0], func=AF.Sigmoid), reads=[psGa[1]], writes=['sga'])
                            P.op('act', lambda e: e.activation(out=sgb[:], in_=psGb[0], func=AF.Sigmoid), reads=[psGb[1]], writes=['sgb'])
                            P.op('dve', lambda e: e.tensor_tensor(out=m1[:], in0=psA[0], in1=sga[:], op=ALU.mult), reads=[psA[1], 'sga'], writes=['m1'])
                            P.op('dve', lambda e: e.tensor_tensor(out=m2[:], in0=psB[0], in1=sgb[:], op=ALU.mult), reads=[psB[1], 'sgb'], writes=['m2'])
                            P.op('pool', lambda e, dco=dco: e.tensor_tensor(out=mT[:, dco, :], in0=m1[:], in1=m2[:], op=ALU.add),
                                 reads=['m1', 'm2'], writes=['mT'])
                        for tt in range(4):
                            tile_i = tbk * 4 + tt
                            trow = tb0 + tt * 128
                            P.dma(xo_t[:], xo_d[trow:trow + 128, :], writes=['xo_t'])
                            for hh in range(2):
                                for dc in range(8):
                                    P.op('pe', lambda e, dc=dc, tt=tt, hh=hh: e.matmul(
                                        psO[hh][0], lhsT=mT[:, dc, tt * 128:(tt + 1) * 128], rhs=WO[:, dc, hh * 512:(hh + 1) * 512],
                                        start=(dc == 0), stop=(dc == 7)), reads=['mT', 'WO'], writes=[psO[hh][1]])
                                P.op('dve', lambda e, hh=hh: e.scalar_tensor_tensor(
                                    out=Zt[:, hh * 512:(hh + 1) * 512], in0=xo_t[:, hh * 512:(hh + 1) * 512], scalar=ALPHA, in1=psO[hh][0],
                                    op0=ALU.mult, op1=ALU.add), reads=['xo_t', psO[hh][1]], writes=['Zt'])
                            layer_norm_tile(Zt, 'Zt', 0, stt, sq_t)
                            P.op('act', lambda e, tile_i=tile_i: e.mul(out=ACC[:, tile_i, :], in_=Zt[:], mul=ALPHA), reads=['Zt'], writes=['ACC%d' % tile_i])
                            for dc in range(8):
                                pt = psTr[dc // 4]
                                P.op('pe', lambda e, dc=dc, pt=pt: e.matmul(pt[0][:, (dc % 4) * 128:(dc % 4 + 1) * 128],
                                                                            lhsT=Zt[:, dc * 128:(dc + 1) * 128], rhs=ident[:], start=True, stop=True),
                                     reads=['Zt', 'ident'], writes=[pt[1]])
                            for q in range(2):
                                pt = psTr[q]
                                P.op('act', lambda e, q=q, pt=pt: e.copy(out=x1Tf[:, q * 4:(q + 1) * 4, :].rearrange("p a t -> p (a t)"), in_=pt[0]),
                                     reads=[pt[1]], writes=['x1Tf'])
                                P.op('dve', lambda e, q=q, pt=pt, tile_i=tile_i: e.tensor_copy(
                                    out=x1T[:, q * 4:(q + 1) * 4, tile_i * 128:(tile_i + 1) * 128],
                                    in_=pt[0].rearrange("p (a t) -> p a t", t=128)), reads=[pt[1]], writes=['x1T'])
                            for dc in range(8):
                                P.op('pe', lambda e, dc=dc: e.matmul(psR[0][:, 0:36], lhsT=x1Tf[:, dc, :], rhs=RW[:, dc, :],
                                                                     start=(dc == 0), stop=(dc == 7)), reads=['x1Tf', 'RW'], writes=[psR[1]])
                            P.op('dve', lambda e: e.tensor_tensor(out=Lg[:], in0=psR[0][:, 0:36], in1=RBB[:], op=ALU.add),
                                 reads=[psR[1], 'RBB'], writes=['Lg'])
                            RT = lambda a, b: rt[:, a:b]
                            dv = lambda fn, rd=('Lg', 'rt'): P.op('dve', fn, reads=list(rd), writes=['rt'])
                            dv(lambda e: e.tensor_reduce(out=RT(0, 1), in_=Lg[:, 0:4], axis=AX.X, op=ALU.max))
                            dv(lambda e: e.tensor_scalar(out=RT(1, 2), in0=RT(0, 1), scalar1=-1.0, scalar2=None, op0=ALU.mult))
                            P.op('act', lambda e: e.activation(out=RT(16, 20), in_=Lg[:, 0:4], func=AF.Exp, bias=RT(1, 2)), reads=['Lg', 'rt'], writes=['rt'])
                            dv(lambda e: e.tensor_reduce(out=RT(2, 3), in_=RT(16, 20), axis=AX.X, op=ALU.add))
                            dv(lambda e: e.reciprocal(out=RT(3, 4), in_=RT(2, 3)))
                            dv(lambda e: e.tensor_scalar(out=RT(16, 20), in0=Lg[:, 0:4], scalar1=RT(0, 1), scalar2=None, op0=ALU.is_equal))
                            P.op('dve', lambda e: e.tensor_tensor(out=sq_t[:, 0:32].rearrange("p (g j) -> p g j", j=8),
                                                                  in0=Lg[:, 4:36].rearrange("p (g j) -> p g j", j=8),
                                                                  in1=RT(16, 20).unsqueeze(2).to_broadcast([128, 4, 8]), op=ALU.mult),
                                 reads=['Lg', 'rt'], writes=['sq_t'])
                            P.op('dve', lambda e: e.tensor_reduce(out=RT(24, 32), in_=sq_t[:, 0:32].rearrange("p (g j) -> p j g", j=8), axis=AX.X, op=ALU.add),
                                 reads=['sq_t', 'rt'], writes=['rt', 'sq_t'])
                            dv(lambda e: e.tensor_reduce(out=RT(4, 5), in_=RT(24, 32), axis=AX.X, op=ALU.max))
                            dv(lambda e: e.tensor_scalar(out=RT(32, 40), in0=RT(24, 32), scalar1=RT(4, 5), scalar2=None, op0=ALU.is_equal))
                            dv(lambda e: e.scalar_tensor_tensor(out=RT(40, 48), in0=RT(32, 40), scalar=-1e30, in1=RT(24, 32), op0=ALU.mult, op1=ALU.add))
                            dv(lambda e: e.tensor_reduce(out=RT(5, 6), in_=RT(40, 48), axis=AX.X, op=ALU.max))
                            dv(lambda e: e.tensor_scalar(out=RT(40, 48), in0=RT(40, 48), scalar1=RT(5, 6), scalar2=None, op0=ALU.is_equal))
                            dv(lambda e: e.tensor_tensor(out=RT(6, 7), in0=RT(4, 5), in1=RT(5, 6), op=ALU.subtract))
                            P.op('act', lambda e: e.activation(out=RT(7, 8), in_=RT(6, 7), func=AF.Sigmoid), reads=['rt'], writes=['rt'])
                            dv(lambda e: e.tensor_tensor(out=RT(8, 9), in0=RT(7, 8), in1=RT(3, 4), op=ALU.mult))
                            dv(lambda e: e.tensor_tensor(out=RT(9, 10), in0=RT(3, 4), in1=RT(8, 9), op=ALU.subtract))
                            dv(lambda e: e.tensor_scalar(out=RT(48, 56), in0=RT(32, 40), scalar1=RT(8, 9), scalar2=None, op0=ALU.mult))
                            dv(lambda e: e.scalar_tensor_tensor(out=RT(48, 56), in0=RT(40, 48), scalar=RT(9, 10), in1=RT(48, 56), op0=ALU.mult, op1=ALU.add))
                            P.op('dve', lambda e, tile_i=tile_i: e.tensor_tensor(out=WtAll[:, tile_i, :].rearrange("p (g j) -> p g j", j=8),
                                                                  in0=RT(16, 20).unsqueeze(2).to_broadcast([128, 4, 8]),
                                                                  in1=RT(48, 56).unsqueeze(1).to_broadcast([128, 4, 8]), op=ALU.mult),
                                 reads=['rt'], writes=['WtAll'])
                    P.flush()
                with ExitStack() as es:
                    P.barrier()
                    PSA.reset()
                    W1b = [P.sb(es, 'W1b', [128, 8, 512], BF16) for _ in range(2)]
                    W3b = [P.sb(es, 'W3b', [128, 8, 512], BF16) for _ in range(2)]
                    W2b = [P.sb(es, 'W2b', [128, 4, D], BF16) for _ in range(2)]
                    wst = [P.sb(es, 'wst', [128, 2048]) for _ in range(3)]
                    sil = [P.sb(es, 'sil', [128, 512]) for _ in range(2)]
                    hwT = [P.sb(es, 'hwT', [128, 4, 512], BF16) for _ in range(2)]
                    psH1 = [PSA.alloc(512, 1), PSA.alloc(512, 2)]
                    psH3 = [PSA.alloc(512, 3), PSA.alloc(512, 4)]
                    psF = [PSA.alloc(512, 5), PSA.alloc(512, 6)]
                    wq = [0]
                    ceng = ['pool', 'act']

                    def wload(dst, dkey, src3, nck):
                        for c in range(nck):
                            i_ = wq[0] % 3
                            wq[0] += 1
                            s_ = wst[i_]
                            k_ = 'wst%d' % i_
                            P.dma(s_[:].rearrange("p (a f) -> p a f", a=src3(c).shape[1]), src3(c), writes=[k_])
                            en = ceng[wq[0] % 2]
                            if en == 'act':
                                P.op('act', lambda e, s_=s_, c=c: e.copy(out=dst(c), in_=s_[:]), reads=[k_], writes=[dkey])
                            else:
                                P.op('pool', lambda e, s_=s_, c=c: e.tensor_copy(out=dst(c), in_=s_[:]), reads=[k_], writes=[dkey])
                    for ex in range(32):
                        pb_ = ex % 2
                        w1v = w1_d[ex].rearrange("(c p) f -> p c f", p=128)
                        w3v = w3_d[ex].rearrange("(c p) f -> p c f", p=128)
                        w2v = w2_d[ex].rearrange("(c p) d -> p c d", p=128)
                        wload(lambda c, pb_=pb_: W1b[pb_][:, c * 4:(c + 1) * 4, :].rearrange("p a f -> p (a f)"), 'W1b%d' % pb_,
                              lambda c, w1v=w1v: w1v[:, c * 4:(c + 1) * 4, :], 2)
                        wload(lambda c, pb_=pb_: W3b[pb_][:, c * 4:(c + 1) * 4, :].rearrange("p a f -> p (a f)"), 'W3b%d' % pb_,
                              lambda c, w3v=w3v: w3v[:, c * 4:(c + 1) * 4, :], 2)
                        wload(lambda c, pb_=pb_: W2b[pb_][:, c * 2:(c + 1) * 2, :].rearrange("p a f -> p (a f)"), 'W2b%d' % pb_,
                              lambda c, w2v=w2v: w2v[:, c * 2:(c + 1) * 2, :], 2)
                        for tbk in range(NT_H // 512):
                            tsl = slice(tbk * 512, (tbk + 1) * 512)
                            hw = hwT[tbk % 2]
                            hwk = 'hwT%d' % (tbk % 2)
                            for fc in range(4):
                                p1, p3 = psH1[fc % 2], psH3[fc % 2]
                                fsl = slice(fc * 128, (fc + 1) * 128)
                                for dc in range(8):
                                    P.op('pe', lambda e, dc=dc, fsl=fsl, tsl=tsl, p1=p1, pb_=pb_: e.matmul(
                                        p1[0], lhsT=W1b[pb_][:, dc, fsl], rhs=x1T[:, dc, tsl], start=(dc == 0), stop=(dc == 7)),
                                        reads=['W1b%d' % pb_, 'x1T'], writes=[p1[1]])
                                for dc in range(8):
                                    P.op('pe', lambda e, dc=dc, fsl=fsl, tsl=tsl, p3=p3, pb_=pb_: e.matmul(
                                        p3[0], lhsT=W3b[pb_][:, dc, fsl], rhs=x1T[:, dc, tsl], start=(dc == 0), stop=(dc == 7)),
                                        reads=['W3b%d' % pb_, 'x1T'], writes=[p3[1]])
                                sl_ = sil[fc % 2]
                                P.op('act', lambda e, sl_=sl_, p1=p1: e.activation(out=sl_[:], in_=p1[0], func=AF.Silu),
                                     reads=[p1[1]], writes=['sil%d' % (fc % 2)])
                                P.op('dve', lambda e, sl_=sl_, hw=hw, fc=fc, p3=p3: e.tensor_tensor(out=hw[:, fc, :], in0=p3[0], in1=sl_[:], op=ALU.mult),
                                     reads=[p3[1], 'sil%d' % (fc % 2)], writes=[hwk])
                            for tt in range(4):
                                tile_i = tbk * 4 + tt
                                for hx in range(2):
                                    pf = psF[hx]
                                    for fc in range(4):
                                        P.op('pe', lambda e, fc=fc, tt=tt, hx=hx, pf=pf, hw=hw, pb_=pb_: e.matmul(
                                            pf[0], lhsT=hw[:, fc, tt * 128:(tt + 1) * 128], rhs=W2b[pb_][:, fc, hx * 512:(hx + 1) * 512],
                                            start=(fc == 0), stop=(fc == 3)), reads=[hwk, 'W2b%d' % pb_], writes=[pf[1]])
                                    P.op('dve', lambda e, tile_i=tile_i, hx=hx, pf=pf, ex=ex: e.scalar_tensor_tensor(
                                        out=ACC[:, tile_i, hx * 512:(hx + 1) * 512], in0=pf[0], scalar=WtAll[:, tile_i, ex:ex + 1],
                                        in1=ACC[:, tile_i, hx * 512:(hx + 1) * 512], op0=ALU.mult, op1=ALU.add),
                                        reads=[pf[1], 'ACC%d' % tile_i, 'WtAll'], writes=['ACC%d' % tile_i])
                    P.flush()
                with ExitStack() as es:
                    P.barrier()
                    PSA.reset()
                    WPG = P.sb(es, 'WPG', [128, 8, D], BF16)
                    load_ln(['ln2_g', 'ln2_b'])
                    WPE = P.sb(es, 'WPE', [128, 2, D], BF16)
                    pTb = P.sb(es, 'pTb', [128, 2, NT_H], BF16)
                    with ExitStack() as es1:
                        load_cast(es1, WPG, 'WPG', lambda c: w_pg_d[c * 128:(c + 1) * 128, :], D, 8)
                        load_cast(es1, WPE, 'WPE', lambda c: w_pe_d[c * 128:(c + 1) * 128, :], D, 2)
                        load_cast(es1, pTb, 'pTb', lambda c: pT_d[c * 128:(c + 1) * 128, :], NT_H, 2, col0=tk0)
                        P.flush()
                    P.barrier()
                    Z2 = P.sb(es, 'Z2', [128, D])
                    sq_t = P.sb(es, 'sq_t2', [128, D])
                    stt = P.sb(es, 'stt2', [128, 2])
                    x2T = P.sb(es, 'x2T', [128, 8, 128], BF16)
                    sgp = P.sb(es, 'sgp', [128, 512])
                    ot_ = [P.sb(es, 'ot_', [128, D]) for _ in range(2)]
                    psTr = [PSA.alloc(512, 0), PSA.alloc(512, 1)]
                    psG = [PSA.alloc(512, 2), PSA.alloc(512, 3)]
                    psP = [PSA.alloc(512, 4), PSA.alloc(512, 5)]
                    for tile_i in range(NT_H // 128):
                        trow = tk0 + tile_i * 128
                        P.op('act', lambda e, tile_i=tile_i: e.copy(out=Z2[:], in_=ACC[:, tile_i, :]), reads=['ACC%d' % tile_i], writes=['Z2'])
                        layer_norm_tile(Z2, 'Z2', 2, stt, sq_t)
                        for dc in range(8):
                            pt = psTr[dc // 4]
                            P.op('pe', lambda e, dc=dc, pt=pt: e.matmul(pt[0][:, (dc % 4) * 128:(dc % 4 + 1) * 128],
                                                                        lhsT=Z2[:, dc * 128:(dc + 1) * 128], rhs=ident[:], start=True, stop=True),
                                 reads=['Z2', 'ident'], writes=[pt[1]])
                        for q in range(2):
                            pt = psTr[q]
                            evac3(x2T[:, q * 4:(q + 1) * 4, :].rearrange("p a t -> p (a t)"), pt[0], [pt[1]], ['x2T'])
                        o_t = ot_[tile_i % 2]
                        ok_ = 'ot_%d' % (tile_i % 2)
                        for hx in range(2):
                            for dc in range(8):
                                P.op('pe', lambda e, dc=dc, hx=hx: e.matmul(psG[hx][0], lhsT=x2T[:, dc, :], rhs=WPG[:, dc, hx * 512:(hx + 1) * 512],
                                                                            start=(dc == 0), stop=(dc == 7)), reads=['x2T', 'WPG'], writes=[psG[hx][1]])
                            for kc in range(2):
                                P.op('pe', lambda e, kc=kc, hx=hx, tile_i=tile_i: e.matmul(
                                    psP[hx][0], lhsT=pTb[:, kc, tile_i * 128:(tile_i + 1) * 128], rhs=WPE[:, kc, hx * 512:(hx + 1) * 512],
                                    start=(kc == 0), stop=(kc == 1)), reads=['pTb', 'WPE'], writes=[psP[hx][1]])
                            P.op('act', lambda e, hx=hx: e.activation(out=sgp[:], in_=psG[hx][0], func=AF.Sigmoid), reads=[psG[hx][1]], writes=['sgp'])
                            P.op('dve', lambda e, hx=hx, o_t=o_t: e.tensor_tensor(out=o_t[:, hx * 512:(hx + 1) * 512], in0=psP[hx][0], in1=sgp[:], op=ALU.mult),
                                 reads=[psP[hx][1], 'sgp'], writes=[ok_])
                            P.op('pool', lambda e, hx=hx, o_t=o_t: e.tensor_tensor(out=o_t[:, hx * 512:(hx + 1) * 512], in0=o_t[:, hx * 512:(hx + 1) * 512],
                                                                                   in1=Z2[:, hx * 512:(hx + 1) * 512], op=ALU.add),
                                 reads=[ok_, 'Z2'], writes=[ok_])
                        P.dma(out_d[trow:trow + 128, :], o_t[:], reads=[ok_], writes=['dram_out_%d' % trow])
                    P.flush()
            for hf_ in range(TOWN // NT_H):
                do_half(hf_)
            P.flush()
        print('NOPS', P.nops, {k: len(v) for k, v in P.streams.items()}, flush=True)
        P.emit()
    return nc


def make_in_maps(inputs):
    f32 = lambda a: np.ascontiguousarray(np.asarray(a, np.float32))
    x = f32(inputs['x'])
    consts = host_consts()
    pv = np.zeros((128, NPV), np.float32)
    pv[:, PV['w0']:PV['w0'] + 4] = colvec(inputs['rw_w0'][0], 4)
    pv[:, PV['a0']:PV['a0'] + 4] = colvec(inputs['rw_a0'][0], 4)
    pv[:, PV['k_k']:PV['k_k'] + 4] = colvec(inputs['rw_k_k'][0], 4)
    pv[:, PV['k_a']:PV['k_a'] + 4] = colvec(inputs['rw_k_a'][0], 4)
    pv[:, PV['r_k']:PV['r_k'] + 4] = colvec(np.asarray(inputs['rw_r_k'][0]).reshape(512), 4)
    pv[:, PV['lb0']:PV['lb0'] + 4] = colvec(inputs['hg_lb_logits'][0], 4)
    pv[:, PV['lb1']:PV['lb1'] + 4] = colvec(inputs['hg_lb_logits'][1], 4)
    rows = np.zeros((1, NROWS), np.float32)

    def setrow(name, v):
        o, n = ROWS[name]
        rows[0, o:o + n] = np.asarray(v, np.float32).reshape(n)
    setrow('mu', inputs['rw_mu'][0])
    setrow('gn_w', inputs['rw_gn_w'][0])
    setrow('gn_b', inputs['rw_gn_b'][0])
    setrow('hg_nw', inputs['hg_norm_w'][0])
    setrow('ln1_g', inputs['ln1_g'][0])
    setrow('ln1_b', inputs['ln1_b'][0])
    setrow('ln2_g', inputs['ln2_g'][0])
    setrow('ln2_b', inputs['ln2_b'][0])
    setrow('rb', np.concatenate([np.asarray(inputs['router_g_b'][0]).reshape(4), np.asarray(inputs['router_e_b'][0]).reshape(32)]))
    gw = np.asarray(inputs['rw_gn_w'][0], np.float32).reshape(4, 2, 1, 64)
    gb = np.asarray(inputs['rw_gn_b'][0], np.float32).reshape(4, 2, 1, 64)
    gnw_t = np.ascontiguousarray(np.broadcast_to(gw, (4, 2, 64, 64)).reshape(4, 128, 64))
    gnb_t = np.ascontiguousarray(np.broadcast_to(gb, (4, 2, 64, 64)).reshape(4, 128, 64))
    shared = dict(consts)
    shared.update(pv=pv, rows=rows, gnw_t=gnw_t, gnb_t=gnb_t,
                  w_in=f32(inputs['w_in'][0]), rw_w_up=f32(inputs['rw_w_up'][0]), rw_a_up=f32(inputs['rw_a_up'][0]),
                  rw_g_up=f32(inputs['rw_g_up'][0]),
                  w_a_out=f32(inputs['w_a_out'][0]), w_b_out=f32(inputs['w_b_out'][0]), w_o=f32(inputs['w_o'][0]),
                  rw=np.ascontiguousarray(np.concatenate([f32(inputs['router_g_w'][0]), f32(inputs['router_e_w'][0])], 1)),
                  w1=f32(inputs['w1'][0]), w3=f32(inputs['w3'][0]), w2=f32(inputs['w2'][0]),
                  w_pe=f32(inputs['w_pe'][0]), w_pg=f32(inputs['w_pg'][0]))
    in_maps = []
    for c in range(8):
        b, half = c // 2, c % 2
        xw = np.zeros((D, T), np.float32)
        if half == 1:
            xw[:, :] = x[b].T
        else:
            xw[:, T - TOWN:] = x[b, :TOWN].T
        m = dict(shared)
        m['xT'] = xw
        m['xo'] = np.ascontiguousarray(x[b, half * TOWN:(half + 1) * TOWN])
        m['pT'] = np.ascontiguousarray(np.asarray(inputs['p'], np.float32)[0, b, half * TOWN:(half + 1) * TOWN].T)
        in_maps.append(m)
    return in_maps


_NC_CACHE = {}


def kernel(**inputs):
    if 'full' not in _NC_CACHE:
        _NC_CACHE['full'] = build('full')
    nc = _NC_CACHE['full']
    in_maps = make_in_maps(inputs)
    keep = set(['xT', 'w_in', 'pv', 'rows', 'ident', 'mask320', 'iu2', 'ii', 'onesblk', 'scanmask',
                'rw_w_up', 'rw_a_up', 'rw_g_up', 'gnw_t', 'gnb_t', 'xo', 'pT', 'w_a_out', 'w_b_out', 'w_o', 'rw',
                'w1', 'w3', 'w2', 'w_pe', 'w_pg'])
    in_maps = [{k: v for k, v in m.items() if k in keep} for m in in_maps]
    res = run_bass_kernel_spmd(nc, in_maps, core_ids=list(range(8)))
    out = np.zeros((4, T, D), np.float32)
    for c in range(8):
        b, half = c // 2, c % 2
        out[b, half * TOWN:(half + 1) * TOWN] = res.results[c]['out']
    return out
```

```python
import os
import numpy as np
from contextlib import ExitStack
import concourse.bass as bass
import concourse.mybir as mybir
from concourse.bass_utils import run_bass_kernel_spmd

F32 = mybir.dt.float32
BF16 = mybir.dt.bfloat16
ALU = mybir.AluOpType
AF = mybir.ActivationFunctionType
AX = mybir.AxisListType

NDSEM = 8
T = 4096
TOWN = 2048
D = 1024
NB = 256
NCH = NB // 64
NBLK = T // NB
OWNB = (T - TOWN) // NB
CDEC = 0.6065306597126334
ALPHA = 2.0 ** 0.25
N_IN = 5888
C_Q = 1792
C_GATE = 3840


class Prog:
    def __init__(self, nc, es):
        self.nc = nc
        self.es = es
        self.engs = ['pe', 'act', 'dve', 'pool', 'sp']
        self.streams = {e: [] for e in self.engs}
        self.prods = ['pe', 'act', 'dve', 'pool']
        self.count = {p: 0 for p in self.prods}
        self.sems = {p: es.enter_context(nc.semaphore('s_' + p)) for p in self.prods}
        self.waited = {}
        self.last_write = {}
        self.readers = {}
        self.ndma = 0
        self.uid = 0
        self.floor = {}

    def barrier(self):
        self.floor = dict(self.count)

    def sb(self, es, name, shape, dt=F32):
        self.uid += 1
        return es.enter_context(self.nc.sbuf_tensor('%s_%d' % (name, self.uid), list(shape), dt))

    def _deps(self, eng, prod, reads, writes):
        deps = {}
        writes = list(writes) + [k for k in reads if k.startswith('bank')]
        reads = [k for k in reads if not k.startswith('bank')]

        def add(p, n):
            if n > deps.get(p, 0):
                deps[p] = n
        for k in reads:
            if k in self.last_write:
                add(*self.last_write[k])
        for k in writes:
            if k in self.last_write:
                add(*self.last_write[k])
            for p, n in self.readers.get(k, {}).items():
                add(p, n)
        waits = []
        for p, n in self.floor.items():
            if n > deps.get(p, 0) and self.waited.get((eng, p), 0) < n:
                deps[p] = n
        for p, n in deps.items():
            if p == 'pe' and eng == 'pe' and self.floor.get('pe', 0) < n:
                continue
            if self.waited.get((eng, p), 0) >= n:
                continue
            self.waited[(eng, p)] = n
            waits.append((p, n))
        self.count[prod] += 1
        idx = self.count[prod]
        for k in writes:
            self.last_write[k] = (prod, idx)
            self.readers[k] = {}
        for k in reads:
            rd = self.readers.setdefault(k, {})
            if rd.get(prod, 0) < idx:
                rd[prod] = idx
        return waits

    def op(self, eng, fn, reads=(), writes=()):
        self.nops = getattr(self, 'nops', 0) + 1
        if self.nops > int(os.environ.get('KMAX', '100000000')):
            return
        waits = self._deps(eng, eng, reads, writes)
        self.streams[eng].append((waits, fn, eng))

    def dma(self, out, in_, reads=(), writes=(), eng='sp'):
        self.nops = getattr(self, 'nops', 0) + 1
        if self.nops > int(os.environ.get('KMAX', '100000000')):
            return
        skey = [k for k in list(writes) + list(reads) if not k.startswith('dram_')][0]
        prod = 'd_' + skey
        if prod not in self.sems:
            self.sems[prod] = self.es.enter_context(self.nc.semaphore('s_' + prod))
            self.count[prod] = 0
            self.prods.append(prod)
        self.ndma += 1
        waits = self._deps(eng, prod, reads, writes)
        self.streams[eng].append((waits, lambda e: e.dma_start(out=out, in_=in_), prod))

    def emit(self):
        self.flush(final=True)

    def flush(self, final=False):
        nc = self.nc
        if not final and not any(self.streams.values()):
            return
        streams = self.streams
        self.streams = {e: [] for e in self.engs}

        def run(ename, e):
            for waits, fn, prod in streams[ename]:
                for p, n in waits:
                    e.wait_ge(self.sems[p], n * 16 if p[0] == 'd' else n)
                ins = fn(e)
                ins.then_inc(self.sems[prod], 16 if prod[0] == 'd' else 1)
            if ename == 'sp' and final:
                for p in self.prods:
                    if self.count[p] > 0:
                        e.wait_ge(self.sems[p], self.count[p] * (16 if p[0] == 'd' else 1))
        with nc.Block() as block:
            @block.tensor
            def _(e):
                run('pe', e)

            @block.scalar
            def _(e):
                run('act', e)

            @block.vector
            def _(e):
                run('dve', e)

            @block.gpsimd
            def _(e):
                run('pool', e)

            @block.sync
            def _(e):
                run('sp', e)


class PsumAlloc:
    def __init__(self, P, es):
        self.banks = [es.enter_context(P.nc.psum_tensor('psb%d' % i, [128, 512], F32)) for i in range(8)]
        self.used = [0] * 8
        self.n = 0

    def alloc(self, cols, bank):
        b = bank
        assert self.used[b] + cols <= 512, (bank, cols, self.used[b])
        o = self.used[b]
        self.used[b] += cols
        return self.banks[b][:, o:o + cols], 'bank%d' % b

    def reset(self):
        self.used = [0] * 8


def colvec(v, n):
    return np.ascontiguousarray(np.asarray(v, np.float32).reshape(n, 128).T)


def host_consts():
    c = {}
    c['ident'] = np.eye(128, dtype=np.float32)
    su = np.triu(np.ones((64, 64), np.float32), 1)
    sl = np.tril(np.ones((64, 64), np.float32), -1)
    iu = np.triu(np.ones((64, 64), np.float32), 0)
    m = np.concatenate([su, sl, su, iu, iu], 1)
    c['mask320'] = np.concatenate([m, m], 0)
    c['iu2'] = np.concatenate([iu, iu], 0)
    i64 = np.eye(64, dtype=np.float32)
    ii = np.concatenate([i64, i64], 1)
    c['ii'] = np.concatenate([ii, ii], 0)
    ob = np.zeros((128, 128), np.float32)
    ob[:64, :64] = 1
    ob[64:, 64:] = 1
    c['onesblk'] = ob
    sm = np.ones((128, NB), np.float32)
    sm[:, ::64] = 0
    c['scanmask'] = sm
    sel = np.zeros((32, 32 * 128), np.float32)
    for e in range(32):
        sel[e, e * 128:(e + 1) * 128] = 1
    c['sel'] = sel
    return c


PV = {}
_o = 0
for _name, _n in [('w0', 4), ('a0', 4), ('k_k', 4), ('k_a', 4), ('r_k', 4), ('lb0', 4), ('lb1', 4)]:
    PV[_name] = _o
    _o += _n
NPV = _o
ROWS = {}
_o = 0
for _name, _n in [('mu', 1792), ('gn_w', 512), ('gn_b', 512), ('hg_nw', 512), ('ln1_g', 1024), ('ln1_b', 1024),
                  ('ln2_g', 1024), ('ln2_b', 1024), ('rb', 36)]:
    ROWS[_name] = (_o, _n)
    _o += _n
NROWS = _o


def build(stage='full', nblk=NBLK, ownb=OWNB):
    nc = bass.Bass("TRN2", target_bir_lowering=False)

    def din(name, shape, dt=F32):
        return nc.dram_tensor(name, list(shape), dt, kind="ExternalInput").ap()

    def dout(name, shape, dt=F32):
        return nc.dram_tensor(name, list(shape), dt, kind="ExternalOutput").ap()

    xT = din('xT', [D, T])
    w_in = din('w_in', [D, N_IN])
    pv_d = din('pv', [128, NPV])
    rows_d = din('rows', [1, NROWS])
    ident_d = din('ident', [128, 128])
    mask320_d = din('mask320', [128, 320])
    iu2_d = din('iu2', [128, 64])
    ii_d = din('ii', [128, 128])
    onesblk_d = din('onesblk', [128, 128])
    scanmask_d = din('scanmask', [128, NB])
    w_up_d = din('rw_w_up', [64, 512])
    a_up_d = din('rw_a_up', [64, 512])
    g_up_d = din('rw_g_up', [128, 512])
    gnw_d = din('gnw_t', [4, 128, 64])
    gnb_d = din('gnb_t', [4, 128, 64])

    if stage == 'full':
        xo_d = din('xo', [TOWN, D])
        pT_d = din('pT', [256, TOWN])
        w_a_out_d = din('w_a_out', [512, D])
        w_b_out_d = din('w_b_out', [512, D])
        w_o_d = din('w_o', [D, D])
        rw_d = din('rw', [D, 36])
        w1_d = din('w1', [32, D, 512])
        w3_d = din('w3', [32, D, 512])
        w2_d = din('w2', [32, 512, D])
        w_pe_d = din('w_pe', [256, D])
        w_pg_d = din('w_pg', [D, D])
    yaT_d = nc.dram_tensor('yaT_s', [512, TOWN], BF16, kind="Internal").ap()
    ybT_d = nc.dram_tensor('ybT_s', [512, TOWN], BF16, kind="Internal").ap()
    if stage == 'rwkv':
        ya_dbg = dout('ya_dbg', [TOWN // 64, 4, 128, 64])
    elif stage == 'hgrn':
        yb_dbg = dout('yb_dbg', [TOWN, 512])
    elif stage == 'fullA':
        yaT_o = dout('yaT_o', [512, TOWN], BF16)
        ybT_o = dout('ybT_o', [512, TOWN], BF16)
    else:
        out_d = dout('out', [TOWN, D])

    with ExitStack() as es0:
        P = Prog(nc, es0)
        PSA = PsumAlloc(P, es0)
        ident = P.sb(es0, 'ident', [128, 128])
        pvt = P.sb(es0, 'pv', [128, NPV])
        P.dma(ident[:], ident_d, writes=['ident'])
        P.dma(pvt[:], pv_d, writes=['pv'])

        def pcol(name, i):
            o = PV[name] + i
            return pvt[:, o:o + 1]

        with ExitStack() as es:
            PSA.reset()
            mask320 = P.sb(es, 'mask320', [128, 320])
            iit = P.sb(es, 'ii', [128, 128])
            onesblk = P.sb(es, 'onesblk', [128, 128])
            scanmask = P.sb(es, 'scanmask', [128, NB])
            P.dma(mask320[:], mask320_d, writes=['mask320'])
            P.dma(iit[:], ii_d, writes=['ii'])
            P.dma(onesblk[:], onesblk_d, writes=['onesblk'])
            P.dma(scanmask[:], scanmask_d, writes=['scanmask'])
            wA = P.sb(es, 'wA', [128, 8, 1792], BF16)
            wB = P.sb(es, 'wB', [128, 8, 1792], BF16)
            LW = P.sb(es, 'LW', [128, 512], BF16)
            LW2 = P.sb(es, 'LW2', [128, 512], BF16)
            GU = P.sb(es, 'GU', [128, 512], BF16)
            gnw = P.sb(es, 'gnw', [128, 4, 64])
            gnb = P.sb(es, 'gnb', [128, 4, 64])
            for hp in range(4):
                P.dma(gnw[:, hp, :], gnw_d[hp], writes=['gnw'])
                P.dma(gnb[:, hp, :], gnb_d[hp], writes=['gnb'])
            if os.environ.get('KSTOP') == '1':
                P.emit()
                return nc
            with ExitStack() as es1:
                MU = P.sb(es1, 'MU', [128, 1792])
                OMM = P.sb(es1, 'OMM', [128, 1792])
                o, n = ROWS['mu']
                P.dma(MU[:], rows_d[:, o:o + n].partition_broadcast(128), writes=['MU'])
                P.op('dve', lambda e: e.tensor_scalar(out=OMM[:], in0=MU[:], scalar1=-1.0, scalar2=1.0,
                                                      op0=ALU.mult, op1=ALU.add), reads=['MU'], writes=['OMM'])
                stg = [P.sb(es1, 'wstg', [128, 1792]) for _ in range(2)]
                for dc in range(8):
                    s = stg[dc % 2]
                    k = 'wstg%d' % (dc % 2)
                    P.dma(s[:], w_in[dc * 128:(dc + 1) * 128, 0:1792], writes=[k])
                    P.op('dve', lambda e, s=s, dc=dc: e.tensor_tensor(out=wA[:, dc, :], in0=s[:], in1=OMM[:], op=ALU.mult),
                         reads=[k, 'OMM'], writes=['wA'])
                    P.op('pool', lambda e, s=s, dc=dc: e.tensor_tensor(out=wB[:, dc, :], in0=s[:], in1=MU[:], op=ALU.mult),
                         reads=[k, 'MU'], writes=['wB'])
                if os.environ.get('KSTOP') == '2':
                    P.emit()
                    return nc
                ls = P.sb(es1, 'lstg', [128, 512])
                P.dma(ls[0:64, :], w_up_d, writes=['lstg'])
                P.dma(ls[64:128, :], a_up_d, writes=['lstg'])
                P.op('pool', lambda e: e.memset(LW[:], 0.0), writes=['LW'])
                P.op('pool', lambda e: e.memset(LW2[:], 0.0), writes=['LW'])
                P.op('dve', lambda e: e.tensor_copy(out=LW[0:64, :], in_=ls[0:64, :]), reads=['lstg'], writes=['LW'])
                P.op('dve', lambda e: e.tensor_copy(out=LW2[64:128, :], in_=ls[64:128, :]), reads=['lstg'], writes=['LW'])
                gs = P.sb(es1, 'gstg', [128, 512])
                P.dma(gs[:], g_up_d, writes=['gstg'])
                P.op('dve', lambda e: e.tensor_copy(out=GU[:], in_=gs[:]), reads=['gstg'], writes=['GU'])
                P.flush()
            if os.environ.get('KSTOP') == '3':
                P.emit()
                return nc
            if os.environ.get('KNOBAR') != '1':
                P.barrier()

            xstg = [P.sb(es, 'xstg', [128, NB + 1]) for _ in range(2)]
            xb = [P.sb(es, 'xb', [128, 8, NB], BF16) for _ in range(2)]
            xbs_ = [P.sb(es, 'xbs', [128, 8, NB], BF16) for _ in range(2)]
            FM = {}
            for nm in ['Rr', 'Kr', 'Vt', 'At', 'Bt', 'Kt', 'Rt', 'Rtb', 'rk', 'eG']:
                FM[nm] = [P.sb(es, nm, [128, NB], BF16 if nm in ('Vt', 'At', 'Bt', 'Kt', 'Rtb') else F32) for _ in range(4)]
            identb = P.sb(es, 'identb', [128, 128], BF16)
            P.op('dve', lambda e: e.tensor_copy(out=identb[:], in_=ident[:]), reads=['ident'], writes=['identb'])
            tmp = {nm: P.sb(es, nm, [128, NB]) for nm in ['sg', 'a', 'Gs', 'Gp', 'eGn', 'eGp', 'kk', 'sq', 'rn', 'kkn', 't1', 'kp']}
            LX = P.sb(es, 'LX', [128, NB], BF16)
            SGX = P.sb(es, 'SGX', [128, NB], BF16)
            HH = [[P.sb(es, 'H', [128, 64]) for _ in range(2)] for _ in range(4)]
            for hp in range(4):
                P.op('pool', lambda e, hp=hp: e.memset(HH[hp][0][:], 0.0), writes=['H%d_0' % hp])
            NPAR = 2
            TM = [P.sb(es, 'TM', [128, 4, 64], BF16) for _ in range(NPAR)]
            MM = [P.sb(es, 'MM', [128, 320], BF16) for _ in range(NPAR)]
            TT = [[P.sb(es, 'TT', [128, 64], BF16) for _ in range(2)] for _ in range(NPAR)]
            PP = [[P.sb(es, 'PP', [128, 128], BF16) for _ in range(2)] for _ in range(NPAR)]
            X2s = [P.sb(es, 'X2s', [128, 64], BF16) for _ in range(NPAR)]
            WW = [P.sb(es, 'WW', [128, 128]) for _ in range(NPAR)]
            Dg = [P.sb(es, 'Dg', [128, 64]) for _ in range(NPAR)]
            HDg = [P.sb(es, 'HDg', [128, 64]) for _ in range(NPAR)]
            Us = [P.sb(es, 'Us', [128, 64], BF16) for _ in range(NPAR)]
            Yb = [P.sb(es, 'Yb', [128, NCH, 64]) for _ in range(2)]
            Yc = [P.sb(es, 'Yc', [128, NCH, 64]) for _ in range(2)]
            st = [P.sb(es, 'st', [128, 4 * NCH]) for _ in range(2)]
            cfs = [P.sb(es, 'cfs', [128, NCH]) for _ in range(2)]
            yaTs = [P.sb(es, 'yaTs', [128, NB], BF16) for _ in range(2)]
            pj = [PSA.alloc(NB, 0), PSA.alloc(NB, 3)]
            pss = PSA.alloc(NB, 0)
            pl = PSA.alloc(2 * NB, 2)
            psY = [PSA.alloc(NB, 3)] * 2
            psT = [PSA.alloc(256, 4), PSA.alloc(256, 1)]
            psW = psT
            psD = [PSA.alloc(128, 4), PSA.alloc(128, 1)]
            psX = [PSA.alloc(64, 4), PSA.alloc(64, 1)]
            psU = [PSA.alloc(64, 4), PSA.alloc(64, 1)]
            psM = [PSA.alloc(320, 5), PSA.alloc(320, 7)]
            psE = [PSA.alloc(64, 5), PSA.alloc(64, 7)]
            psH = [PSA.alloc(64, 5), PSA.alloc(64, 7)]
            pgate = [PSA.alloc(NB, 6)] * 2
            pyt = [PSA.alloc(NB, 6)] * 2
            pcf = [PSA.alloc(NCH, 7)] * 2

            evq = [0]

            def evac_copy(out, in_, reads, writes):
                evq[0] += 1
                if evq[0] % 2:
                    P.op('act', lambda e: e.copy(out=out, in_=in_), reads=reads, writes=writes)
                else:
                    P.op('dve', lambda e: e.tensor_copy(out=out, in_=in_), reads=reads, writes=writes)

            for tb in range(nblk if stage != 'hgrn' else 0):
                own = tb >= ownb
                t0 = tb * NB
                xbt = xb[tb % 2]
                xbst = xbs_[tb % 2]
                xk = 'xb%d' % (tb % 2)
                for dc in range(8):
                    s = xstg[dc % 2]
                    k = 'xstg%d' % (dc % 2)
                    if tb == 0:
                        P.op('pool', lambda e, s=s: e.memset(s[:, 0:1], 0.0), writes=[k])
                        P.dma(s[:, 1:NB + 1], xT[dc * 128:(dc + 1) * 128, 0:NB], writes=[k])
                    else:
                        P.dma(s[:], xT[dc * 128:(dc + 1) * 128, t0 - 1:t0 + NB], writes=[k])
                    P.op('pool', lambda e, s=s, dc=dc, xbt=xbt: e.tensor_copy(out=xbt[:, dc, :], in_=s[:, 1:NB + 1]),
                         reads=[k], writes=[xk])
                    P.op('dve', lambda e, s=s, dc=dc, xbst=xbst: e.tensor_copy(out=xbst[:, dc, :], in_=s[:, 0:NB]),
                         reads=[k], writes=[xk])
                for pc in range(14):
                    pap, pk = pj[pc % 2]
                    for dc in range(8):
                        P.op('pe', lambda e, pap=pap, dc=dc, pc=pc, xbt=xbt: e.matmul(
                            pap, lhsT=wA[:, dc, pc * 128:(pc + 1) * 128], rhs=xbt[:, dc, :], start=(dc == 0), stop=False),
                            reads=['wA', xk], writes=[pk])
                    for dc in range(8):
                        P.op('pe', lambda e, pap=pap, dc=dc, pc=pc, xbst=xbst: e.matmul(
                            pap, lhsT=wB[:, dc, pc * 128:(pc + 1) * 128], rhs=xbst[:, dc, :], start=False, stop=(dc == 7)),
                            reads=['wB', xk], writes=[pk])
                    if pc < 12:
                        nm = ['Rr', 'Kr', 'Vt'][pc // 4]
                        hp = pc % 4
                        if nm == 'Rr' and not own:
                            pass
                        else:
                            evac_copy(FM[nm][hp][:], pap, [pk], ['%s%d' % (nm, hp)])
                    elif pc == 12:
                        P.op('act', lambda e, pap=pap: e.activation(out=LX[0:64, :], in_=pap[0:64, :], func=AF.Tanh),
                             reads=[pk], writes=['LX'])
                        P.op('dve', lambda e, pap=pap: e.tensor_copy(out=LX[64:128, :], in_=pap[64:128, :]),
                             reads=[pk], writes=['LX'])
                    else:
                        if own:
                            P.op('act', lambda e, pap=pap: e.activation(out=SGX[:], in_=pap, func=AF.Sigmoid),
                                 reads=[pk], writes=['SGX'])
                for hp in range(4):
                    Rr, Kr, Vt = FM['Rr'][hp], FM['Kr'][hp], FM['Vt'][hp]
                    At, Bt, Kt, Rt, rk, eG = (FM[n_][hp] for n_ in ['At', 'Bt', 'Kt', 'Rt', 'rk', 'eG'])
                    Rtb = FM['Rtb'][hp]
                    kn = lambda n_: '%s%d' % (n_, hp)
                    plap, plk = pl
                    P.op('pe', lambda e, hp=hp: e.matmul(plap[:, 0:NB], lhsT=LW[:, hp * 128:(hp + 1) * 128], rhs=LX[:],
                                                         start=True, stop=True), reads=['LW', 'LX'], writes=[plk])
                    P.op('pe', lambda e, hp=hp: e.matmul(plap[:, NB:2 * NB], lhsT=LW2[:, hp * 128:(hp + 1) * 128], rhs=LX[:],
                                                         start=True, stop=True), reads=['LW', 'LX'], writes=[plk])
                    sg, a, Gs, Gp, eGn, eGp, kk, sq, rn, kkn, t1, kp = (tmp[n_] for n_ in
                                                                         ['sg', 'a', 'Gs', 'Gp', 'eGn', 'eGp', 'kk', 'sq', 'rn', 'kkn', 't1', 'kp'])
                    P.op('act', lambda e, hp=hp: e.activation(out=sg[:], in_=plap[:, 0:NB], func=AF.Sigmoid, bias=pcol('w0', hp)),
                         reads=[plk, 'pv'], writes=['sg'])
                    P.op('act', lambda e, hp=hp: e.activation(out=a[:], in_=plap[:, NB:2 * NB], func=AF.Sigmoid, bias=pcol('a0', hp)),
                         reads=[plk, 'pv'], writes=['a'])
                    P.op('dve', lambda e: e.tensor_tensor_scan(out=Gs[:], data0=scanmask[:], data1=sg[:], initial=0.0,
                                                               op0=ALU.mult, op1=ALU.add), reads=['scanmask', 'sg'], writes=['Gs'])
                    P.op('pool', lambda e: e.tensor_tensor(out=Gp[:], in0=Gs[:], in1=sg[:], op=ALU.subtract),
                         reads=['Gs', 'sg'], writes=['Gp'])
                    P.op('act', lambda e, eG=eG: e.activation(out=eG[:], in_=Gs[:], func=AF.Exp, scale=-CDEC),
                         reads=['Gs'], writes=[kn('eG')])
                    P.op('act', lambda e: e.activation(out=eGn[:], in_=Gs[:], func=AF.Exp, scale=CDEC),
                         reads=['Gs'], writes=['eGn'])
                    P.op('act', lambda e: e.activation(out=eGp[:], in_=Gp[:], func=AF.Exp, scale=-CDEC),
                         reads=['Gp'], writes=['eGp'])
                    P.op('dve', lambda e, Kr=Kr, hp=hp: e.tensor_scalar(out=kk[:], in0=Kr[:], scalar1=pcol('k_k', hp), scalar2=None,
                                                                        op0=ALU.mult), reads=[kn('Kr'), 'pv'], writes=['kk'])
                    P.op('pool', lambda e: e.tensor_tensor(out=sq[:], in0=kk[:], in1=kk[:], op=ALU.mult), reads=['kk'], writes=['sq'])
                    psap, psk = pss
                    P.op('pe', lambda e: e.matmul(psap, lhsT=onesblk[:], rhs=sq[:], start=True, stop=True),
                         reads=['onesblk', 'sq'], writes=[psk])
                    P.op('act', lambda e: e.activation(out=rn[:], in_=psap, func=AF.Sqrt), reads=[psk], writes=['rn'])
                    P.op('dve', lambda e: e.tensor_scalar(out=rn[:], in0=rn[:], scalar1=1e-12, scalar2=None, op0=ALU.max),
                         reads=['rn'], writes=['rn'])
                    P.op('dve', lambda e: e.reciprocal(out=rn[:], in_=rn[:]), reads=['rn'], writes=['rn'])
                    P.op('dve', lambda e: e.tensor_tensor(out=kkn[:], in0=kk[:], in1=rn[:], op=ALU.mult),
                         reads=['kk', 'rn'], writes=['kkn'])
                    P.op('dve', lambda e, hp=hp: e.tensor_scalar(out=t1[:], in0=a[:], scalar1=-1.0, scalar2=pcol('k_a', hp),
                                                                 op0=ALU.add, op1=ALU.mult), reads=['a', 'pv'], writes=['t1'])
                    P.op('pool', lambda e: e.tensor_scalar(out=t1[:], in0=t1[:], scalar1=1.0, scalar2=None, op0=ALU.add),
                         reads=['t1'], writes=['t1'])
                    P.op('dve', lambda e, Kr=Kr: e.tensor_tensor(out=kp[:], in0=Kr[:], in1=t1[:], op=ALU.mult),
                         reads=[kn('Kr'), 't1'], writes=['kp'])
                    P.op('dve', lambda e, At=At: e.scalar_tensor_tensor(out=At[:], in0=kkn[:], scalar=-1.0, in1=eGp[:],
                                                                         op0=ALU.mult, op1=ALU.mult),
                         reads=['kkn', 'eGp'], writes=[kn('At')])
                    P.op('pool', lambda e: e.tensor_tensor(out=t1[:], in0=kkn[:], in1=a[:], op=ALU.mult),
                         reads=['kkn', 'a'], writes=['t1'])
                    P.op('pool', lambda e, Bt=Bt: e.tensor_tensor(out=Bt[:], in0=t1[:], in1=eGn[:], op=ALU.mult),
                         reads=['t1', 'eGn'], writes=[kn('Bt')])
                    P.op('dve', lambda e, Kt=Kt: e.tensor_tensor(out=Kt[:], in0=kp[:], in1=eGn[:], op=ALU.mult),
                         reads=['kp', 'eGn'], writes=[kn('Kt')])
                    if own:
                        P.op('pool', lambda e, Rt=Rt, Rr=Rr, eG=eG: e.tensor_tensor(out=Rt[:], in0=Rr[:], in1=eG[:], op=ALU.mult),
                             reads=[kn('Rr'), kn('eG')], writes=[kn('Rt')])
                        P.op('pool', lambda e, Rt=Rt, Rtb=Rtb: e.tensor_copy(out=Rtb[:], in_=Rt[:]), reads=[kn('Rt')], writes=[kn('Rtb')])
                        P.op('dve', lambda e, rk=rk, Rr=Rr, hp=hp: e.scalar_tensor_tensor(out=rk[:], in0=Rr[:], scalar=pcol('r_k', hp),
                                                                                           in1=kp[:], op0=ALU.mult, op1=ALU.mult),
                             reads=[kn('Rr'), 'kp', 'pv'], writes=[kn('rk')])
                    yb_i = hp % 2
                    def chunk_ops(c, par):
                        gc = tb * NCH + c
                        cs = slice(c * 64, (c + 1) * 64)
                        Hc, Hn = HH[hp][gc % 2], HH[hp][(gc + 1) % 2]
                        hck, hnk = 'H%d_%d' % (hp, gc % 2), 'H%d_%d' % (hp, (gc + 1) % 2)
                        tmk, mmk = 'TM%d' % par, 'MM%d' % par
                        ptap, ptk = psT[par]
                        for i_, (X, xn) in enumerate([(Vt, 'Vt'), (At, 'At'), (Bt, 'Bt'), (Kt, 'Kt')]):
                            for h2 in range(2):
                                hs = slice(64 * h2, 64 * h2 + 64)
                                P.op('pe', lambda e, X=X, hs=hs, i_=i_, cs=cs, ptap=ptap: e.matmul(
                                    ptap[hs, i_ * 64:(i_ + 1) * 64], lhsT=X[hs, cs], rhs=identb[hs, hs], start=True, stop=True),
                                    reads=[kn(xn), 'identb'], writes=[ptk])
                        evac_copy(TM[par][:].rearrange("p a b -> p (a b)"), ptap, [ptk], [tmk])
                        yield 'pre'
                        pmap, pmk = psM[par]
                        prs = [(Bt, At, 'Bt', 'At'), (At, Bt, 'At', 'Bt'), (Kt, At, 'Kt', 'At')]
                        if own:
                            prs += [(Bt, Rtb, 'Bt', 'Rtb'), (Kt, Rtb, 'Kt', 'Rtb')]
                        for i_, (L_, R_, ln, rn_) in enumerate(prs):
                            for h2 in range(2):
                                hs = slice(64 * h2, 64 * h2 + 64)
                                P.op('pe', lambda e, L_=L_, R_=R_, hs=hs, i_=i_, cs=cs, pmap=pmap: e.matmul(
                                    pmap[hs, i_ * 64:(i_ + 1) * 64], lhsT=L_[hs, cs], rhs=R_[hs, cs], start=True, stop=True),
                                    reads=[kn(ln), kn(rn_)], writes=[pmk])
                        ncol = 64 * len(prs)
                        P.op('dve', lambda e, par=par, pmap=pmap, ncol=ncol: e.tensor_tensor(
                            out=MM[par][:, 0:ncol], in0=pmap[:, 0:ncol], in1=mask320[:, 0:ncol], op=ALU.mult),
                            reads=[pmk, 'mask320'], writes=[mmk])
                        yield 'pre'
                        ttk = ['TT%d_%d' % (par, i_) for i_ in range(2)]
                        ppk = ['PP%d_%d' % (par, i_) for i_ in range(2)]
                        P.op('pool', lambda e, par=par: e.tensor_tensor(out=TT[par][0][:], in0=MM[par][:, 0:64], in1=iit[:, 0:64], op=ALU.add),
                             reads=[mmk, 'ii'], writes=[ttk[0]])
                        Pcur, Pk = MM[par][:, 0:128], mmk
                        tcur = 0
                        pdap, pdk = psD[par]
                        peap, pek = psE[par]

                        def square(Pcur, Pk, dst, dstk):
                            for h2 in range(2):
                                hs = slice(64 * h2, 64 * h2 + 64)
                                P.op('pe', lambda e, Pcur=Pcur, hs=hs: e.matmul(
                                    pdap[hs, 0:64], lhsT=Pcur[hs, 64:128], rhs=Pcur[hs, 0:64], start=True, stop=True),
                                    reads=[Pk], writes=[pdk])
                                P.op('pe', lambda e, Pcur=Pcur, hs=hs: e.matmul(
                                    pdap[hs, 64:128], lhsT=Pcur[hs, 0:64], rhs=Pcur[hs, 64:128], start=True, stop=True),
                                    reads=[Pk], writes=[pdk])
                            evac_copy(dst[:], pdap, [pdk], [dstk])
                        square(Pcur, Pk, PP[par][0], ppk[0])
                        yield 'pre'
                        Pcur, Pk = PP[par][0][:], ppk[0]
                        for lvl in range(1, 6):
                            Tc = TT[par][tcur]
                            Tn = TT[par][1 - tcur]
                            for h2 in range(2):
                                hs = slice(64 * h2, 64 * h2 + 64)
                                P.op('pe', lambda e, Tc=Tc, Pcur=Pcur, hs=hs: e.matmul(
                                    peap[hs, 0:64], lhsT=Pcur[hs, 64:128], rhs=Tc[hs, :], start=True, stop=True),
                                    reads=[ttk[tcur], Pk], writes=[pek])
                            if lvl < 5:
                                square(Pcur, Pk, PP[par][lvl % 2], ppk[lvl % 2])
                            P.op('dve', lambda e, Tc=Tc, Tn=Tn: e.tensor_tensor(out=Tn[:], in0=peap[:, 0:64], in1=Tc[:], op=ALU.add),
                                 reads=[pek, ttk[tcur]], writes=[ttk[1 - tcur]])
                            if lvl < 5:
                                Pcur, Pk = PP[par][lvl % 2][:], ppk[lvl % 2]
                            tcur = 1 - tcur
                            yield 'pre'
                        Tt = TT[par][tcur]
                        tk = ttk[tcur]
                        pxap, pxk = psX[par]
                        for h2 in range(2):
                            hs = slice(64 * h2, 64 * h2 + 64)
                            P.op('pe', lambda e, hs=hs, par=par, pxap=pxap: e.matmul(
                                pxap[hs, :], lhsT=MM[par][hs, 128:192], rhs=TM[par][hs, 0, :], start=True, stop=True),
                                reads=[mmk, tmk], writes=[pxk])
                        evac_copy(X2s[par][:], pxap, [pxk], ['X2s%d' % par])
                        yield 'pre'
                        pwap, pwk = psW[par]
                        for h2 in range(2):
                            hs = slice(64 * h2, 64 * h2 + 64)
                            P.op('pe', lambda e, hs=hs, par=par, Tt=Tt, pwap=pwap: e.matmul(
                                pwap[hs, 0:64], lhsT=Tt[hs, :], rhs=X2s[par][hs, :], start=True, stop=True),
                                reads=[tk, 'X2s%d' % par], writes=[pwk])
                            P.op('pe', lambda e, hs=hs, par=par, Tt=Tt, pwap=pwap: e.matmul(
                                pwap[hs, 64:128], lhsT=TM[par][hs, 1, :], rhs=Tt[hs, :], start=True, stop=True),
                                reads=[tk, tmk], writes=[pwk])
                            P.op('pe', lambda e, hs=hs, par=par, pwap=pwap: e.matmul(
                                pwap[hs, 128:192], lhsT=TM[par][hs, 3, :], rhs=TM[par][hs, 0, :], start=True, stop=True),
                                reads=[tmk], writes=[pwk])
                        wwk = 'WW%d' % par
                        evac_copy(WW[par][:], pwap[:, 0:128], [pwk], [wwk])
                        gam = eG[:, c * 64 + 63:c * 64 + 64]
                        P.op('dve', lambda e, par=par, gam=gam, pwap=pwap: e.tensor_scalar(out=Dg[par][:], in0=pwap[:, 128:192], scalar1=gam,
                                                                                            scalar2=None, op0=ALU.mult),
                             reads=[pwk, kn('eG')], writes=['Dg%d' % par])
                        yield 'pre'
                        yield 'seq'
                        P.op('dve', lambda e, par=par, gam=gam, Hc=Hc: e.scalar_tensor_tensor(
                            out=HDg[par][:], in0=Hc[:], scalar=gam, in1=Dg[par][:], op0=ALU.mult, op1=ALU.add),
                            reads=[hck, kn('eG'), 'Dg%d' % par], writes=['HDg%d' % par])
                        puap, puk = psU[par]
                        for h2 in range(2):
                            hs = slice(64 * h2, 64 * h2 + 64)
                            P.op('pe', lambda e, hs=hs, par=par, Hc=Hc, puap=puap: e.matmul(
                                puap[hs, :], lhsT=WW[par][hs, 64:128], rhs=Hc[hs, :], start=True, stop=True),
                                reads=[wwk, hck], writes=[puk])
                        P.op('dve', lambda e, par=par, puap=puap: e.tensor_tensor(out=Us[par][:], in0=puap, in1=WW[par][:, 0:64], op=ALU.add),
                             reads=[puk, wwk], writes=['Us%d' % par])
                        if own:
                            pyap, pyk = psY[yb_i]
                            for h2 in range(2):
                                hs = slice(64 * h2, 64 * h2 + 64)
                                P.op('pe', lambda e, hs=hs, Rt=Rt, cs=cs, Hc=Hc, pyap=pyap: e.matmul(
                                    pyap[hs, cs], lhsT=Rt[hs, cs], rhs=Hc[hs, :], start=True, stop=False),
                                    reads=[kn('Rt'), hck], writes=[pyk])
                                P.op('pe', lambda e, hs=hs, par=par, cs=cs, pyap=pyap: e.matmul(
                                    pyap[hs, cs], lhsT=MM[par][hs, 192:256], rhs=Us[par][hs, :], start=False, stop=False),
                                    reads=[mmk, 'Us%d' % par], writes=[pyk])
                                P.op('pe', lambda e, hs=hs, par=par, cs=cs, pyap=pyap: e.matmul(
                                    pyap[hs, cs], lhsT=MM[par][hs, 256:320], rhs=TM[par][hs, 0, :], start=False, stop=True),
                                    reads=[mmk, tmk], writes=[pyk])
                            pcap, pck = pcf[yb_i]
                            pgap, pgk = pgate[yb_i]
                            for h2 in range(2):
                                hs = slice(64 * h2, 64 * h2 + 64)
                                P.op('pe', lambda e, hs=hs, rk=rk, cs=cs, c=c, pcap=pcap: e.matmul(
                                    pcap[hs, c:c + 1], lhsT=rk[hs, cs], rhs=onesblk[hs, 64 * (hs.start // 64):64 * (hs.start // 64) + 1],
                                    start=True, stop=True), reads=[kn('rk'), 'onesblk'], writes=[pck])
                                h = 2 * hp + h2
                                P.op('pe', lambda e, hs=hs, cs=cs, h=h, pgap=pgap: e.matmul(
                                    pgap[hs, cs], lhsT=SGX[:, cs], rhs=GU[:, h * 64:(h + 1) * 64], start=True, stop=True),
                                    reads=['SGX', 'GU'], writes=[pgk])
                            P.op('pool', lambda e, par=par, c=c, yb_i=yb_i: e.tensor_copy(out=Yc[yb_i][:, c, :], in_=TM[par][:, 0, :]),
                                 reads=[tmk], writes=['Yc%d' % yb_i])
                        phap, phk = psH[par]
                        for h2 in range(2):
                            hs = slice(64 * h2, 64 * h2 + 64)
                            P.op('pe', lambda e, hs=hs, par=par, phap=phap: e.matmul(
                                phap[hs, :], lhsT=TM[par][hs, 2, :], rhs=Us[par][hs, :], start=True, stop=True),
                                reads=[tmk, 'Us%d' % par], writes=[phk])
                        P.op('dve', lambda e, par=par, gam=gam, Hn=Hn, phap=phap: e.scalar_tensor_tensor(
                            out=Hn[:], in0=phap, scalar=gam, in1=HDg[par][:], op0=ALU.mult, op1=ALU.add),
                            reads=[phk, kn('eG'), 'HDg%d' % par], writes=[hnk])
                    for c0 in range(0, NCH, 2):
                        gens = [chunk_ops(c0, 0), chunk_ops(c0 + 1, 1)]
                        pre_done = [False, False]
                        while not all(pre_done):
                            for gi_, g_ in enumerate(gens):
                                if not pre_done[gi_]:
                                    if next(g_) == 'seq':
                                        pre_done[gi_] = True
                        for g_ in gens:
                            for _ in g_:
                                pass
                    if own:
                        pyap, pyk = psY[yb_i]
                        pcap, pck = pcf[yb_i]
                        pgap, pgk = pgate[yb_i]
                        Y = Yb[yb_i]
                        yk = 'Yb%d' % yb_i
                        V3 = Yc[yb_i]
                        vk = 'Yc%d' % yb_i
                        S = st[yb_i]
                        sk = 'st%d' % yb_i
                        y3 = lambda ap: ap.rearrange("p (c v) -> p c v", v=64)
                        bc = lambda ap: ap.unsqueeze(2).to_broadcast([128, NCH, 64])
                        evac_copy(Y[:].rearrange("p c v -> p (c v)"), pyap, [pyk], [yk])
                        P.op('dve', lambda e, Y=Y, S=S: e.tensor_reduce(out=S[:, 0:NCH], in_=Y[:], axis=AX.X, op=ALU.add),
                             reads=[yk], writes=[sk])
                        P.op('dve', lambda e, S=S: e.tensor_scalar(out=S[:, 0:NCH], in0=S[:, 0:NCH], scalar1=1.0 / 64, scalar2=None, op0=ALU.mult),
                             reads=[sk], writes=[sk])
                        P.op('dve', lambda e, Y=Y, S=S: e.tensor_tensor(out=Y[:], in0=Y[:], in1=bc(S[:, 0:NCH]), op=ALU.subtract),
                             reads=[yk, sk], writes=[yk])
                        sqb = tmp['sq']
                        P.op('pool', lambda e, Y=Y: e.tensor_tensor(out=y3(sqb[:]), in0=Y[:], in1=Y[:], op=ALU.mult),
                             reads=[yk], writes=['sq'])
                        P.op('dve', lambda e, S=S: e.tensor_reduce(out=S[:, NCH:2 * NCH], in_=y3(sqb[:]), axis=AX.X, op=ALU.add),
                             reads=['sq'], writes=[sk])
                        P.op('dve', lambda e, S=S: e.tensor_scalar(out=S[:, NCH:2 * NCH], in0=S[:, NCH:2 * NCH], scalar1=1.0 / 64, scalar2=64e-5,
                                                                   op0=ALU.mult, op1=ALU.add), reads=[sk], writes=[sk])
                        P.op('act', lambda e, S=S: e.activation(out=S[:, NCH:2 * NCH], in_=S[:, NCH:2 * NCH], func=AF.Sqrt),
                             reads=[sk], writes=[sk])
                        P.op('dve', lambda e, S=S: e.reciprocal(out=S[:, NCH:2 * NCH], in_=S[:, NCH:2 * NCH]), reads=[sk], writes=[sk])
                        P.op('dve', lambda e, Y=Y, S=S: e.tensor_tensor(out=Y[:], in0=Y[:], in1=bc(S[:, NCH:2 * NCH]), op=ALU.mult),
                             reads=[yk, sk], writes=[yk])
                        gw = gnw[:, hp, :].unsqueeze(1).to_broadcast([128, NCH, 64])
                        gb = gnb[:, hp, :].unsqueeze(1).to_broadcast([128, NCH, 64])
                        P.op('pool', lambda e, Y=Y, gw=gw: e.tensor_tensor(out=Y[:], in0=Y[:], in1=gw, op=ALU.mult),
                             reads=[yk, 'gnw'], writes=[yk])
                        P.op('pool', lambda e, Y=Y, gb=gb: e.tensor_tensor(out=Y[:], in0=Y[:], in1=gb, op=ALU.add),
                             reads=[yk, 'gnb'], writes=[yk])
                        cf = cfs[yb_i]
                        ck = 'cfs%d' % yb_i
                        evac_copy(cf[:], pcap, [pck], [ck])
                        P.op('dve', lambda e, V3=V3, cf=cf: e.tensor_tensor(out=V3[:], in0=V3[:], in1=bc(cf[:]), op=ALU.mult),
                             reads=[vk, ck], writes=[vk])
                        P.op('pool', lambda e, Y=Y, V3=V3: e.tensor_tensor(out=Y[:], in0=Y[:], in1=V3[:], op=ALU.add),
                             reads=[yk, vk], writes=[yk])
                        P.op('dve', lambda e, Y=Y, pgap=pgap: e.tensor_tensor(out=Y[:].rearrange("p c v -> p (c v)"),
                                                                              in0=Y[:].rearrange("p c v -> p (c v)"), in1=pgap, op=ALU.mult),
                             reads=[yk, pgk], writes=[yk])
                        ob = tb - ownb
                        if stage == 'rwkv':
                            for c in range(NCH):
                                P.dma(ya_dbg[ob * NCH + c, hp], Y[:, c, :], reads=[yk])
                        ptap2, ptk2 = pyt[yb_i]
                        for c in range(NCH):
                            for h2 in range(2):
                                hs = slice(64 * h2, 64 * h2 + 64)
                                P.op('pe', lambda e, Y=Y, hs=hs, c=c, ptap2=ptap2: e.matmul(
                                    ptap2[hs, c * 64:(c + 1) * 64], lhsT=Y[hs, c, :], rhs=ident[hs, hs], start=True, stop=True),
                                    reads=[yk, 'ident'], writes=[ptk2])
                        yT = yaTs[yb_i]
                        ytk = 'yaTs%d' % yb_i
                        evac_copy(yT[:], ptap2, [ptk2], [ytk])
                        P.dma(yaT_d[hp * 128:(hp + 1) * 128, ob * NB:(ob + 1) * NB], yT[:], reads=[ytk], writes=['dram_yaT_%d_%d' % (hp, ob)])
            P.flush()
        if stage in ('hgrn', 'full', 'fullA'):
          P.barrier()
          with ExitStack() as es:
            PSA.reset()
            scanmask_h = P.sb(es, 'scanmask_h', [128, NB])
            iu2 = P.sb(es, 'iu2', [128, 64])
            P.dma(scanmask_h[:], scanmask_d, writes=['scanmask_h'])
            P.dma(iu2[:], iu2_d, writes=['iu2'])
            wH = P.sb(es, 'wH', [128, 8, 2048], BF16)
            NWB = P.sb(es, 'NWB', [128, 512])
            o_, n_ = ROWS['hg_nw']
            P.dma(NWB[:], rows_d[:, o_:o_ + n_].partition_broadcast(128), writes=['NWB'])
            lbt = P.sb(es, 'lbt', [128, 12])
            P.op('dve', lambda e: e.tensor_tensor(out=lbt[:, 0:4], in0=pvt[:, PV['lb0']:PV['lb0'] + 4],
                                                  in1=pvt[:, PV['lb1']:PV['lb1'] + 4], op=ALU.subtract),
                 reads=['pv'], writes=['lbt'])
            P.op('act', lambda e: e.activation(out=lbt[:, 0:4], in_=lbt[:, 0:4], func=AF.Sigmoid), reads=['lbt'], writes=['lbt'])
            P.op('dve', lambda e: e.tensor_scalar(out=lbt[:, 4:8], in0=lbt[:, 0:4], scalar1=-1.0, scalar2=1.0, op0=ALU.mult, op1=ALU.add),
                 reads=['lbt'], writes=['lbt'])
            P.op('dve', lambda e: e.tensor_scalar(out=lbt[:, 8:12], in0=lbt[:, 4:8], scalar1=-1.0, scalar2=None, op0=ALU.mult),
                 reads=['lbt'], writes=['lbt'])
            with ExitStack() as es1:
                stg = [P.sb(es1, 'whstg', [128, 2048]) for _ in range(2)]
                for dc in range(8):
                    s = stg[dc % 2]
                    k = 'whstg%d' % (dc % 2)
                    P.dma(s[:], w_in[dc * 128:(dc + 1) * 128, C_Q:C_GATE], writes=[k])
                    P.op('dve' if dc % 2 else 'pool', lambda e, s=s, dc=dc: e.tensor_copy(out=wH[:, dc, :], in_=s[:]),
                         reads=[k], writes=['wH'])
                P.flush()
            P.barrier()
            xstg = [P.sb(es, 'hxstg', [128, NB]) for _ in range(2)]
            xb = [P.sb(es, 'hxb', [128, 8, NB], BF16) for _ in range(2)]
            NSET = 4
            fm = [{nm: P.sb(es, 'h' + nm, [128, NB], BF16 if nm in ('kt', 'qtb') else F32)
                   for nm in ['q', 'sf', 'lf', 'k', 'b', 'eb', 'enb', 'qt', 'kt', 'qtb']} for _ in range(NSET)]
            identb_h = P.sb(es, 'identb_h', [128, 128], BF16)
            P.op('dve', lambda e: e.tensor_copy(out=identb_h[:], in_=ident[:]), reads=['ident'], writes=['identb_h'])
            Vtok = [[P.sb(es, 'Vtok', [128, 512], BF16) for _ in range(NB // 128)] for _ in range(2)]
            SGo = [P.sb(es, 'SGo', [128, 512]) for _ in range(NB // 128)]
            KTs = [[P.sb(es, 'KTs', [128, 128], BF16) for _ in range(2)] for _ in range(4)]
            ATs = [[P.sb(es, 'ATs', [128, 64], BF16) for _ in range(2)] for _ in range(4)]
            for h_ in range(4):
                for c2_ in range(2):
                    P.op('pool', lambda e, h_=h_, c2_=c2_: e.memset(KTs[h_][c2_][:], 0.0), writes=['KTs%d_%d' % (h_, c2_)])
            Dgs = [[P.sb(es, 'hDg', [128, 128]) for _ in range(2)] for _ in range(4)]
            SS = [[P.sb(es, 'hS', [128, 128]) for _ in range(2)] for _ in range(4)]
            for h in range(4):
                P.op('pool', lambda e, h=h: e.memset(SS[h][0][:], 0.0), writes=['hS%d_0' % h])
            Ot = P.sb(es, 'Ot', [128, 4, 128])
            Osq = P.sb(es, 'Osq', [128, 4, 128])
            ost = P.sb(es, 'ost', [128, 8])
            ybTs = P.sb(es, 'ybTs', [128, 4, 128], BF16)
            pj = [PSA.alloc(NB, 0), PSA.alloc(NB, 1)]
            pv_ = PSA.alloc(512, 2)
            pog = PSA.alloc(512, 3)
            pK = [PSA.alloc(128, 4), PSA.alloc(128, 5)]
            pA = [PSA.alloc(64, 4), PSA.alloc(64, 5)]
            pD = [PSA.alloc(256, 4), PSA.alloc(256, 5)]
            pO = [PSA.alloc(512, 6), PSA.alloc(512, 7)]
            pT = pv_
            evq2 = [0]

            def evac2(out, in_, reads, writes):
                evq2[0] += 1
                if evq2[0] % 2:
                    P.op('act', lambda e: e.copy(out=out, in_=in_), reads=reads, writes=writes)
                else:
                    P.op('dve', lambda e: e.tensor_copy(out=out, in_=in_), reads=reads, writes=writes)

            for tb in range(nblk):
                own = tb >= ownb
                ob = tb - ownb
                t0 = tb * NB
                xbt = xb[tb % 2]
                xk = 'hxb%d' % (tb % 2)
                for dc in range(8):
                    s = xstg[dc % 2]
                    k = 'hxstg%d' % (dc % 2)
                    P.dma(s[:], xT[dc * 128:(dc + 1) * 128, t0:t0 + NB], writes=[k])
                    P.op('pool' if dc % 2 else 'dve', lambda e, s=s, dc=dc, xbt=xbt: e.tensor_copy(out=xbt[:, dc, :], in_=s[:]),
                         reads=[k], writes=[xk])
                vt = Vtok[tb % 2]
                for tt in range(NB // 128):
                    vk = 'Vtok%d_%d' % (tb % 2, tt)
                    pap, pk = pv_
                    for dc in range(8):
                        P.op('pe', lambda e, pap=pap, dc=dc, tt=tt, xbt=xbt: e.matmul(
                            pap, lhsT=xbt[:, dc, tt * 128:(tt + 1) * 128], rhs=wH[:, dc, 1024:1536], start=(dc == 0), stop=(dc == 7)),
                            reads=['wH', xk], writes=[pk])
                    evac2(vt[tt][:], pap, [pk], [vk])
                    if own:
                        pap, pk = pog
                        for dc in range(8):
                            P.op('pe', lambda e, pap=pap, dc=dc, tt=tt, xbt=xbt: e.matmul(
                                pap, lhsT=xbt[:, dc, tt * 128:(tt + 1) * 128], rhs=wH[:, dc, 1536:2048], start=(dc == 0), stop=(dc == 7)),
                                reads=['wH', xk], writes=[pk])
                        P.op('act', lambda e, pap=pap, tt=tt: e.activation(out=SGo[tt][:], in_=pap, func=AF.Sigmoid),
                             reads=[pk], writes=['SGo%d' % tt])
                for h in range(4):
                    F = fm[h % NSET]
                    fk = lambda n_: 'h%s%d' % (n_, h % NSET)
                    pap, pk = pj[0]
                    if own:
                        for dc in range(8):
                            P.op('pe', lambda e, pap=pap, dc=dc, h=h, xbt=xbt: e.matmul(
                                pap, lhsT=wH[:, dc, h * 128:(h + 1) * 128], rhs=xbt[:, dc, :], start=(dc == 0), stop=(dc == 7)),
                                reads=['wH', xk], writes=[pk])
                        P.op('act', lambda e, pap=pap, F=F: e.activation(out=F['q'][:], in_=pap, func=AF.Silu), reads=[pk], writes=[fk('q')])
                    pap, pk = pj[1]
                    for dc in range(8):
                        P.op('pe', lambda e, pap=pap, dc=dc, h=h, xbt=xbt: e.matmul(
                            pap, lhsT=wH[:, dc, 512 + h * 128:512 + (h + 1) * 128], rhs=xbt[:, dc, :], start=(dc == 0), stop=(dc == 7)),
                            reads=['wH', xk], writes=[pk])
                    P.op('act', lambda e, pap=pap, F=F: e.activation(out=F['sf'][:], in_=pap, func=AF.Sigmoid), reads=[pk], writes=[fk('sf')])
                    P.op('act', lambda e, F=F, h=h: e.activation(out=F['lf'][:], in_=F['sf'][:], func=AF.Ln, bias=lbt[:, h:h + 1],
                                                                 scale=lbt[:, 4 + h:5 + h]), reads=[fk('sf'), 'lbt'], writes=[fk('lf')])
                    P.op('dve', lambda e, F=F, h=h: e.tensor_scalar(out=F['k'][:], in0=F['sf'][:], scalar1=lbt[:, 8 + h:9 + h],
                                                                    scalar2=lbt[:, 4 + h:5 + h], op0=ALU.mult, op1=ALU.add),
                         reads=[fk('sf'), 'lbt'], writes=[fk('k')])
                    P.op('dve', lambda e, F=F: e.tensor_tensor_scan(out=F['b'][:], data0=scanmask_h[:], data1=F['lf'][:], initial=0.0,
                                                                    op0=ALU.mult, op1=ALU.add), reads=['scanmask_h', fk('lf')], writes=[fk('b')])
                    P.op('act', lambda e, F=F: e.activation(out=F['eb'][:], in_=F['b'][:], func=AF.Exp), reads=[fk('b')], writes=[fk('eb')])
                    P.op('act', lambda e, F=F: e.activation(out=F['enb'][:], in_=F['b'][:], func=AF.Exp, scale=-1.0),
                         reads=[fk('b')], writes=[fk('enb')])
                    P.op('pool', lambda e, F=F: e.tensor_tensor(out=F['kt'][:], in0=F['k'][:], in1=F['enb'][:], op=ALU.mult),
                         reads=[fk('k'), fk('enb')], writes=[fk('kt')])
                    if own:
                        P.op('pool', lambda e, F=F: e.tensor_tensor(out=F['qt'][:], in0=F['q'][:], in1=F['eb'][:], op=ALU.mult),
                             reads=[fk('q'), fk('eb')], writes=[fk('qt')])
                        P.op('pool', lambda e, F=F: e.tensor_copy(out=F['qtb'][:], in_=F['qt'][:]), reads=[fk('qt')], writes=[fk('qtb')])
                for cp in range(NB // 128):
                    vk = 'Vtok%d_%d' % (tb % 2, cp)
                    for c2 in range(2):
                      for h in range(4):
                            F = fm[h]
                            fk = lambda n_, h=h: 'h%s%d' % (n_, h)
                            c = 2 * cp + c2
                            gc = tb * NCH + c
                            cs = slice(c * 64, (c + 1) * 64)
                            ps_ = slice(64 * c2, 64 * c2 + 64)
                            Sc, Sn = SS[h][gc % 2], SS[h][(gc + 1) % 2]
                            sck, snk = 'hS%d_%d' % (h, gc % 2), 'hS%d_%d' % (h, (gc + 1) % 2)
                            kts, ktk = KTs[h][c2], 'KTs%d_%d' % (h, c2)
                            ats, atk = ATs[h][c2], 'ATs%d_%d' % (h, c2)
                            dgs, dgk = Dgs[h][c2], 'hDg%d_%d' % (h, c2)
                            pkap, pkk = pK[h % 2]
                            P.op('pe', lambda e, F=F, cs=cs, ps_=ps_, pkap=pkap: e.matmul(
                                pkap[ps_, :], lhsT=F['kt'][:, cs], rhs=identb_h[:], start=True, stop=True),
                                reads=[fk('kt'), 'identb_h'], writes=[pkk])
                            evac2(kts[ps_, :], pkap[ps_, :], [pkk], [ktk])
                            if own:
                                paap, pak = pA[h % 2]
                                P.op('pe', lambda e, F=F, cs=cs, ps_=ps_, paap=paap: e.matmul(
                                    paap[ps_, :], lhsT=F['kt'][:, cs], rhs=F['qtb'][:, cs], start=True, stop=True),
                                    reads=[fk('kt'), fk('qtb')], writes=[pak])
                                P.op('dve', lambda e, ats=ats, ps_=ps_, paap=paap: e.tensor_tensor(
                                    out=ats[ps_, :], in0=paap[ps_, :], in1=iu2[ps_, :], op=ALU.mult),
                                    reads=[pak, 'iu2'], writes=[atk])
                            pdap, pdk = pD[h % 2]
                            P.op('pe', lambda e, kts=kts, ps_=ps_, cp=cp, h=h, c2=c2, pdap=pdap, vt=vt: e.matmul(
                                pdap[:, c2 * 128:(c2 + 1) * 128], lhsT=kts[:, :], rhs=vt[cp][:, h * 128:(h + 1) * 128],
                                start=True, stop=True), reads=[ktk, vk], writes=[pdk])
                            gl = F['eb'][:, c * 64 + 63:c * 64 + 64]
                            P.op('dve', lambda e, dgs=dgs, pdap=pdap, c2=c2, gl=gl: e.tensor_scalar(
                                out=dgs[:], in0=pdap[:, c2 * 128:(c2 + 1) * 128], scalar1=gl, scalar2=None, op0=ALU.mult),
                                reads=[pdk, fk('eb')], writes=[dgk])
                            if own:
                                poap, pok = pO[cp]
                                P.op('pe', lambda e, ats=ats, ps_=ps_, cp=cp, h=h, poap=poap, vt=vt: e.matmul(
                                    poap[ps_, h * 128:(h + 1) * 128], lhsT=ats[ps_, :], rhs=vt[cp][ps_, h * 128:(h + 1) * 128],
                                    start=True, stop=False), reads=[atk, vk], writes=[pok])
                                P.op('pe', lambda e, F=F, cs=cs, ps_=ps_, h=h, Sc=Sc, poap=poap: e.matmul(
                                    poap[ps_, h * 128:(h + 1) * 128], lhsT=F['qt'][:, cs], rhs=Sc[:], start=False, stop=True),
                                    reads=[fk('qt'), sck], writes=[pok])
                            P.op('dve', lambda e, Sc=Sc, Sn=Sn, gl=gl, dgs=dgs: e.scalar_tensor_tensor(
                                out=Sn[:], in0=Sc[:], scalar=gl, in1=dgs[:], op0=ALU.mult, op1=ALU.add),
                                reads=[sck, fk('eb'), dgk], writes=[snk])
                if own:
                    for cp in range(NB // 128):
                        poap, pok = pO[cp]
                        evac2(Ot[:].rearrange("p h v -> p (h v)"), poap, [pok], ['Ot'])
                        P.op('pool', lambda e: e.tensor_tensor(out=Osq[:], in0=Ot[:], in1=Ot[:], op=ALU.mult), reads=['Ot'], writes=['Osq'])
                        P.op('dve', lambda e: e.tensor_reduce(out=ost[:, 0:4], in_=Osq[:], axis=AX.X, op=ALU.add), reads=['Osq'], writes=['ost'])
                        P.op('dve', lambda e: e.tensor_scalar(out=ost[:, 0:4], in0=ost[:, 0:4], scalar1=1.0 / 128, scalar2=1e-6,
                                                              op0=ALU.mult, op1=ALU.add), reads=['ost'], writes=['ost'])
                        P.op('act', lambda e: e.activation(out=ost[:, 0:4], in_=ost[:, 0:4], func=AF.Sqrt), reads=['ost'], writes=['ost'])
                        P.op('dve', lambda e: e.reciprocal(out=ost[:, 0:4], in_=ost[:, 0:4]), reads=['ost'], writes=['ost'])
                        P.op('dve', lambda e: e.tensor_tensor(out=Ot[:], in0=Ot[:], in1=ost[:, 0:4].unsqueeze(2).to_broadcast([128, 4, 128]),
                                                              op=ALU.mult), reads=['Ot', 'ost'], writes=['Ot'])
                        P.op('pool', lambda e: e.tensor_tensor(out=Ot[:].rearrange("p h v -> p (h v)"), in0=Ot[:].rearrange("p h v -> p (h v)"),
                                                               in1=NWB[:], op=ALU.mult), reads=['Ot', 'NWB'], writes=['Ot'])
                        P.op('dve', lambda e, cp=cp: e.tensor_tensor(out=Ot[:].rearrange("p h v -> p (h v)"), in0=Ot[:].rearrange("p h v -> p (h v)"),
                                                                     in1=SGo[cp][:], op=ALU.mult), reads=['Ot', 'SGo%d' % cp], writes=['Ot'])
                        tok0 = ob * NB + cp * 128
                        if stage == 'hgrn':
                            P.dma(yb_dbg[tok0:tok0 + 128, :], Ot[:].rearrange("p h v -> p (h v)"), reads=['Ot'])
                        ptap, ptk = pT
                        for h in range(4):
                            P.op('pe', lambda e, h=h, ptap=ptap: e.matmul(ptap[:, h * 128:(h + 1) * 128], lhsT=Ot[:, h, :], rhs=ident[:],
                                                                         start=True, stop=True), reads=['Ot', 'ident'], writes=[ptk])
                        evac2(ybTs[:].rearrange("p h t -> p (h t)"), ptap, [ptk], ['ybTs'])
                        P.dma(ybT_d.rearrange("(h p) t -> p h t", p=128)[:, :, tok0:tok0 + 128], ybTs[:], reads=['ybTs'],
                              writes=['dram_ybT_%d' % (tok0 // 128)])
            P.flush()
        if stage == 'fullA':
            with ExitStack() as es:
                P.barrier()
                bt = P.sb(es, 'bt', [128, 4, TOWN], BF16)
                for src, dst, kp_ in [(yaT_d, yaT_o, 'a'), (ybT_d, ybT_o, 'b')]:
                    rk_ = (['dram_yaT_%d_%d' % (hp_, o__) for hp_ in range(4) for o__ in range(nblk - ownb)] if kp_ == 'a'
                           else ['dram_ybT_%d' % j_ for j_ in range(TOWN // 128)])
                    P.dma(bt[:], src.rearrange("(c p) t -> p c t", p=128), reads=rk_, writes=['bt'])
                    P.dma(dst.rearrange("(c p) t -> p c t", p=128), bt[:], reads=['bt'], writes=['dram_o' + kp_])
                P.flush()
        if stage == 'full':
          NT_H = 1024
          own_off = T - TOWN
          with ExitStack() as esB:
            P.barrier()
            PSA.reset()
            ACC = P.sb(esB, 'ACC', [128, NT_H // 128, D])
            x1T = P.sb(esB, 'x1T', [128, 8, NT_H], BF16)
            WtAll = P.sb(esB, 'WtAll', [128, NT_H // 128, 32])
            LNB = P.sb(esB, 'LNB', [128, 2, D])

            def load_ln(names):
                for i_, nm in enumerate(names):
                    o_, n_ = ROWS[nm]
                    P.dma(LNB[:, i_, :], rows_d[:, o_:o_ + n_].partition_broadcast(128), writes=['LNB'])
            RBB = P.sb(esB, 'RBB', [128, 36])
            o_, n_ = ROWS['rb']
            P.dma(RBB[:], rows_d[:, o_:o_ + n_].partition_broadcast(128), writes=['RBB'])
            evq3 = [0]

            def evac3(out, in_, reads, writes):
                evq3[0] += 1
                if evq3[0] % 2:
                    P.op('act', lambda e: e.copy(out=out, in_=in_), reads=reads, writes=writes)
                else:
                    P.op('dve', lambda e: e.tensor_copy(out=out, in_=in_), reads=reads, writes=writes)

            lc_n = [0]

            def load_cast(stg_, dst, dkey, src_rows, ncols, nchunk, col0=0, cast_engs=('dve', 'pool')):
                n_ = lc_n[0]
                for c in range(nchunk):
                    for c1 in range(0, ncols, 1024):
                        s_ = stg_[n_ % 2]
                        k_ = 'lcs%d' % (n_ % 2)
                        P.dma(s_[:], src_rows(c)[:, col0 + c1:col0 + c1 + 1024], writes=[k_])
                        P.op(cast_engs[n_ % len(cast_engs)], lambda e, s_=s_, c=c, c1=c1: e.tensor_copy(out=dst[:, c, c1:c1 + 1024], in_=s_[:]),
                             reads=[k_], writes=[dkey])
                        n_ += 1
                lc_n[0] = n_

            def layer_norm_tile(Zt, zk, gi, stt, sq_t):
                P.op('dve', lambda e: e.tensor_reduce(out=stt[:, 0:1], in_=Zt[:], axis=AX.X, op=ALU.add), reads=[zk], writes=['stt'])
                P.op('dve', lambda e: e.tensor_scalar(out=stt[:, 0:1], in0=stt[:, 0:1], scalar1=1.0 / D, scalar2=None, op0=ALU.mult),
                     reads=['stt'], writes=['stt'])
                P.op('dve', lambda e: e.tensor_scalar(out=Zt[:], in0=Zt[:], scalar1=stt[:, 0:1], scalar2=None, op0=ALU.subtract),
                     reads=[zk, 'stt'], writes=[zk])
                P.op('pool', lambda e: e.tensor_tensor(out=sq_t[:], in0=Zt[:], in1=Zt[:], op=ALU.mult), reads=[zk], writes=['sq_t'])
                P.op('dve', lambda e: e.tensor_reduce(out=stt[:, 1:2], in_=sq_t[:], axis=AX.X, op=ALU.add), reads=['sq_t'], writes=['stt'])
                P.op('dve', lambda e: e.tensor_scalar(out=stt[:, 1:2], in0=stt[:, 1:2], scalar1=1.0 / D, scalar2=1e-5, op0=ALU.mult, op1=ALU.add),
                     reads=['stt'], writes=['stt'])
                P.op('act', lambda e: e.activation(out=stt[:, 1:2], in_=stt[:, 1:2], func=AF.Sqrt), reads=['stt'], writes=['stt'])
                P.op('dve', lambda e: e.reciprocal(out=stt[:, 1:2], in_=stt[:, 1:2]), reads=['stt'], writes=['stt'])
                P.op('dve', lambda e: e.tensor_scalar(out=Zt[:], in0=Zt[:], scalar1=stt[:, 1:2], scalar2=None, op0=ALU.mult),
                     reads=[zk, 'stt'], writes=[zk])
                P.op('pool', lambda e: e.tensor_tensor(out=Zt[:], in0=Zt[:], in1=LNB[:, 0, :], op=ALU.mult), reads=[zk, 'LNB'], writes=[zk])
                P.op('dve', lambda e: e.tensor_tensor(out=Zt[:], in0=Zt[:], in1=LNB[:, 1, :], op=ALU.add), reads=[zk, 'LNB'], writes=[zk])

            def do_half(hf):
                tk0 = hf * NT_H
                with ExitStack() as es:
                    P.barrier()
                    PSA.reset()
                    WA = P.sb(es, 'WA', [128, 4, D], BF16)
                    WB = P.sb(es, 'WB', [128, 4, D], BF16)
                    WG = P.sb(es, 'WG', [128, 8, 2048], BF16)
                    WO = P.sb(es, 'WO', [128, 8, D], BF16)
                    RW = P.sb(es, 'RW', [128, 8, 36])
                    load_ln(['ln1_g', 'ln1_b'])
                    lcstg = [P.sb(es, 'lcs', [128, 1024]) for _ in range(2)]
                    load_cast(lcstg, WA, 'WA', lambda c: w_a_out_d[c * 128:(c + 1) * 128, :], D, 4)
                    load_cast(lcstg, WB, 'WB', lambda c: w_b_out_d[c * 128:(c + 1) * 128, :], D, 4)
                    load_cast(lcstg, WG, 'WG', lambda c: w_in[c * 128:(c + 1) * 128, :], 2048, 8, col0=C_GATE)
                    load_cast(lcstg, WO, 'WO', lambda c: w_o_d[c * 128:(c + 1) * 128, :], D, 8)
                    for c in range(8):
                        P.dma(RW[:, c, :], rw_d[c * 128:(c + 1) * 128, :], writes=['RW'])
                    yaTb = P.sb(es, 'yaTb', [128, 4, 512], BF16)
                    ybTb = P.sb(es, 'ybTb', [128, 4, 512], BF16)
                    xgs = [P.sb(es, 'xgs', [128, 512]) for _ in range(2)]
                    xgb = P.sb(es, 'xgb', [128, 8, 512], BF16)
                    sga = P.sb(es, 'sga', [128, 512])
                    sgb = P.sb(es, 'sgb', [128, 512])
                    m1 = P.sb(es, 'm1', [128, 512])
                    m2 = P.sb(es, 'm2', [128, 512])
                    mT = P.sb(es, 'mT', [128, 8, 512], BF16)
                    xo_t = P.sb(es, 'xo_t', [128, D])
                    Zt = P.sb(es, 'Zt', [128, D])
                    sq_t = P.sb(es, 'sq_t', [128, D])
                    stt = P.sb(es, 'stt', [128, 2])
                    x1Tf = P.sb(es, 'x1Tf', [128, 8, 128])
                    Lg = P.sb(es, 'Lg', [128, 36])
                    rt = P.sb(es, 'rt', [128, 64])
                    psA = PSA.alloc(512, 0)
                    psB = PSA.alloc(512, 1)
                    psGa = PSA.alloc(512, 2)
                    psGb = PSA.alloc(512, 3)
                    psO = [PSA.alloc(512, 4), PSA.alloc(512, 5)]
                    psTr = [PSA.alloc(512, 6), PSA.alloc(512, 7)]
                    psR = psA
                    for tbk in range(NT_H // 512):
                        tb0 = tk0 + tbk * 512
                        P.dma(yaTb[:], yaT_d.rearrange("(c p) t -> p c t", p=128)[:, :, tb0:tb0 + 512],
                              reads=['dram_yaT_%d_%d' % (hp_, tb0 // NB + j_) for hp_ in range(4) for j_ in range(512 // NB)], writes=['yaTb'])
                        P.dma(ybTb[:], ybT_d.rearrange("(c p) t -> p c t", p=128)[:, :, tb0:tb0 + 512],
                              reads=['dram_ybT_%d' % (tb0 // 128 + j_) for j_ in range(4)], writes=['ybTb'])
                        for dc in range(8):
                            s_ = xgs[dc % 2]
                            k_ = 'xgs%d' % (dc % 2)
                            P.dma(s_[:], xT[dc * 128:(dc + 1) * 128, own_off + tb0:own_off + tb0 + 512], writes=[k_])
                            P.op('pool' if dc % 2 else 'dve', lambda e, s_=s_, dc=dc: e.tensor_copy(out=xgb[:, dc, :], in_=s_[:]),
                                 reads=[k_], writes=['xgb'])
                        for dco in range(8):
                            cso = slice(dco * 128, (dco + 1) * 128)
                            for fc in range(4):
                                P.op('pe', lambda e, fc=fc, cso=cso: e.matmul(psA[0], lhsT=WA[:, fc, cso], rhs=yaTb[:, fc, :],
                                                                             start=(fc == 0), stop=(fc == 3)), reads=['WA', 'yaTb'], writes=[psA[1]])
                            for fc in range(4):
                                P.op('pe', lambda e, fc=fc, cso=cso: e.matmul(psB[0], lhsT=WB[:, fc, cso], rhs=ybTb[:, fc, :],
                                                                             start=(fc == 0), stop=(fc == 3)), reads=['WB', 'ybTb'], writes=[psB[1]])
                            for dc in range(8):
                                P.op('pe', lambda e, dc=dc, dco=dco: e.matmul(psGa[0], lhsT=WG[:, dc, dco * 128:(dco + 1) * 128], rhs=xgb[:, dc, :],
                                                                             start=(dc == 0), stop=(dc == 7)), reads=['WG', 'xgb'], writes=[psGa[1]])
                            for dc in range(8):
                                P.op('pe', lambda e, dc=dc, dco=dco: e.matmul(psGb[0], lhsT=WG[:, dc, 1024 + dco * 128:1024 + (dco + 1) * 128],
                                                                             rhs=xgb[:, dc, :], start=(dc == 0), stop=(dc == 7)),
                                     reads=['WG', 'xgb'], writes=[psGb[1]])
                            P.op('act', lambda e: e.activation(out=sga[:], in_=psGa[0], func=AF.Sigmoid), reads=[psGa[1]], writes=['sga'])
                            P.op('act', lambda e: e.activation(out=sgb[:], in_=psGb[0], func=AF.Sigmoid), reads=[psGb[1]], writes=['sgb'])
                            P.op('dve', lambda e: e.tensor_tensor(out=m1[:], in0=psA[0], in1=sga[:], op=ALU.mult), reads=[psA[1], 'sga'], writes=['m1'])
                            P.op('dve', lambda e: e.tensor_tensor(out=m2[:], in0=psB[0], in1=sgb[:], op=ALU.mult), reads=[psB[1], 'sgb'], writes=['m2'])
                            P.op('pool', lambda e, dco=dco: e.tensor_tensor(out=mT[:, dco, :], in0=m1[:], in1=m2[:], op=ALU.add),
                                 reads=['m1', 'm2'], writes=['mT'])
                        for tt in range(4):
                            tile_i = tbk * 4 + tt
                            trow = tb0 + tt * 128
                            P.dma(xo_t[:], xo_d[trow:trow + 128, :], writes=['xo_t'])
                            for hh in range(2):
                                for dc in range(8):
                                    P.op('pe', lambda e, dc=dc, tt=tt, hh=hh: e.matmul(
                                        psO[hh][0], lhsT=mT[:, dc, tt * 128:(tt + 1) * 128], rhs=WO[:, dc, hh * 512:(hh + 1) * 512],
                                        start=(dc == 0), stop=(dc == 7)), reads=['mT', 'WO'], writes=[psO[hh][1]])
                                P.op('dve', lambda e, hh=hh: e.scalar_tensor_tensor(
                                    out=Zt[:, hh * 512:(hh + 1) * 512], in0=xo_t[:, hh * 512:(hh + 1) * 512], scalar=ALPHA, in1=psO[hh][0],
                                    op0=ALU.mult, op1=ALU.add), reads=['xo_t', psO[hh][1]], writes=['Zt'])
                            layer_norm_tile(Zt, 'Zt', 0, stt, sq_t)
                            P.op('act', lambda e, tile_i=tile_i: e.mul(out=ACC[:, tile_i, :], in_=Zt[:], mul=ALPHA), reads=['Zt'], writes=['ACC%d' % tile_i])
                            for dc in range(8):
                                pt = psTr[dc // 4]
                                P.op('pe', lambda e, dc=dc, pt=pt: e.matmul(pt[0][:, (dc % 4) * 128:(dc % 4 + 1) * 128],
                                                                            lhsT=Zt[:, dc * 128:(dc + 1) * 128], rhs=ident[:], start=True, stop=True),
                                     reads=['Zt', 'ident'], writes=[pt[1]])
                            for q in range(2):
                                pt = psTr[q]
                                P.op('act', lambda e, q=q, pt=pt: e.copy(out=x1Tf[:, q * 4:(q + 1) * 4, :].rearrange("p a t -> p (a t)"), in_=pt[0]),
                                     reads=[pt[1]], writes=['x1Tf'])
                                P.op('dve', lambda e, q=q, pt=pt, tile_i=tile_i: e.tensor_copy(
                                    out=x1T[:, q * 4:(q + 1) * 4, tile_i * 128:(tile_i + 1) * 128],
                                    in_=pt[0].rearrange("p (a t) -> p a t", t=128)), reads=[pt[1]], writes=['x1T'])
                            for dc in range(8):
                                P.op('pe', lambda e, dc=dc: e.matmul(psR[0][:, 0:36], lhsT=x1Tf[:, dc, :], rhs=RW[:, dc, :],
                                                                     start=(dc == 0), stop=(dc == 7)), reads=['x1Tf', 'RW'], writes=[psR[1]])
                            P.op('dve', lambda e: e.tensor_tensor(out=Lg[:], in0=psR[0][:, 0:36], in1=RBB[:], op=ALU.add),
                                 reads=[psR[1], 'RBB'], writes=['Lg'])
                            RT = lambda a, b: rt[:, a:b]
                            dv = lambda fn, rd=('Lg', 'rt'): P.op('dve', fn, reads=list(rd), writes=['rt'])
                            dv(lambda e: e.tensor_reduce(out=RT(0, 1), in_=Lg[:, 0:4], axis=AX.X, op=ALU.max))
                            dv(lambda e: e.tensor_scalar(out=RT(1, 2), in0=RT(0, 1), scalar1=-1.0, scalar2=None, op0=ALU.mult))
                            P.op('act', lambda e: e.activation(out=RT(16, 20), in_=Lg[:, 0:4], func=AF.Exp, bias=RT(1, 2)), reads=['Lg', 'rt'], writes=['rt'])
                            dv(lambda e: e.tensor_reduce(out=RT(2, 3), in_=RT(16, 20), axis=AX.X, op=ALU.add))
                            dv(lambda e: e.reciprocal(out=RT(3, 4), in_=RT(2, 3)))
                            dv(lambda e: e.tensor_scalar(out=RT(16, 20), in0=Lg[:, 0:4], scalar1=RT(0, 1), scalar2=None, op0=ALU.is_equal))
                            P.op('dve', lambda e: e.tensor_tensor(out=sq_t[:, 0:32].rearrange("p (g j) -> p g j", j=8),
                                                                  in0=Lg[:, 4:36].rearrange("p (g j) -> p g j", j=8),
                                                                  in1=RT(16, 20).unsqueeze(2).to_broadcast([128, 4, 8]), op=ALU.mult),
                                 reads=['Lg', 'rt'], writes=['sq_t'])
                            P.op('dve', lambda e: e.tensor_reduce(out=RT(24, 32), in_=sq_t[:, 0:32].rearrange("p (g j) -> p j g", j=8), axis=AX.X, op=ALU.add),
                                 reads=['sq_t', 'rt'], writes=['rt', 'sq_t'])
                            dv(lambda e: e.tensor_reduce(out=RT(4, 5), in_=RT(24, 32), axis=AX.X, op=ALU.max))
                            dv(lambda e: e.tensor_scalar(out=RT(32, 40), in0=RT(24, 32), scalar1=RT(4, 5), scalar2=None, op0=ALU.is_equal))
                            dv(lambda e: e.scalar_tensor_tensor(out=RT(40, 48), in0=RT(32, 40), scalar=-1e30, in1=RT(24, 32), op0=ALU.mult, op1=ALU.add))
                            dv(lambda e: e.tensor_reduce(out=RT(5, 6), in_=RT(40, 48), axis=AX.X, op=ALU.max))
                            dv(lambda e: e.tensor_scalar(out=RT(40, 48), in0=RT(40, 48), scalar1=RT(5, 6), scalar2=None, op0=ALU.is_equal))
                            dv(lambda e: e.tensor_tensor(out=RT(6, 7), in0=RT(4, 5), in1=RT(5, 6), op=ALU.subtract))
                            P.op('act', lambda e: e.activation(out=RT(7, 8), in_=RT(6, 7), func=AF.Sigmoid), reads=['rt'], writes=['rt'])
                            dv(lambda e: e.tensor_tensor(out=RT(8, 9), in0=RT(7, 8), in1=RT(3, 4), op=ALU.mult))
                            dv(lambda e: e.tensor_tensor(out=RT(9, 10), in0=RT(3, 4), in1=RT(8, 9), op=ALU.subtract))
                            dv(lambda e: e.tensor_scalar(out=RT(48, 56), in0=RT(32, 40), scalar1=RT(8, 9), scalar2=None, op0=ALU.mult))
                            dv(lambda e: e.scalar_tensor_tensor(out=RT(48, 56), in0=RT(40, 48), scalar=RT(9, 10), in1=RT(48, 56), op0=ALU.mult, op1=ALU.add))
                            P.op('dve', lambda e, tile_i=tile_i: e.tensor_tensor(out=WtAll[:, tile_i, :].rearrange("p (g j) -> p g j", j=8),
                                                                  in0=RT(16, 20).unsqueeze(2).to_broadcast([128, 4, 8]),
                                                                  in1=RT(48, 56).unsqueeze(1).to_broadcast([128, 4, 8]), op=ALU.mult),
                                 reads=['rt'], writes=['WtAll'])
                    P.flush()
                with ExitStack() as es:
                    P.barrier()
                    PSA.reset()
                    W1b = [P.sb(es, 'W1b', [128, 8, 512], BF16) for _ in range(2)]
                    W3b = [P.sb(es, 'W3b', [128, 8, 512], BF16) for _ in range(2)]
                    W2b = [P.sb(es, 'W2b', [128, 4, D], BF16) for _ in range(2)]
                    wst = [P.sb(es, 'wst', [128, 2048]) for _ in range(3)]
                    sil = [P.sb(es, 'sil', [128, 512]) for _ in range(2)]
                    hwT = [P.sb(es, 'hwT', [128, 4, 512], BF16) for _ in range(2)]
                    psH1 = [PSA.alloc(512, 1), PSA.alloc(512, 2)]
                    psH3 = [PSA.alloc(512, 3), PSA.alloc(512, 4)]
                    psF = [PSA.alloc(512, 5), PSA.alloc(512, 6)]
                    wq = [0]
                    ceng = ['pool', 'act']

                    def wload(dst, dkey, src3, nck):
                        for c in range(nck):
                            i_ = wq[0] % 3
                            wq[0] += 1
                            s_ = wst[i_]
                            k_ = 'wst%d' % i_
                            P.dma(s_[:].rearrange("p (a f) -> p a f", a=src3(c).shape[1]), src3(c), writes=[k_])
                            en = ceng[wq[0] % 2]
                            if en == 'act':
                                P.op('act', lambda e, s_=s_, c=c: e.copy(out=dst(c), in_=s_[:]), reads=[k_], writes=[dkey])
                            else:
                                P.op('pool', lambda e, s_=s_, c=c: e.tensor_copy(out=dst(c), in_=s_[:]), reads=[k_], writes=[dkey])
                    for ex in range(32):
                        pb_ = ex % 2
                        w1v = w1_d[ex].rearrange("(c p) f -> p c f", p=128)
                        w3v = w3_d[ex].rearrange("(c p) f -> p c f", p=128)
                        w2v = w2_d[ex].rearrange("(c p) d -> p c d", p=128)
                        wload(lambda c, pb_=pb_: W1b[pb_][:, c * 4:(c + 1) * 4, :].rearrange("p a f -> p (a f)"), 'W1b%d' % pb_,
                              lambda c, w1v=w1v: w1v[:, c * 4:(c + 1) * 4, :], 2)
                        wload(lambda c, pb_=pb_: W3b[pb_][:, c * 4:(c + 1) * 4, :].rearrange("p a f -> p (a f)"), 'W3b%d' % pb_,
                              lambda c, w3v=w3v: w3v[:, c * 4:(c + 1) * 4, :], 2)
                        wload(lambda c, pb_=pb_: W2b[pb_][:, c * 2:(c + 1) * 2, :].rearrange("p a f -> p (a f)"), 'W2b%d' % pb_,
                              lambda c, w2v=w2v: w2v[:, c * 2:(c + 1) * 2, :], 2)
                        for tbk in range(NT_H // 512):
                            tsl = slice(tbk * 512, (tbk + 1) * 512)
                            hw = hwT[tbk % 2]
                            hwk = 'hwT%d' % (tbk % 2)
                            for fc in range(4):
                                p1, p3 = psH1[fc % 2], psH3[fc % 2]
                                fsl = slice(fc * 128, (fc + 1) * 128)
                                for dc in range(8):
                                    P.op('pe', lambda e, dc=dc, fsl=fsl, tsl=tsl, p1=p1, pb_=pb_: e.matmul(
                                        p1[0], lhsT=W1b[pb_][:, dc, fsl], rhs=x1T[:, dc, tsl], start=(dc == 0), stop=(dc == 7)),
                                        reads=['W1b%d' % pb_, 'x1T'], writes=[p1[1]])
                                for dc in range(8):
                                    P.op('pe', lambda e, dc=dc, fsl=fsl, tsl=tsl, p3=p3, pb_=pb_: e.matmul(
                                        p3[0], lhsT=W3b[pb_][:, dc, fsl], rhs=x1T[:, dc, tsl], start=(dc == 0), stop=(dc == 7)),
                                        reads=['W3b%d' % pb_, 'x1T'], writes=[p3[1]])
                                sl_ = sil[fc % 2]
                                P.op('act', lambda e, sl_=sl_, p1=p1: e.activation(out=sl_[:], in_=p1[0], func=AF.Silu),
                                     reads=[p1[1]], writes=['sil%d' % (fc % 2)])
                                P.op('dve', lambda e, sl_=sl_, hw=hw, fc=fc, p3=p3: e.tensor_tensor(out=hw[:, fc, :], in0=p3[0], in1=sl_[:], op=ALU.mult),
                                     reads=[p3[1], 'sil%d' % (fc % 2)], writes=[hwk])
                            for tt in range(4):
                                tile_i = tbk * 4 + tt
                                for hx in range(2):
                                    pf = psF[hx]
                                    for fc in range(4):
                                        P.op('pe', lambda e, fc=fc, tt=tt, hx=hx, pf=pf, hw=hw, pb_=pb_: e.matmul(
                                            pf[0], lhsT=hw[:, fc, tt * 128:(tt + 1) * 128], rhs=W2b[pb_][:, fc, hx * 512:(hx + 1) * 512],
                                            start=(fc == 0), stop=(fc == 3)), reads=[hwk, 'W2b%d' % pb_], writes=[pf[1]])
                                    P.op('dve', lambda e, tile_i=tile_i, hx=hx, pf=pf, ex=ex: e.scalar_tensor_tensor(
                                        out=ACC[:, tile_i, hx * 512:(hx + 1) * 512], in0=pf[0], scalar=WtAll[:, tile_i, ex:ex + 1],
                                        in1=ACC[:, tile_i, hx * 512:(hx + 1) * 512], op0=ALU.mult, op1=ALU.add),
                                        reads=[pf[1], 'ACC%d' % tile_i, 'WtAll'], writes=['ACC%d' % tile_i])
                    P.flush()
                with ExitStack() as es:
                    P.barrier()
                    PSA.reset()
                    WPG = P.sb(es, 'WPG', [128, 8, D], BF16)
                    load_ln(['ln2_g', 'ln2_b'])
                    WPE = P.sb(es, 'WPE', [128, 2, D], BF16)
                    pTb = P.sb(es, 'pTb', [128, 2, NT_H], BF16)
                    lcstg = [P.sb(es, 'lcs', [128, 1024]) for _ in range(2)]
                    load_cast(lcstg, WPG, 'WPG', lambda c: w_pg_d[c * 128:(c + 1) * 128, :], D, 8)
                    load_cast(lcstg, WPE, 'WPE', lambda c: w_pe_d[c * 128:(c + 1) * 128, :], D, 2)
                    load_cast(lcstg, pTb, 'pTb', lambda c: pT_d[c * 128:(c + 1) * 128, :], NT_H, 2, col0=tk0)
                    Z2 = P.sb(es, 'Z2', [128, D])
                    sq_t = P.sb(es, 'sq_t2', [128, D])
                    stt = P.sb(es, 'stt2', [128, 2])
                    x2T = P.sb(es, 'x2T', [128, 8, 128], BF16)
                    sgp = P.sb(es, 'sgp', [128, 512])
                    ot_ = [P.sb(es, 'ot_', [128, D]) for _ in range(2)]
                    psTr = [PSA.alloc(512, 0), PSA.alloc(512, 1)]
                    psG = [PSA.alloc(512, 2), PSA.alloc(512, 3)]
                    psP = [PSA.alloc(512, 4), PSA.alloc(512, 5)]
                    for tile_i in range(NT_H // 128):
                        trow = tk0 + tile_i * 128
                        P.op('act', lambda e, tile_i=tile_i: e.copy(out=Z2[:], in_=ACC[:, tile_i, :]), reads=['ACC%d' % tile_i], writes=['Z2'])
                        layer_norm_tile(Z2, 'Z2', 2, stt, sq_t)
                        for dc in range(8):
                            pt = psTr[dc // 4]
                            P.op('pe', lambda e, dc=dc, pt=pt: e.matmul(pt[0][:, (dc % 4) * 128:(dc % 4 + 1) * 128],
                                                                        lhsT=Z2[:, dc * 128:(dc + 1) * 128], rhs=ident[:], start=True, stop=True),
                                 reads=['Z2', 'ident'], writes=[pt[1]])
                        for q in range(2):
                            pt = psTr[q]
                            evac3(x2T[:, q * 4:(q + 1) * 4, :].rearrange("p a t -> p (a t)"), pt[0], [pt[1]], ['x2T'])
                        o_t = ot_[tile_i % 2]
                        ok_ = 'ot_%d' % (tile_i % 2)
                        for hx in range(2):
                            for dc in range(8):
                                P.op('pe', lambda e, dc=dc, hx=hx: e.matmul(psG[hx][0], lhsT=x2T[:, dc, :], rhs=WPG[:, dc, hx * 512:(hx + 1) * 512],
                                                                            start=(dc == 0), stop=(dc == 7)), reads=['x2T', 'WPG'], writes=[psG[hx][1]])
                            for kc in range(2):
                                P.op('pe', lambda e, kc=kc, hx=hx, tile_i=tile_i: e.matmul(
                                    psP[hx][0], lhsT=pTb[:, kc, tile_i * 128:(tile_i + 1) * 128], rhs=WPE[:, kc, hx * 512:(hx + 1) * 512],
                                    start=(kc == 0), stop=(kc == 1)), reads=['pTb', 'WPE'], writes=[psP[hx][1]])
                            P.op('act', lambda e, hx=hx: e.activation(out=sgp[:], in_=psG[hx][0], func=AF.Sigmoid), reads=[psG[hx][1]], writes=['sgp'])
                            P.op('dve', lambda e, hx=hx, o_t=o_t: e.tensor_tensor(out=o_t[:, hx * 512:(hx + 1) * 512], in0=psP[hx][0], in1=sgp[:], op=ALU.mult),
                                 reads=[psP[hx][1], 'sgp'], writes=[ok_])
                            P.op('pool', lambda e, hx=hx, o_t=o_t: e.tensor_tensor(out=o_t[:, hx * 512:(hx + 1) * 512], in0=o_t[:, hx * 512:(hx + 1) * 512],
                                                                                   in1=Z2[:, hx * 512:(hx + 1) * 512], op=ALU.add),
                                 reads=[ok_, 'Z2'], writes=[ok_])
                        P.dma(out_d[trow:trow + 128, :], o_t[:], reads=[ok_], writes=['dram_out_%d' % trow])
                    P.flush()
            for hf_ in range(TOWN // NT_H):
                do_half(hf_)
            P.flush()
        print('NOPS', P.nops, {k: len(v) for k, v in P.streams.items()}, flush=True)
        P.emit()
    return nc


def make_in_maps(inputs):
    f32 = lambda a: np.ascontiguousarray(np.asarray(a, np.float32))
    x = f32(inputs['x'])
    consts = host_consts()
    pv = np.zeros((128, NPV), np.float32)
    pv[:, PV['w0']:PV['w0'] + 4] = colvec(inputs['rw_w0'][0], 4)
    pv[:, PV['a0']:PV['a0'] + 4] = colvec(inputs['rw_a0'][0], 4)
    pv[:, PV['k_k']:PV['k_k'] + 4] = colvec(inputs['rw_k_k'][0], 4)
    pv[:, PV['k_a']:PV['k_a'] + 4] = colvec(inputs['rw_k_a'][0], 4)
    pv[:, PV['r_k']:PV['r_k'] + 4] = colvec(np.asarray(inputs['rw_r_k'][0]).reshape(512), 4)
    pv[:, PV['lb0']:PV['lb0'] + 4] = colvec(inputs['hg_lb_logits'][0], 4)
    pv[:, PV['lb1']:PV['lb1'] + 4] = colvec(inputs['hg_lb_logits'][1], 4)
    rows = np.zeros((1, NROWS), np.float32)

    def setrow(name, v):
        o, n = ROWS[name]
        rows[0, o:o + n] = np.asarray(v, np.float32).reshape(n)
    setrow('mu', inputs['rw_mu'][0])
    setrow('gn_w', inputs['rw_gn_w'][0])
    setrow('gn_b', inputs['rw_gn_b'][0])
    setrow('hg_nw', inputs['hg_norm_w'][0])
    setrow('ln1_g', inputs['ln1_g'][0])
    setrow('ln1_b', inputs['ln1_b'][0])
    setrow('ln2_g', inputs['ln2_g'][0])
    setrow('ln2_b', inputs['ln2_b'][0])
    setrow('rb', np.concatenate([np.asarray(inputs['router_g_b'][0]).reshape(4), np.asarray(inputs['router_e_b'][0]).reshape(32)]))
    gw = np.asarray(inputs['rw_gn_w'][0], np.float32).reshape(4, 2, 1, 64)
    gb = np.asarray(inputs['rw_gn_b'][0], np.float32).reshape(4, 2, 1, 64)
    gnw_t = np.ascontiguousarray(np.broadcast_to(gw, (4, 2, 64, 64)).reshape(4, 128, 64))
    gnb_t = np.ascontiguousarray(np.broadcast_to(gb, (4, 2, 64, 64)).reshape(4, 128, 64))
    shared = dict(consts)
    shared.update(pv=pv, rows=rows, gnw_t=gnw_t, gnb_t=gnb_t,
                  w_in=f32(inputs['w_in'][0]), rw_w_up=f32(inputs['rw_w_up'][0]), rw_a_up=f32(inputs['rw_a_up'][0]),
                  rw_g_up=f32(inputs['rw_g_up'][0]),
                  w_a_out=f32(inputs['w_a_out'][0]), w_b_out=f32(inputs['w_b_out'][0]), w_o=f32(inputs['w_o'][0]),
                  rw=np.ascontiguousarray(np.concatenate([f32(inputs['router_g_w'][0]), f32(inputs['router_e_w'][0])], 1)),
                  w1=f32(inputs['w1'][0]), w3=f32(inputs['w3'][0]), w2=f32(inputs['w2'][0]),
                  w_pe=f32(inputs['w_pe'][0]), w_pg=f32(inputs['w_pg'][0]))
    in_maps = []
    for c in range(8):
        b, half = c // 2, c % 2
        xw = np.zeros((D, T), np.float32)
        if half == 1:
            xw[:, :] = x[b].T
        else:
            xw[:, T - TOWN:] = x[b, :TOWN].T
        m = dict(shared)
        m['xT'] = xw
        m['xo'] = np.ascontiguousarray(x[b, half * TOWN:(half + 1) * TOWN])
        m['pT'] = np.ascontiguousarray(np.asarray(inputs['p'], np.float32)[0, b, half * TOWN:(half + 1) * TOWN].T)
        in_maps.append(m)
    return in_maps


_NC_CACHE = {}


def kernel(**inputs):
    if 'full' not in _NC_CACHE:
        _NC_CACHE['full'] = build('full')
    nc = _NC_CACHE['full']
    in_maps = make_in_maps(inputs)
    keep = set(['xT', 'w_in', 'pv', 'rows', 'ident', 'mask320', 'iu2', 'ii', 'onesblk', 'scanmask',
                'rw_w_up', 'rw_a_up', 'rw_g_up', 'gnw_t', 'gnb_t', 'xo', 'pT', 'w_a_out', 'w_b_out', 'w_o', 'rw',
                'w1', 'w3', 'w2', 'w_pe', 'w_pg'])
    in_maps = [{k: v for k, v in m.items() if k in keep} for m in in_maps]
    res = run_bass_kernel_spmd(nc, in_maps, core_ids=list(range(8)))
    out = np.zeros((4, T, D), np.float32)
    for c in range(8):
        b, half = c // 2, c % 2
        out[b, half * TOWN:(half + 1) * TOWN] = res.results[c]['out']
    return out
```
